# Optimizing a Trainium2 kernel written in Bass

```python
import math
import jax, jax.numpy as jnp
from jax import lax
import numpy as np

D_MODEL = 1024
BATCH = 4
SEQ = 4096
DEPTH = 4

HEAD_DIM = 64
ROPE_THETA = 10000.0
NORM_EPS = 1e-6
NEG_INF = -1e30

DIFF_HEADS = 4
DIFF_QK_DIM = HEAD_DIM // 2
DIFF_V_DIM = HEAD_DIM
DENSE_BLOCK = 128
SWA_Q_HEADS = 8
SWA_KV_HEADS = 2
SWA_REP = SWA_Q_HEADS // SWA_KV_HEADS
SWA_WINDOW = 128
DIL_HEADS = 4
DIL_PATTERNS = ((128, 1), (512, 4), (2048, 16))
BAND_BLOCK = 128

A_QK = DIFF_HEADS * DIFF_QK_DIM
A_WIDTH = 4 * A_QK + DIFF_HEADS * DIFF_V_DIM
B_WIDTH = (SWA_Q_HEADS + 2 * SWA_KV_HEADS) * HEAD_DIM
C_WIDTH = 3 * DIL_HEADS * HEAD_DIM
IN_WIDTH = A_WIDTH + B_WIDTH + C_WIDTH
MIX_WIDTH = DIFF_HEADS * DIFF_V_DIM + SWA_Q_HEADS * HEAD_DIM + DIL_HEADS * HEAD_DIM

N_GROUPS = 4
EXPERTS_PER_GROUP = 4
N_EXPERTS = N_GROUPS * EXPERTS_PER_GROUP
TOP_K_IN_GROUP = 2
EXPERT_FF = 512
N_ADA = 6

kernel_name = "hymba_style_diff_swa_dilated_hmoe"

F32 = jnp.float32


def rmsnorm(x, g):
    xf = x.astype(F32)
    y = xf * lax.rsqrt(jnp.mean(xf * xf, axis=-1, keepdims=True) + NORM_EPS)
    return (y * g.astype(F32)).astype(x.dtype)


def rope_tables(positions, dim):
    inv_freq = ROPE_THETA ** (-jnp.arange(0, dim, 2, dtype=F32) / dim)
    ang = positions.astype(F32)[:, None, :, None] * inv_freq
    return jnp.cos(ang), jnp.sin(ang)


def rope(t, cos, sin):
    tf = t.astype(F32)
    h = tf.shape[-1] // 2
    t1, t2 = tf[..., :h], tf[..., h:]
    return jnp.concatenate([t1 * cos - t2 * sin, t1 * sin + t2 * cos], axis=-1).astype(t.dtype)


def split_heads(t, n, dh):
    b, s, _ = t.shape
    return t.reshape(b, s, n, dh).transpose(0, 2, 1, 3)


def merge_heads(t):
    b, h, s, dh = t.shape
    return t.transpose(0, 2, 1, 3).reshape(b, s, h * dh)


def banded_attention(q, k, v, max_dist, sink=None):
    n, g, r, length, dh = q.shape
    blk = min(BAND_BLOCK, length)
    nb = -(-length // blk)
    pad = nb * blk - length
    qb = jnp.pad(q, ((0, 0), (0, 0), (0, 0), (0, pad), (0, 0))).reshape(n, g, r, nb, blk, dh)
    kp = jnp.pad(k, ((0, 0), (0, 0), (blk, pad), (0, 0))).reshape(n, g, nb + 1, blk, dh)
    vp = jnp.pad(v, ((0, 0), (0, 0), (blk, pad), (0, 0))).reshape(n, g, nb + 1, blk, dh)
    kw = jnp.concatenate([kp[:, :, :-1], kp[:, :, 1:]], axis=3)
    vw = jnp.concatenate([vp[:, :, :-1], vp[:, :, 1:]], axis=3).astype(F32)
    s = jnp.einsum('ngrbqd,ngbkd->ngrbqk', qb, kw).astype(F32) * (dh ** -0.5)
    qi = jnp.arange(blk)[:, None] + blk
    kj = jnp.arange(2 * blk)[None, :]
    dist = qi - kj
    key_abs = (jnp.arange(nb) * blk)[:, None, None] + kj[None] - blk
    valid = (dist >= 0) & (dist <= max_dist) & (key_abs >= 0)
    s = jnp.where(valid, s, NEG_INF)
    lse = jax.nn.logsumexp(s, axis=-1)
    denom = lse if sink is None else jnp.logaddexp(lse, sink.astype(F32)[None, :, :, None, None])
    p = jnp.exp(s - denom[..., None])
    out = jnp.einsum('ngrbqk,ngbkd->ngrbqd', p, vw)
    out = out.reshape(n, g, r, nb * blk, dh)[:, :, :, :length]
    lse = lse.reshape(n, g, r, nb * blk)[..., :length]
    return out, lse


def differential_attention(q1, q2, k1, k2, v, lam):
    b, h, s, dqk = q1.shape
    nb = s // DENSE_BLOCK
    scale = dqk ** -0.5
    vf = v.astype(F32)
    kpos = jnp.arange(s)

    def to_blocks(t):
        return t.reshape(b, h, nb, DENSE_BLOCK, dqk).transpose(2, 0, 1, 3, 4)

    def one_block(args):
        q1b, q2b, start = args
        causal = (start + jnp.arange(DENSE_BLOCK))[:, None] >= kpos[None, :]
        s1 = jnp.einsum('bhqd,bhkd->bhqk', q1b, k1).astype(F32) * scale
        s2 = jnp.einsum('bhqd,bhkd->bhqk', q2b, k2).astype(F32) * scale
        p1 = jax.nn.softmax(jnp.where(causal, s1, NEG_INF), axis=-1)
        p2 = jax.nn.softmax(jnp.where(causal, s2, NEG_INF), axis=-1)
        return jnp.einsum('bhqk,bhkd->bhqd', p1 - lam * p2, vf)

    out = lax.map(one_block, (to_blocks(q1), to_blocks(q2), jnp.arange(nb) * DENSE_BLOCK))
    return out.transpose(1, 2, 0, 3, 4).reshape(b, h, s, v.shape[-1])


def dilated_branch(q, k, v, window, dilation):
    b, h, s, dh = q.shape
    length = s // dilation

    def gather(t):
        return t.reshape(b, h, length, dilation, dh).transpose(0, 3, 1, 2, 4).reshape(b * dilation, h, length, dh)

    out, lse = banded_attention(gather(q)[:, :, None], gather(k), gather(v), window // dilation)
    out = out.reshape(b, dilation, h, length, dh).transpose(0, 2, 3, 1, 4).reshape(b, h, s, dh)
    lse = lse.reshape(b, dilation, h, length).transpose(0, 2, 3, 1).reshape(b, h, s)
    return out, lse


def dilated_mixture(q, k, v):
    outs, lses = [], []
    for window, dilation in DIL_PATTERNS:
        o, l = dilated_branch(q, k, v, window, dilation)
        outs.append(o)
        lses.append(l)
    w = jax.nn.softmax(jnp.stack(lses, axis=0), axis=0)
    return jnp.sum(w[..., None] * jnp.stack(outs, axis=0), axis=0)


def hierarchical_moe(h, wg, bg, we, be, w_gate, w_up, w_down):
    b, s, d = h.shape
    t = h.reshape(-1, d)
    g_prob = jax.nn.softmax((t @ wg + bg).astype(F32), axis=-1)
    g_w, g_idx = lax.top_k(g_prob, 1)
    e_logits = (t @ we + be).astype(F32).reshape(-1, N_GROUPS, EXPERTS_PER_GROUP)
    e_sel = jnp.take_along_axis(e_logits, g_idx[:, :, None], axis=1)[:, 0]
    top_w, top_i = lax.top_k(jax.nn.softmax(e_sel, axis=-1), TOP_K_IN_GROUP)
    top_w = top_w / jnp.sum(top_w, axis=-1, keepdims=True)
    weights = g_w * top_w
    expert_id = g_idx * EXPERTS_PER_GROUP + top_i
    gates = jnp.einsum('nk,nke->ne', weights, jax.nn.one_hot(expert_id, N_EXPERTS, dtype=F32))
    hg = jnp.einsum('nd,edf->nef', t, w_gate)
    hu = jnp.einsum('nd,edf->nef', t, w_up)
    act = jax.nn.silu(hg) * hu * gates[:, :, None].astype(t.dtype)
    y = jnp.einsum('nef,efd->nd', act, w_down)
    return y.reshape(b, s, d)


def setup_inputs(seed: int = 0) -> dict:
    key = jax.random.key(seed)
    ks = jax.random.split(key, 24)

    def nrm(k, shape, scale):
        return jax.random.normal(k, shape, F32) * scale

    x = nrm(ks[0], (BATCH, SEQ, D_MODEL), 1.0)
    c = nrm(ks[1], (BATCH, D_MODEL), 1.0)
    positions = (jax.random.randint(ks[2], (BATCH, 1), 0, 4096, dtype=jnp.int32)
                 + jnp.arange(SEQ, dtype=jnp.int32)[None, :])
    return {
        "x": x,
        "c": c,
        "positions": positions,
        "ada_w": nrm(ks[3], (DEPTH, D_MODEL, N_ADA * D_MODEL), 0.5 * D_MODEL ** -0.5),
        "ada_b": nrm(ks[4], (DEPTH, N_ADA * D_MODEL), 0.02),
        "norm_mix_g": 1.0 + nrm(ks[5], (DEPTH, D_MODEL), 0.02),
        "norm_ffn_g": 1.0 + nrm(ks[6], (DEPTH, D_MODEL), 0.02),
        "w_in": nrm(ks[7], (DEPTH, D_MODEL, IN_WIDTH), D_MODEL ** -0.5),
        "w_out": nrm(ks[8], (DEPTH, MIX_WIDTH, D_MODEL), MIX_WIDTH ** -0.5),
        "diff_lambda_q1": nrm(ks[9], (DEPTH, DIFF_QK_DIM), 0.1),
        "diff_lambda_k1": nrm(ks[10], (DEPTH, DIFF_QK_DIM), 0.1),
        "diff_lambda_q2": nrm(ks[11], (DEPTH, DIFF_QK_DIM), 0.1),
        "diff_lambda_k2": nrm(ks[12], (DEPTH, DIFF_QK_DIM), 0.1),
        "diff_subln_g": 1.0 + nrm(ks[13], (DEPTH, DIFF_V_DIM), 0.02),
        "swa_sinks": nrm(ks[14], (DEPTH, SWA_Q_HEADS), 0.5),
        "router_group_w": nrm(ks[15], (DEPTH, D_MODEL, N_GROUPS), D_MODEL ** -0.5),
        "router_group_b": nrm(ks[16], (DEPTH, N_GROUPS), 0.01),
        "router_expert_w": nrm(ks[17], (DEPTH, D_MODEL, N_EXPERTS), D_MODEL ** -0.5),
        "router_expert_b": nrm(ks[18], (DEPTH, N_EXPERTS), 0.01),
        "expert_w_gate": nrm(ks[19], (DEPTH, N_EXPERTS, D_MODEL, EXPERT_FF), D_MODEL ** -0.5),
        "expert_w_up": nrm(ks[20], (DEPTH, N_EXPERTS, D_MODEL, EXPERT_FF), D_MODEL ** -0.5),
        "expert_w_down": nrm(ks[21], (DEPTH, N_EXPERTS, EXPERT_FF, D_MODEL), EXPERT_FF ** -0.5),
        "final_norm_g": 1.0 + nrm(ks[22], (D_MODEL,), 0.02),
    }


def reference(x, c, positions, ada_w, ada_b, norm_mix_g, norm_ffn_g, w_in, w_out,
              diff_lambda_q1, diff_lambda_k1, diff_lambda_q2, diff_lambda_k2, diff_subln_g,
              swa_sinks, router_group_w, router_group_b, router_expert_w, router_expert_b,
              expert_w_gate, expert_w_up, expert_w_down, final_norm_g):
    b, s, _ = x.shape
    cos64, sin64 = rope_tables(positions, HEAD_DIM)
    cos32, sin32 = rope_tables(positions, DIFF_QK_DIM)
    c_act = jax.nn.silu(c)

    for l in range(DEPTH):
        mod = (c_act @ ada_w[l] + ada_b[l]).astype(x.dtype)
        sh1, sc1, g1, sh2, sc2, g2 = [m[:, None, :] for m in jnp.split(mod, N_ADA, axis=-1)]

        h = rmsnorm(x, norm_mix_g[l]) * (1 + sc1) + sh1
        proj = h @ w_in[l]
        pa, pb, pc = jnp.split(proj, [A_WIDTH, A_WIDTH + B_WIDTH], axis=-1)

        qa1, qa2, ka1, ka2, va = jnp.split(pa, [A_QK, 2 * A_QK, 3 * A_QK, 4 * A_QK], axis=-1)
        qa1, qa2, ka1, ka2 = [rope(split_heads(t, DIFF_HEADS, DIFF_QK_DIM), cos32, sin32)
                              for t in (qa1, qa2, ka1, ka2)]
        va = split_heads(va, DIFF_HEADS, DIFF_V_DIM)
        lambda_init = 0.8 - 0.6 * math.exp(-0.3 * l)
        lam = (jnp.exp(jnp.sum(diff_lambda_q1[l].astype(F32) * diff_lambda_k1[l].astype(F32)))
               - jnp.exp(jnp.sum(diff_lambda_q2[l].astype(F32) * diff_lambda_k2[l].astype(F32)))
               + lambda_init)
        oa = differential_attention(qa1, qa2, ka1, ka2, va, lam)
        oa = rmsnorm(oa, diff_subln_g[l]) * (1.0 - lambda_init)
        oa = merge_heads(oa.astype(x.dtype))

        qb, kb, vb = jnp.split(pb, [SWA_Q_HEADS * HEAD_DIM, (SWA_Q_HEADS + SWA_KV_HEADS) * HEAD_DIM], axis=-1)
        qb = rope(split_heads(qb, SWA_Q_HEADS, HEAD_DIM), cos64, sin64)
        kb = rope(split_heads(kb, SWA_KV_HEADS, HEAD_DIM), cos64, sin64)
        vb = split_heads(vb, SWA_KV_HEADS, HEAD_DIM)
        qb = qb.reshape(b, SWA_KV_HEADS, SWA_REP, s, HEAD_DIM)
        ob, _ = banded_attention(qb, kb, vb, SWA_WINDOW - 1,
                                 sink=swa_sinks[l].reshape(SWA_KV_HEADS, SWA_REP))
        ob = merge_heads(ob.reshape(b, SWA_Q_HEADS, s, HEAD_DIM).astype(x.dtype))

        qc, kc, vc = jnp.split(pc, 3, axis=-1)
        qc = rope(split_heads(qc, DIL_HEADS, HEAD_DIM), cos64, sin64)
        kc = rope(split_heads(kc, DIL_HEADS, HEAD_DIM), cos64, sin64)
        vc = split_heads(vc, DIL_HEADS, HEAD_DIM)
        oc = merge_heads(dilated_mixture(qc, kc, vc).astype(x.dtype))

        mix = jnp.concatenate([oa, ob, oc], axis=-1) @ w_out[l]
        x = x + g1 * mix

        h = rmsnorm(x, norm_ffn_g[l]) * (1 + sc2) + sh2
        y = hierarchical_moe(h, router_group_w[l], router_group_b[l], router_expert_w[l],
                             router_expert_b[l], expert_w_gate[l], expert_w_up[l], expert_w_down[l])
        x = x + g2 * y

    return rmsnorm(x, final_norm_g)
```

```python
import math
import contextlib
import numpy as np
import concourse.bass as bass
import concourse.mybir as mybir

F32 = mybir.dt.float32
BF16 = mybir.dt.bfloat16
I32 = mybir.dt.int32
AF = mybir.ActivationFunctionType
ALU = mybir.AluOpType

ENGS = ("tensor", "vector", "scalar", "gpsimd", "sync")
NDMASEM = 12


class Prog:
    def __init__(self, nc):
        self.nc = nc
        self.ops = []
        self.last_writer = {}
        self.readers = {}

    def op(self, eng, fn, reads=(), writes=(), dma=False):
        idx = len(self.ops)
        deps = set()
        for r in reads:
            lw = self.last_writer.get(r)
            if lw is not None:
                deps.add(lw)
        for w in writes:
            lw = self.last_writer.get(w)
            if lw is not None:
                deps.add(lw)
            for rd in self.readers.get(w, ()):
                deps.add(rd)
        deps.discard(idx)
        for w in writes:
            self.last_writer[w] = idx
            self.readers[w] = []
        for r in reads:
            if r not in writes:
                self.readers.setdefault(r, []).append(idx)
        self.ops.append(dict(eng=eng, fn=fn, deps=deps, dma=dma, idx=idx, cc=False))
        return idx

    def cc(self, fn, reads=(), writes=()):
        idx = self.op("gpsimd", fn, reads, writes)
        self.ops[idx]["cc"] = True
        return idx

    def mm(self, fn, reads=(), writes=()):
        return self.op("tensor", fn, reads, writes)

    def dve(self, fn, reads=(), writes=()):
        return self.op("vector", fn, reads, writes)

    def act(self, fn, reads=(), writes=()):
        return self.op("scalar", fn, reads, writes)

    def pool(self, fn, reads=(), writes=()):
        return self.op("gpsimd", fn, reads, writes)

    def dma(self, eng, out, in_, reads=(), writes=()):
        return self.op(eng, lambda e: e.dma_start(out=out, in_=in_), reads, writes, dma=True)

    def emit(self, final_wait_ops=()):
        nc = self.nc
        ops = self.ops
        has_dep = [False] * len(ops)
        for o in ops:
            for d in o["deps"]:
                if ops[d]["eng"] == "tensor" and o["eng"] == "tensor" and not ops[d]["dma"]:
                    continue
                has_dep[d] = True
        for d in final_wait_ops:
            has_dep[d] = True
        eng_cnt = {e: 0 for e in ENGS}
        dma_cnt = {e: 0 for e in ENGS}
        dma_semval = {}
        for o in ops:
            e = o["eng"]
            if o["cc"]:
                o["sig"] = ("cc", o["idx"])
            elif o["dma"]:
                k = dma_cnt[e] % NDMASEM
                dma_cnt[e] += 1
                key = (e, k)
                dma_semval[key] = dma_semval.get(key, 0) + 16
                o["sig"] = ("dma", e, k, dma_semval[key])
            elif has_dep[o["idx"]]:
                eng_cnt[e] += 1
                o["sig"] = ("eng", e, eng_cnt[e])
            else:
                o["sig"] = None
        import contextlib
        with contextlib.ExitStack() as es:
            esem = {e: es.enter_context(nc.semaphore("s_" + e)) for e in ENGS}
            dsem = {}
            for e in ("sync", "gpsimd", "scalar"):
                for k in range(NDMASEM):
                    dsem[(e, k)] = es.enter_context(nc.semaphore("d_%s_%d" % (e, k)))
            ccsem = {o["idx"]: es.enter_context(nc.semaphore("cc_%d" % o["idx"])) for o in ops if o["cc"]}
            block = es.enter_context(nc.Block())

            def make(engname):
                def body(eng):
                    waited = {}
                    for o in ops:
                        if o["eng"] != engname:
                            continue
                        need = {}
                        for d in o["deps"]:
                            od = ops[d]
                            if od["eng"] == "tensor" and engname == "tensor" and not od["dma"]:
                                continue
                            s = od["sig"]
                            if s is None:
                                continue
                            if s[0] == "dma":
                                key = ("dma", s[1], s[2])
                                val = s[3]
                            elif s[0] == "cc":
                                key = ("cc", s[1])
                                val = 1
                            else:
                                key = ("eng", s[1])
                                val = s[2]
                            if need.get(key, 0) < val:
                                need[key] = val
                        if o["dma"]:
                            s = o["sig"]
                            if s[3] > 16:
                                key = ("dma", s[1], s[2])
                                if need.get(key, 0) < s[3] - 16:
                                    need[key] = s[3] - 16
                        for key, val in need.items():
                            if waited.get(key, 0) >= val:
                                continue
                            waited[key] = val
                            if key[0] == "dma":
                                sem = dsem[(key[1], key[2])]
                            elif key[0] == "cc":
                                sem = ccsem[key[1]]
                            else:
                                sem = esem[key[1]]
                            eng.wait_ge(sem, val)
                        ins = o["fn"](eng)
                        s = o["sig"]
                        if s is not None:
                            if s[0] == "dma":
                                ins.then_inc(dsem[(s[1], s[2])], 16)
                            elif s[0] == "cc":
                                ins.then_inc(ccsem[s[1]])
                            else:
                                ins.then_inc(esem[s[1]], 1)
                    if engname == "sync":
                        for d in final_wait_ops:
                            s = ops[d]["sig"]
                            if s[0] == "dma":
                                eng.wait_ge(dsem[(s[1], s[2])], s[3])
                            else:
                                eng.wait_ge(esem[s[1]], s[2])
                return body

            block.tensor(make("tensor"))
            block.vector(make("vector"))
            block.scalar(make("scalar"))
            block.gpsimd(make("gpsimd"))
            block.sync(make("sync"))


def _barrier(self):
    last = {}
    dmas = {}
    for o in self.ops:
        if o["dma"]:
            dmas.setdefault(o["eng"], []).append(o["idx"])
        elif o["cc"]:
            pass
        else:
            last[o["eng"]] = o["idx"]
    s = set(last.values())
    for e, lst in dmas.items():
        s.update(lst[-NDMASEM:])
    for o in self.ops:
        if o["cc"]:
            s.add(o["idx"])
    self.pending_barrier = s
    self.barrier_done = set()


_orig_op = Prog.op


def _op(self, eng, fn, reads=(), writes=(), dma=False):
    idx = _orig_op(self, eng, fn, reads, writes, dma)
    pb = getattr(self, "pending_barrier", None)
    if pb and eng not in self.barrier_done:
        self.ops[idx]["deps"] |= set(pb)
        self.barrier_done.add(eng)
    return idx


Prog.op = _op
Prog.barrier = _barrier


T = 2048
GS = 512
NM = 14
LOOK = 2
NST = 4
MKINDS = ['A0', 'A1', 'B0p', 'B0d', 'B1p', 'B1d', 'C0p', 'C0d', 'C1p', 'C1d', 'Dp', 'Dd', 'Dp', 'Dd']
BIG = 1.0e9
PI = math.pi
DEPTH = 4
X = mybir.AxisListType.X


def build_fused(depth=DEPTH):
    nc = bass.Bass("TRN2", target_bir_lowering=False)
    dt = nc.dram_tensor

    def inp(name, shape, dtype=F32):
        return dt(name, shape, dtype, kind="ExternalInput").ap()
    xT = inp("xT", [128, 8, T])
    cT = inp("cT", [128, 8])
    posb = inp("posb", [128, T], I32)
    cst = inp("cst", [128, 8])
    masks = inp("masks", [128, NM, 128], BF16)
    ada_w = inp("ada_w", [depth, 1024, 6144])
    ada_b = inp("ada_b", [128, depth, 48])
    ngm = inp("ngm", [128, depth, 8])
    ngf = inp("ngf", [128, depth, 8])
    w_in = inp("w_in", [depth, 1024, 2304])
    w_out = inp("w_out", [depth, 1024, 1024])
    lamv = inp("lamv", [128, depth, 4, 32])
    small = inp("small", [128, depth, 16])
    wr = inp("wr", [128, depth, 8, 20])
    rbias = inp("rbias", [128, depth, 20])
    ident = inp("ident", [128, 128])
    sel = inp("sel", [16, 16, 128])
    wgate = inp("wgate", [depth, 16, 1024, 512])
    wup = inp("wup", [depth, 16, 1024, 512])
    wdown = inp("wdown", [depth, 16, 512, 1024])
    fng = inp("fng", [128, 8])
    xo = dt("xo", [128, 8, T], F32, kind="ExternalOutput").ap()
    tabD = dt("tabD", [128, 4, T], F32).ap()
    QTd = dt("QTd", [128, 8, T], BF16).ap()
    KTd_h = [dt("KTd%d" % i, [128, 3 * T], BF16) for i in range(2)]
    KTg_h = [dt("KTg%d" % i, [256, 3 * T], BF16) for i in range(2)]
    Vd_h = [dt("Vd%d" % i, [T // 2, 640], BF16) for i in range(2)]
    Vg_h = [dt("Vg%d" % i, [T, 640], BF16) for i in range(2)]
    KTd3 = [h.ap().rearrange("p (c n) -> p c n", c=3) for h in KTd_h]
    KTg4 = [h.ap().rearrange("(a p) (c n) -> a p c n", a=2, c=3) for h in KTg_h]
    Vdh = [h.ap() for h in Vd_h]
    Vgh = [h.ap() for h in Vg_h]

    def ktd(c):
        return KTd3[c // 3][:, c % 3, :]

    def ktg(a, c):
        return KTg4[c // 3][a][:, c % 3, :]

    def vd_rows(n0, cnt, step=1):
        hh = n0 // 1024
        r = n0 % 1024
        return Vdh[hh][r:r + step * (cnt - 1) + 1:step, :]

    def vg_tile(a, t):
        hh = t // 8
        r = a * 1024 + (t % 8) * 128
        return Vgh[hh][r:r + 128, :]
    groups = [[0, 1], [2, 3], [4, 5], [6, 7]]

    P = Prog(nc)
    with contextlib.ExitStack() as es:
        def sb(name, shape, dtype):
            return es.enter_context(nc.sbuf_tensor(name, shape, dtype))
        x_sb = sb("x_sb", [128, 8, T], F32)
        ARN = 50432
        arena = sb("arena", [128, ARN], BF16)
        tar = sb("tar", [128, 9, 512], F32)
        off = [0]

        def carve(n):
            a = arena[:, off[0]:off[0] + n]
            off[0] += n
            assert off[0] <= ARN, off[0]
            return a
        off[0] = 0
        Wqk = carve(8 * 1664).rearrange("p (k n) -> p k n", k=8)
        Wsqk = carve(8 * 1664).rearrange("p (k n) -> p k n", k=8)
        Wkb = carve(2048).rearrange("p (k g n) -> p k g n", k=8, g=2)
        Wskb = carve(2048).rearrange("p (k g n) -> p k g n", k=8, g=2)
        Wv = carve(8 * 640).rearrange("p (k n) -> p k n", k=8)
        sqP = carve(4096).rearrange("p (k n) -> p k n", k=8)
        hTP = carve(4096).rearrange("p (k n) -> p k n", k=8)
        tab = carve(4096).bitcast(F32).rearrange("p (k n) -> p k n", k=4)
        qkst = [carve(512) for i in range(2)]
        vst = [carve(640) for i in range(2)]
        WadaP = [arena[:, i * 8192:(i + 1) * 8192].rearrange("p (k n) -> p k n", k=8) for i in range(2)]
        posi = arena[:, 16384:16384 + 1024].bitcast(I32)
        posf = arena[:, 17408:17408 + 1024].bitcast(F32)
        ang = arena[:, 18432:18432 + 1024].bitcast(F32)
        mi = arena[:, 19456:19456 + 1024].bitcast(I32)
        off[0] = 0
        q_sb = carve(4 * T).rearrange("p (c n) -> p c n", c=4)
        k_sb = carve(4 * T).rearrange("p (a c n) -> p a c n", a=2, c=2)
        v_sb = carve(2 * 16 * 260).rearrange("p (a t c) -> p a t c", a=2, t=16)
        v2_sb = carve(16 * 260).rearrange("p (t c) -> p t c", t=16)
        v8_sb = carve(16 * 260).rearrange("p (t c) -> p t c", t=16)
        mix = carve(8 * T).rearrange("p (c n) -> p c n", c=8)
        V_OFF = 8 * T
        V2_OFF = V_OFF + 2 * 16 * 260
        V8_OFF = V2_OFF + 16 * 260

        def vwide(base, tile_idx, col):
            o = base + tile_idx * 260 + col
            return arena[:, o:o + 128]
        off[0] = 0
        hT = carve(8 * T).rearrange("p (c n) -> p c n", c=8)
        wbuf = []
        for i in range(2):
            wg_ = carve(8 * 512).rearrange("p (k f) -> p k f", k=8)
            wu_ = carve(8 * 512).rearrange("p (k f) -> p k f", k=8)
            wd_ = carve(4 * 1024).rearrange("p (c d) -> p c d", c=4)
            wbuf.append((wg_, wu_, wd_))
        actb = [carve(4 * 512).rearrange("p (c n) -> p c n", c=4) for i in range(2)]
        gT = carve(2 * T).bitcast(F32)
        w1base = 8 * T + 3 * 4096
        h32 = arena[:, w1base:w1base + 8192].bitcast(F32).rearrange("p (k n) -> p k n", k=8)
        sq = arena[:, w1base + 8192:w1base + 8192 + 4096].rearrange("p (k n) -> p k n", k=8)
        Wo = arena[:, 0:8192].rearrange("p (k n) -> p k n", k=8)
        rr = tar[:, 0, :]; bcs = tar[:, 1, :]; tmpA = [tar[:, 2 + i, :] for i in range(3)]
        rstd = tar[:, 0, :]; hn = [tar[:, 1 + i, :] for i in range(2)]
        gbs = [tar[:, 3 + i, :] for i in range(2)]; s_sb = [tar[:, 5 + i, :] for i in range(2)]; t_sb = [tar[:, 7 + i, :] for i in range(2)]
        t1 = [tar[:, 3 + i, :] for i in range(2)]; t2 = [tar[:, 5 + i, :] for i in range(2)]

        PT = [sb("PT%d" % i, [128, 512], BF16) for i in range(NST)]
        msk = sb("msk", [128, NM, 128], BF16)
        c_sb = sb("c_sb", [128, 8], F32)
        cact = sb("cact", [128, 8], BF16)
        adab = sb("adab", [128, depth, 48], F32)
        ngm_sb = sb("ngm_sb", [128, depth, 8], F32)
        ngf_sb = sb("ngf_sb", [128, depth, 8], F32)
        fngs = sb("fngs", [128, 8], F32)
        cs = sb("cs", [128, 8], F32)
        mod = sb("mod", [128, depth, 48], F32)
        gs1 = sb("gs1", [128, 8], F32)
        gs2 = sb("gs2", [128, 8], F32)
        ones = sb("ones", [128, 128], BF16)
        onesf = sb("onesf", [128, 128], F32)
        epsb = sb("epsb", [128, 1], F32)
        lam_sb = sb("lam_sb", [128, depth, 4, 32], F32)
        lamt = sb("lamt", [128, 2, 32], F32)
        lams = sb("lams", [128, 8], F32)
        sm = sb("sm", [128, depth, 16], F32)
        sinke = sb("sinke", [128, 8], F32)
        wr_sb = sb("wr_sb", [128, depth, 8, 20], F32)
        rb_sb = sb("rb_sb", [128, depth, 20], F32)
        id_sb = sb("id_sb", [128, 128], F32)
        sel_sb = sb("sel_sb", [16, 16, 128], F32)
        L = sb("L", [128, 16, 20], F32)
        gates = sb("gates", [128, 16, 16], F32)
        rt = sb("rt", [128, 64], F32)
        ps = es.enter_context(nc.psum_tensor("ps", [128, 8 * 512], F32))

        def bank(i):
            return ps[:, i * 512:(i + 1) * 512]

        P.dma("sync", x_sb[:], xT, writes=["x"])
        for (d_, s_, n_) in ((c_sb, cT, "c_sb"), (adab, ada_b, "adab"), (ngm_sb, ngm, "ngm"), (ngf_sb, ngf, "ngf"), (fngs, fng, "fngs"), (lam_sb, lamv, "lam_sb"), (sm, small, "sm"),
                             (wr_sb, wr, "wr_sb"), (rb_sb, rbias, "rb_sb"), (id_sb, ident, "id_sb"), (sel_sb, sel, "sel_sb"), (msk, masks, "msk"), (cs, cst, "cs")):
            P.dma("sync", d_[:], s_, writes=[n_])
        P.pool(lambda e: e.memset(ones[:], 1.0), writes=["ones"])
        P.pool(lambda e: e.memset(onesf[:], 1.0), writes=["onesf"])
        P.pool(lambda e: e.memset(epsb[:], 1e-6), writes=["epsb"])
        P.act(lambda e: e.activation(out=cact[:], in_=c_sb[:], func=AF.Silu), reads=["c_sb"], writes=["cact"])
        for tg in range(4):
            tsl = slice(tg * GS, (tg + 1) * GS)
            P.dma("sync", posi, posb[:, tsl], writes=["posi"])
            P.dve(lambda e: e.tensor_copy(posf, posi), reads=["posi"], writes=["posf"])
            for ti, (fcol, scol) in enumerate(((0, 2), (1, 3))):
                P.dve(lambda e, fcol=fcol: e.tensor_scalar(out=ang, in0=posf, scalar1=cs[:, fcol:fcol + 1], scalar2=None, op0=ALU.mult), reads=["posf", "cs"], writes=["ang"])
                for which in range(2):
                    if which == 0:
                        P.dve(lambda e: e.tensor_scalar(out=t1[0], in0=ang, scalar1=0.5 * PI, scalar2=None, op0=ALU.add), reads=["ang"], writes=["t1_0"])
                    else:
                        P.dve(lambda e: e.tensor_copy(t1[0], ang), reads=["ang"], writes=["t1_0"])
                    P.dve(lambda e: e.tensor_scalar(out=t2[0], in0=t1[0], scalar1=1.0 / (2 * PI), scalar2=None, op0=ALU.mult), reads=["t1_0"], writes=["t2_0"])
                    P.dve(lambda e: e.tensor_copy(mi, t2[0]), reads=["t2_0"], writes=["mi"])
                    P.dve(lambda e: e.tensor_copy(t2[0], mi), reads=["mi"], writes=["t2_0"])
                    P.dve(lambda e: e.scalar_tensor_tensor(out=t1[0], in0=t2[0], scalar=-2 * PI, in1=t1[0], op0=ALU.mult, op1=ALU.add), reads=["t2_0", "t1_0"], writes=["t1_0"])
                    P.dve(lambda e: e.tensor_scalar(out=t2[0], in0=t1[0], scalar1=PI, scalar2=2 * PI, op0=ALU.is_gt, op1=ALU.mult), reads=["t1_0"], writes=["t2_0"])
                    P.dve(lambda e: e.tensor_tensor(out=t1[0], in0=t1[0], in1=t2[0], op=ALU.subtract), reads=["t1_0", "t2_0"], writes=["t1_0"])
                    P.dve(lambda e: e.tensor_scalar(out=t1[0], in0=t1[0], scalar1=PI, scalar2=-PI, op0=ALU.min, op1=ALU.max), reads=["t1_0"], writes=["t1_0"])
                    if which == 0:
                        P.act(lambda e, ti=ti: e.activation(out=tab[:, 2 * ti, :], in_=t1[0], func=AF.Sin), reads=["t1_0"], writes=["tab"])
                    else:
                        P.act(lambda e, ti=ti, scol=scol: e.activation(out=tab[:, 2 * ti + 1, :], in_=t1[0], func=AF.Sin, scale=cs[:, scol:scol + 1]), reads=["t1_0", "cs"], writes=["tab"])
            P.dma("sync", tabD[:, :, tsl], tab, reads=["tab"], writes=["tabD"])
        cnt = 0
        for l in range(depth):
            ada_v = ada_w[l].rearrange("(k p) n -> p k n", p=128)
            for which in range(6):
                Wa = WadaP[cnt % 2]
                wn = "Wada%d" % (cnt % 2)
                cnt += 1
                P.dma("gpsimd", Wa, ada_v[:, :, which * 1024:(which + 1) * 1024], writes=[wn])
                for j in range(8):
                    for k in range(8):
                        P.mm(lambda e, j=j, k=k, which=which, Wa=Wa: e.matmul(bank(0)[:, which * 8 + j: which * 8 + j + 1], lhsT=Wa[:, k, j * 128:(j + 1) * 128], rhs=cact[:, k:k + 1], start=(k == 0), stop=(k == 7)),
                             reads=[wn, "cact"], writes=["modps"])
            P.dve(lambda e, l=l: e.tensor_tensor(out=mod[:, l, :], in0=bank(0)[:, 0:48], in1=adab[:, l, :], op=ALU.add), reads=["modps", "adab"], writes=["mod"])
        P.barrier()

        st = dict(i=0, pend=[], mi=0, ubank_started=set())

        def run_batch(units, scale, rowgrp):
            b = st["i"] % NST
            st["i"] += 1
            ptn = "PT%d" % b
            stn = "ST%d" % b
            o = 0
            for u in units:
                u["off"] = o
                P.mm(lambda e, u=u, o=o, b=b: e.matmul(bank(b)[:, o:o + u["n"]], lhsT=u["kt"], rhs=u["q"], start=True, stop=True, tile_position=(rowgrp, 0)),
                     reads=u["kres"] + u["qres"], writes=[stn])
                o += u["n"]
            tot = o
            P.act(lambda e, b=b, tot=tot: e.activation(out=PT[b][:, 0:tot], in_=bank(b)[:, 0:tot], func=AF.Exp, scale=scale), reads=[stn], writes=[ptn])
            merged = False
            if len(units) > 1 and all(u["mask"] is not None and u["mask"][1] == 128 and u["n"] == 128 for u in units):
                kinds = [MKINDS[u["mask"][2]] for u in units]
                for s0 in range(NM - len(units) + 1):
                    if MKINDS[s0:s0 + len(units)] == kinds:
                        eng = "gpsimd" if st["mi"] % 2 == 0 else "vector"
                        st["mi"] += 1
                        nn = len(units)
                        P.op(eng, lambda e, b=b, s0=s0, nn=nn: e.tensor_tensor(out=PT[b][:, 0:nn * 128], in0=PT[b][:, 0:nn * 128], in1=msk[:, s0:s0 + nn, :].rearrange("p a b -> p (a b)"), op=ALU.mult),
                             reads=[ptn, "msk"], writes=[ptn])
                        merged = True
                        break
            if not merged:
                for u in units:
                    if u["mask"] is not None:
                        eng = "gpsimd" if st["mi"] % 2 == 0 else "vector"
                        st["mi"] += 1
                        o = u["off"]
                        m = u["mask"]
                        P.op(eng, lambda e, b=b, o=o, m=m: e.tensor_tensor(out=PT[b][:, o:o + m[1]], in0=PT[b][:, o:o + m[1]], in1=m[0], op=ALU.mult), reads=[ptn, "msk"], writes=[ptn])
            st["pend"].append((units, b))
            while len(st["pend"]) > LOOK:
                emit_pv(*st["pend"].pop(0))

        def emit_pv(units, b):
            ptn = "PT%d" % b
            for u in units:
                first = u["ubank"] not in st["ubank_started"]
                st["ubank_started"].add(u["ubank"])
                o = u["off"]
                P.mm(lambda e, u=u, o=o, b=b, first=first: e.matmul(u["u"], lhsT=u["v"], rhs=PT[b][:, o:o + u["n"]], start=first, stop=False, skip_group_check=True),
                     reads=[ptn] + u["vres"], writes=[u["ures"]])

        def flush():
            while st["pend"]:
                emit_pv(*st["pend"].pop(0))
        ucount = [0]

        def new_ubank():
            ub = NST + (ucount[0] % 3)
            ucount[0] += 1
            st["ubank_started"].discard(ub)
            return ub

        def finalize_simple(ub, head_feat, g, sink_col):
            ures = "U%d" % ub
            if sink_col is not None:
                P.dve(lambda e: e.tensor_scalar(out=rr[64:65, :], in0=bank(ub)[64:65, :], scalar1=sinke[64:65, sink_col:sink_col + 1], scalar2=None, op0=ALU.add), reads=[ures, "sinke"], writes=["rr"])
                P.dve(lambda e: e.reciprocal(rr[64:65, :], rr[64:65, :]), reads=["rr"], writes=["rr"])
            else:
                P.dve(lambda e: e.reciprocal(rr[64:65, :], bank(ub)[64:65, :]), reads=[ures], writes=["rr"])
            P.mm(lambda e: e.matmul(bank(7)[0:64, :], lhsT=onesf[64:65, 0:64], rhs=rr[64:65, :], start=True, stop=True), reads=["rr", "onesf"], writes=["BC"])
            P.act(lambda e: e.activation(out=bcs[0:64, :], in_=bank(7)[0:64, :], func=AF.Copy), reads=["BC"], writes=["bcs"])
            P.dve(lambda e: e.tensor_tensor(out=tmpA[0][0:64, :], in0=bank(ub)[0:64, :], in1=bcs[0:64, :], op=ALU.mult), reads=[ures, "bcs"], writes=["tmpA0"])
            ch, po = head_feat // 128, head_feat % 128
            P.act(lambda e: e.activation(out=mix[po:po + 64, ch, g * GS:(g + 1) * GS], in_=tmpA[0][0:64, :], func=AF.Copy), reads=["tmpA0"], writes=["mix"])

        def load_v1(dst, src_rows_ap, nheads, resname, extra_reads):
            P.dma("sync", dst.rearrange("p (h c) -> p h c", c=65)[:, :, 0:64], src_rows_ap.rearrange("p (h c) -> p h c", c=64), reads=extra_reads, writes=[resname])

        chunks = []
        chunks += [("w", 0, 32, ("q", 0)), ("w", 128, 32, ("q", 1)), ("w", 256, 32, ("k", 0)), ("w", 384, 32, ("k", 1))]
        chunks += [("w", 512 + 128 * i, 64, ("q", 2 + i)) for i in range(4)]
        chunks += [("kb", g, 64, ("k", 2 + g)) for g in range(2)]
        chunks += [("w", 1152 + 128 * i, 64, ("q", 6 + i)) for i in range(2)]
        chunks += [("w", 1408 + 128 * i, 64, ("k", 4 + i)) for i in range(2)]

        for l in range(depth):
            last = (l == depth - 1)
            lam_init_col = sm[:, l, 0:1]
            P.dve(lambda e, l=l: e.scalar_tensor_tensor(out=gs1[:], in0=mod[:, l, 8:16], scalar=1.0, in1=ngm_sb[:, l, :], op0=ALU.add, op1=ALU.mult), reads=["mod", "ngm"], writes=["gs1"])
            P.dve(lambda e, l=l: e.scalar_tensor_tensor(out=gs2[:], in0=mod[:, l, 32:40], scalar=1.0, in1=ngf_sb[:, l, :], op0=ALU.add, op1=ALU.mult), reads=["mod", "ngf"], writes=["gs2"])
            for i in range(2):
                P.dve(lambda e, i=i, l=l: e.tensor_tensor(out=lamt[:, i, :], in0=lam_sb[:, l, 2 * i, :], in1=lam_sb[:, l, 2 * i + 1, :], op=ALU.mult), reads=["lam_sb"], writes=["lamt"])
                P.dve(lambda e, i=i: e.reduce_sum(out=lams[:, i:i + 1], in_=lamt[:, i, :], axis=X), reads=["lamt"], writes=["lams"])
            P.act(lambda e: e.activation(out=lams[:, 2:4], in_=lams[:, 0:2], func=AF.Exp), reads=["lams"], writes=["lams"])
            P.dve(lambda e: e.tensor_tensor(out=lams[:, 4:5], in0=lams[:, 3:4], in1=lams[:, 2:3], op=ALU.subtract), reads=["lams"], writes=["lams"])
            P.dve(lambda e, l=l: e.tensor_tensor(out=lams[:, 4:5], in0=lams[:, 4:5], in1=sm[:, l, 0:1], op=ALU.subtract), reads=["lams", "sm"], writes=["lams"])
            P.dve(lambda e, l=l: e.tensor_scalar(out=lams[:, 5:6], in0=sm[:, l, 0:1], scalar1=-1.0, scalar2=1.0, op0=ALU.mult, op1=ALU.add), reads=["sm"], writes=["lams"])
            P.dve(lambda e, l=l: e.tensor_tensor(out=lams[:, 6:7], in0=lams[:, 5:6], in1=sm[:, l, 1:2], op=ALU.mult), reads=["lams", "sm"], writes=["lams"])
            P.act(lambda e, l=l: e.activation(out=sinke[:], in_=sm[:, l, 8:16], func=AF.Exp), reads=["sm"], writes=["sinke"])

            w_in_v = w_in[l].rearrange("(k p) n -> p k n", p=128)
            for k in range(8):
                for (d0, s0, n) in ((0, 0, 512), (512, 768, 640), (1152, 1536, 512)):
                    P.dma("gpsimd", Wqk[:, k, d0:d0 + n], w_in_v[:, k, s0:s0 + n], writes=["W%d" % k])
                for (d0, s0, n) in ((0, 512, 256), (256, 1408, 128), (384, 2048, 256)):
                    P.dma("gpsimd", Wv[:, k, d0:d0 + n], w_in_v[:, k, s0:s0 + n], writes=["Wv%d" % k])

            def swapcopy(k, c0, nheads, dh):
                h = dh // 2
                d = Wsqk[:, k, c0:c0 + nheads * dh].rearrange("p (a t c) -> p a t c", t=2, c=h)
                s = Wqk[:, k, c0:c0 + nheads * dh].rearrange("p (a t c) -> p a t c", t=2, c=h)
                P.pool(lambda e: e.tensor_copy(d[:, :, 0, :], s[:, :, 1, :]), reads=["W%d" % k], writes=["Ws%d" % k])
                P.pool(lambda e: e.tensor_copy(d[:, :, 1, :], s[:, :, 0, :]), reads=["W%d" % k], writes=["Ws%d" % k])
            for k in range(8):
                swapcopy(k, 0, 16, 32)
                swapcopy(k, 512, 10, 64)
                swapcopy(k, 1152, 8, 64)
                for g in range(2):
                    for rep in range(2):
                        P.pool(lambda e, k=k, g=g, rep=rep: e.tensor_copy(Wkb[:, k, g, rep * 64:(rep + 1) * 64], Wqk[:, k, 1024 + g * 64:1024 + (g + 1) * 64]), reads=["W%d" % k], writes=["Wkb%d" % k])
                        P.pool(lambda e, k=k, g=g, rep=rep: e.tensor_copy(Wskb[:, k, g, rep * 64:(rep + 1) * 64], Wsqk[:, k, 1024 + g * 64:1024 + (g + 1) * 64]), reads=["Ws%d" % k], writes=["Wskb%d" % k])
            qi = 0
            for tg in range(4):
                tsl = slice(tg * GS, (tg + 1) * GS)
                P.dma("sync", tab, tabD[:, :, tsl], reads=["tabD"], writes=["tab"])
                P.act(lambda e, tg=tg: e.activation(out=sqP[:], in_=x_sb[:, :, tg * GS:(tg + 1) * GS], func=AF.Square), reads=["x"], writes=["sqP"])
                for k in range(8):
                    P.mm(lambda e, k=k: e.matmul(bank(7), lhsT=ones[:], rhs=sqP[:, k, :], start=(k == 0), stop=(k == 7)), reads=["sqP", "ones"], writes=["ssps"])
                P.act(lambda e: e.activation(out=rstd, in_=bank(7), func=AF.Sqrt, scale=1.0 / 1024, bias=epsb[:]), reads=["ssps", "epsb"], writes=["rstd"])
                P.dve(lambda e: e.reciprocal(rstd, rstd), reads=["rstd"], writes=["rstd"])
                for k in range(8):
                    hb = hn[k % 2]
                    hbn = "hn%d" % (k % 2)
                    P.dve(lambda e, k=k, hb=hb, tg=tg: e.tensor_tensor(out=hb, in0=x_sb[:, k, tg * GS:(tg + 1) * GS], in1=rstd, op=ALU.mult), reads=["x", "rstd"], writes=[hbn])
                    P.act(lambda e, k=k, hb=hb, l=l: e.activation(out=hTP[:, k, :], in_=hb, func=AF.Identity, scale=gs1[:, k:k + 1], bias=mod[:, l, k:k + 1]), reads=[hbn, "gs1", "mod"], writes=["hTP%d" % k])
                for ci, (kind, src, tt, dest) in enumerate(chunks):
                    b1 = 1 + 2 * (ci % 2)
                    b2 = b1 + 1
                    ti = 0 if tt == 64 else 1
                    for k in range(8):
                        if kind == "w":
                            l1 = Wqk[:, k, src:src + 128]; r1 = "W%d" % k
                        else:
                            l1 = Wkb[:, k, src, :]; r1 = "Wkb%d" % k
                        P.mm(lambda e, k=k, l1=l1, b1=b1: e.matmul(bank(b1), lhsT=l1, rhs=hTP[:, k, :], start=(k == 0), stop=(k == 7)), reads=[r1, "hTP%d" % k], writes=["bank%d" % b1])
                    for k in range(8):
                        if kind == "w":
                            l2 = Wsqk[:, k, src:src + 128]; r2 = "Ws%d" % k
                        else:
                            l2 = Wskb[:, k, src, :]; r2 = "Wskb%d" % k
                        P.mm(lambda e, k=k, l2=l2, b2=b2: e.matmul(bank(b2), lhsT=l2, rhs=hTP[:, k, :], start=(k == 0), stop=(k == 7)), reads=[r2, "hTP%d" % k], writes=["bank%d" % b2])
                    a = t1[ci % 2]; an = "t1_%d" % (ci % 2)
                    bb = t2[ci % 2]; bn = "t2_%d" % (ci % 2)
                    P.dve(lambda e, a=a, b1=b1, ti=ti: e.tensor_tensor(out=a, in0=bank(b1), in1=tab[:, 2 * ti, :], op=ALU.mult), reads=["bank%d" % b1, "tab"], writes=[an])
                    P.dve(lambda e, bb=bb, b2=b2, ti=ti: e.tensor_tensor(out=bb, in0=bank(b2), in1=tab[:, 2 * ti + 1, :], op=ALU.mult), reads=["bank%d" % b2, "tab"], writes=[bn])
                    so = qkst[qi % 2]; son = "qkst%d" % (qi % 2)
                    qi += 1
                    P.pool(lambda e, a=a, bb=bb, so=so: e.tensor_tensor(out=so, in0=a, in1=bb, op=ALU.add), reads=[an, bn], writes=[son])
                    if dest[0] == "q":
                        P.dma("sync", QTd[:, dest[1], tsl], so, reads=[son], writes=["QTd"])
                    else:
                        P.dma("sync", ktd(dest[1])[:, tsl], so, reads=[son], writes=["KTd"])
                for tt4 in range(4):
                    vb = vst[tt4 % 2]; vn = "vst%d" % (tt4 % 2)
                    for k in range(8):
                        P.mm(lambda e, k=k, tt4=tt4: e.matmul(bank(5), lhsT=hTP[:, k, tt4 * 128:(tt4 + 1) * 128], rhs=Wv[:, k, 0:512], start=(k == 0), stop=(k == 7)), reads=["Wv%d" % k, "hTP%d" % k], writes=["bank5"])
                    for k in range(8):
                        P.mm(lambda e, k=k, tt4=tt4: e.matmul(bank(6)[:, 0:128], lhsT=hTP[:, k, tt4 * 128:(tt4 + 1) * 128], rhs=Wv[:, k, 512:640], start=(k == 0), stop=(k == 7)), reads=["Wv%d" % k, "hTP%d" % k], writes=["bank6"])
                    P.act(lambda e, vb=vb: e.activation(out=vb[:, 0:512], in_=bank(5), func=AF.Copy), reads=["bank5"], writes=[vn])
                    P.act(lambda e, vb=vb: e.activation(out=vb[:, 512:640], in_=bank(6)[:, 0:128], func=AF.Copy), reads=["bank6"], writes=[vn])
                    r0 = tg * GS + tt4 * 128
                    P.dma("sync", vd_rows(r0, 128), vb, reads=[vn], writes=["Vd"])
            P.barrier()
            for i in range(2):
                P.cc(lambda e, i=i: e.collective_compute("AllGather", ALU.bypass, replica_groups=groups, ins=[KTd_h[i].ap().opt()], outs=[KTg_h[i].ap().opt()]), reads=["KTd"], writes=["KTg"])
            for i in range(2):
                P.cc(lambda e, i=i: e.collective_compute("AllGather", ALU.bypass, replica_groups=groups, ins=[Vd_h[i].ap().opt()], outs=[Vg_h[i].ap().opt()]), reads=["Vd"], writes=["Vg"])

            P.dma("sync", q_sb[:, 0:2, :], QTd[:, 0:2, :], reads=["QTd"], writes=["q_sb"])
            for a in range(2):
                for ci in range(2):
                    P.dma("sync", k_sb[:, a, ci, :], ktg(a, 0 + ci), reads=["KTg"], writes=["k_sb"])
            P.pool(lambda e: e.memset(v_sb[:], 1.0), writes=["v_sb"])
            for a in range(2):
                for t in range(16):
                    load_v1(v_sb[:, a, t, :], vg_tile(a, t)[:, 0:256], 4, "v_sb", ["Vg"])
            scaleA = 32 ** -0.5
            for h in range(4):
                for g in range(4):
                    ubs = []
                    for tau in range(2):
                        ub = new_ubank()
                        ubs.append(ub)
                        for jp in range(4 * g + 4):
                            for a in range(2):
                                if jp < 4 * g:
                                    q0, n, m = g * GS, GS, None
                                else:
                                    q0 = jp * 128
                                    n = (g + 1) * GS - q0
                                    m = (msk[:, a, :], 128, a)
                                u = dict(kt=k_sb[32 * h:32 * h + 32, a, tau, jp * 128:(jp + 1) * 128], q=q_sb[32 * h:32 * h + 32, tau, q0:q0 + n], n=n, mask=m,
                                         v=vwide(V_OFF, a * 16 + jp, 65 * h), u=bank(ub)[:, q0 - g * GS:q0 - g * GS + n], ures="U%d" % ub, ubank=ub,
                                         kres=["k_sb"], qres=["q_sb"], vres=["v_sb"])
                                run_batch([u], scaleA, 32 * h)
                    flush()
                    for tau in range(2):
                        ub = ubs[tau]
                        ures = "U%d" % ub
                        P.dve(lambda e, ub=ub: e.reciprocal(rr[64:65, :], bank(ub)[64:65, :]), reads=[ures], writes=["rr"])
                        P.mm(lambda e: e.matmul(bank(7)[0:64, :], lhsT=onesf[64:65, 0:64], rhs=rr[64:65, :], start=True, stop=True), reads=["rr", "onesf"], writes=["BC"])
                        P.act(lambda e: e.activation(out=bcs[0:64, :], in_=bank(7)[0:64, :], func=AF.Copy), reads=["BC"], writes=["bcs"])
                        P.dve(lambda e, ub=ub, tau=tau: e.tensor_tensor(out=tmpA[tau][0:64, :], in0=bank(ub)[0:64, :], in1=bcs[0:64, :], op=ALU.mult), reads=[ures, "bcs"], writes=["tmpA%d" % tau])
                    P.dve(lambda e: e.scalar_tensor_tensor(out=tmpA[0][0:64, :], in0=tmpA[1][0:64, :], scalar=lams[0:64, 4:5], in1=tmpA[0][0:64, :], op0=ALU.mult, op1=ALU.add), reads=["tmpA0", "tmpA1", "lams"], writes=["tmpA0"])
                    P.act(lambda e: e.activation(out=tmpA[2][0:64, :], in_=tmpA[0][0:64, :], func=AF.Square), reads=["tmpA0"], writes=["tmpA2"])
                    P.mm(lambda e: e.matmul(bank(7)[0:64, :], lhsT=onesf[0:64, 0:64], rhs=tmpA[2][0:64, :], start=True, stop=True), reads=["tmpA2", "onesf"], writes=["BC"])
                    P.act(lambda e: e.activation(out=bcs[0:64, :], in_=bank(7)[0:64, :], func=AF.Sqrt, scale=1.0 / 64, bias=epsb[0:64, :]), reads=["BC", "epsb"], writes=["bcs"])
                    P.dve(lambda e: e.reciprocal(bcs[0:64, :], bcs[0:64, :]), reads=["bcs"], writes=["bcs"])
                    P.dve(lambda e: e.tensor_tensor(out=tmpA[0][0:64, :], in0=tmpA[0][0:64, :], in1=bcs[0:64, :], op=ALU.mult), reads=["tmpA0", "bcs"], writes=["tmpA0"])
                    ch, po = (64 * h) // 128, (64 * h) % 128
                    P.act(lambda e, ch=ch, po=po, g=g: e.activation(out=mix[po:po + 64, ch, g * GS:(g + 1) * GS], in_=tmpA[0][0:64, :], func=AF.Copy, scale=lams[0:64, 6:7]), reads=["tmpA0", "lams"], writes=["mix"])

            P.dma("sync", q_sb[:, 0:4, :], QTd[:, 2:6, :], reads=["QTd"], writes=["q_sb"])
            for a in range(2):
                for ci in range(2):
                    P.dma("sync", k_sb[:, a, ci, :], ktg(a, 2 + ci), reads=["KTg"], writes=["k_sb"])
            for a in range(2):
                for t in range(16):
                    load_v1(v_sb[:, a, t, 0:130], vg_tile(a, t)[:, 256:384], 2, "v_sb", ["Vg"])
            scaleB = 64 ** -0.5
            for h in range(8):
                kv = h // 4
                rg = 64 * (h % 2)
                qc = h // 2
                for g in range(4):
                    ub = new_ubank()
                    for j in range(4 * g, 4 * g + 4):
                        units = []
                        for (a, jj, mid) in ((0, j - 1, 2), (0, j, 3), (1, j - 1, 4), (1, j, 5)):
                            if jj < 0:
                                continue
                            units.append(dict(kt=k_sb[rg:rg + 64, a, kv, jj * 128:(jj + 1) * 128], q=q_sb[rg:rg + 64, qc, j * 128:(j + 1) * 128], n=128, mask=(msk[:, mid, :], 128, mid),
                                              v=vwide(V_OFF, a * 16 + jj, 65 * kv), u=bank(ub)[:, (j - 4 * g) * 128:(j - 4 * g + 1) * 128], ures="U%d" % ub, ubank=ub,
                                              kres=["k_sb"], qres=["q_sb"], vres=["v_sb"]))
                        run_batch(units, scaleB, rg)
                    flush()
                    finalize_simple(ub, 256 + 64 * h, g, h)

            P.dma("sync", q_sb[:, 0:2, :], QTd[:, 6:8, :], reads=["QTd"], writes=["q_sb"])
            for ci in range(2):
                P.dma("sync", q_sb[:, 2 + ci, :], ktd(4 + ci), reads=["KTd"], writes=["q_sb"])
            for a in range(2):
                for ci in range(2):
                    P.dma("sync", k_sb[:, a, ci, :], ktg(a, 4 + ci), reads=["KTg"], writes=["k_sb"])
            for a in range(2):
                for t in range(16):
                    load_v1(v_sb[:, a, t, :], vg_tile(a, t)[:, 384:640], 4, "v_sb", ["Vg"])
            P.pool(lambda e: e.memset(v2_sb[:], 1.0), writes=["v2_sb"])
            P.pool(lambda e: e.memset(v8_sb[:], 1.0), writes=["v8_sb"])
            for cp in range(2):
                for bt in range(8):
                    n0 = cp + 2 * 128 * bt
                    load_v1(v2_sb[:, cp * 8 + bt, :], vd_rows(n0, 128, 2)[:, 384:640], 4, "v2_sb", ["Vd"])
            for cp in range(8):
                for bt in range(2):
                    n0 = cp + 8 * 128 * bt
                    load_v1(v8_sb[:, cp * 2 + bt, :], vd_rows(n0, 128, 8)[:, 384:640], 4, "v8_sb", ["Vd"])
            for h in range(4):
                rg = 64 * (h % 2)
                qc = h // 2
                for g in range(4):
                    ub = new_ubank()
                    for j in range(4 * g, 4 * g + 4):
                        units = []
                        for (a, jj, mid) in ((0, j - 1, 6), (0, j, 7), (1, j - 1, 8), (1, j, 9)):
                            if jj < 0:
                                continue
                            units.append(dict(kt=k_sb[rg:rg + 64, a, qc, jj * 128:(jj + 1) * 128], q=q_sb[rg:rg + 64, qc, j * 128:(j + 1) * 128], n=128, mask=(msk[:, mid, :], 128, mid),
                                              v=vwide(V_OFF, a * 16 + jj, 65 * h), u=bank(ub)[:, (j - 4 * g) * 128:(j - 4 * g + 1) * 128], ures="U%d" % ub, ubank=ub,
                                              kres=["k_sb"], qres=["q_sb"], vres=["v_sb"]))
                        run_batch(units, scaleB, rg)
                    units = []
                    for cp in range(2):
                        for bt in (2 * g, 2 * g + 1):
                            qn0 = cp + 256 * bt
                            for (bb_, mid) in ((bt - 1, 10), (bt, 11)):
                                if bb_ < 0:
                                    continue
                                kn0 = cp + 256 * bb_
                                units.append(dict(kt=q_sb[rg:rg + 64, 2 + qc, kn0:kn0 + 255:2], q=q_sb[rg:rg + 64, qc, qn0:qn0 + 255:2], n=128, mask=(msk[:, mid, :], 128, mid),
                                                  v=vwide(V2_OFF, cp * 8 + bb_, 65 * h), u=bank(ub)[:, qn0 - g * GS:qn0 - g * GS + 255:2], ures="U%d" % ub, ubank=ub,
                                                  kres=["q_sb"], qres=["q_sb"], vres=["v2_sb"]))
                    for i in range(0, len(units), 4):
                        run_batch(units[i:i + 4], scaleB, rg)
                    units = []
                    bt = g // 2
                    half = g % 2
                    for cp in range(8):
                        qn0 = cp + 1024 * bt + 8 * 64 * half
                        for (bb_, mid) in ((bt - 1, 10), (bt, 11)):
                            if bb_ < 0:
                                continue
                            kn0 = cp + 1024 * bb_
                            units.append(dict(kt=q_sb[rg:rg + 64, 2 + qc, kn0:kn0 + 1017:8], q=q_sb[rg:rg + 64, qc, qn0:qn0 + 505:8], n=64, mask=(msk[:, mid, 64 * half:64 * half + 64], 64, mid),
                                              v=vwide(V8_OFF, cp * 2 + bb_, 65 * h), u=bank(ub)[:, qn0 - g * GS:qn0 - g * GS + 505:8], ures="U%d" % ub, ubank=ub,
                                              kres=["q_sb"], qres=["q_sb"], vres=["v8_sb"]))
                    for i in range(0, len(units), 8):
                        run_batch(units[i:i + 8], scaleB, rg)
                    flush()
                    finalize_simple(ub, 768 + 64 * h, g, None)
            P.barrier()
            wo_v = w_out[l].rearrange("(k p) n -> p k n", p=128)
            for k in range(8):
                P.dma("gpsimd", Wo[:, k, :], wo_v[:, k, :], writes=["Wo%d" % k])
            cnt = 0
            for g in range(4):
                for oc in range(8):
                    b = cnt % 2
                    cnt += 1
                    for k in range(8):
                        P.mm(lambda e, k=k, oc=oc, g=g, b=b: e.matmul(bank(b), lhsT=Wo[:, k, oc * 128:(oc + 1) * 128], rhs=mix[:, k, g * GS:(g + 1) * GS], start=(k == 0), stop=(k == 7)),
                             reads=["Wo%d" % k, "mix"], writes=["ob%d" % b])
                    P.dve(lambda e, oc=oc, g=g, b=b, l=l: e.scalar_tensor_tensor(out=x_sb[:, oc, g * GS:(g + 1) * GS], in0=bank(b), scalar=mod[:, l, 16 + oc:17 + oc], in1=x_sb[:, oc, g * GS:(g + 1) * GS], op0=ALU.mult, op1=ALU.add),
                          reads=["ob%d" % b, "mod", "x"], writes=["x"])
            P.barrier()
            def load_expert(e_, l=l):
                wg_, wu_, wd_ = wbuf[e_ % 2]
                nm = "w%d" % (e_ % 2)
                extra = (["h32_%d" % k for k in range(8)] + ["sq"]) if e_ == 1 else []
                P.dma("gpsimd", wg_, wgate[l, e_].rearrange("(k p) f -> p k f", p=128), writes=[nm + "g"] + extra)
                P.dma("gpsimd", wu_, wup[l, e_].rearrange("(k p) f -> p k f", p=128), writes=[nm + "u"] + extra)
                P.dma("gpsimd", wd_, wdown[l, e_].rearrange("(c p) d -> p c d", p=128), writes=[nm + "d"] + extra)
            load_expert(0)
            for g in range(4):
                P.act(lambda e, g=g: e.activation(out=sq[:], in_=x_sb[:, :, g * GS:(g + 1) * GS], func=AF.Square), reads=["x"], writes=["sq"])
                for k in range(8):
                    P.mm(lambda e, k=k: e.matmul(bank(7), lhsT=ones[:], rhs=sq[:, k, :], start=(k == 0), stop=(k == 7)), reads=["sq", "ones"], writes=["ssps"])
                P.act(lambda e: e.activation(out=rstd, in_=bank(7), func=AF.Sqrt, scale=1.0 / 1024, bias=epsb[:]), reads=["ssps", "epsb"], writes=["rstd"])
                P.dve(lambda e: e.reciprocal(rstd, rstd), reads=["rstd"], writes=["rstd"])
                for k in range(8):
                    hb = hn[k % 2]
                    hbn = "hn%d" % (k % 2)
                    P.dve(lambda e, k=k, hb=hb, g=g: e.tensor_tensor(out=hb, in0=x_sb[:, k, g * GS:(g + 1) * GS], in1=rstd, op=ALU.mult), reads=["x", "rstd"], writes=[hbn])
                    P.act(lambda e, k=k, hb=hb, l=l: e.activation(out=h32[:, k, :], in_=hb, func=AF.Identity, scale=gs2[:, k:k + 1], bias=mod[:, l, 24 + k:25 + k]), reads=[hbn, "gs2", "mod"], writes=["h32_%d" % k])
                    P.pool(lambda e, k=k, g=g: e.tensor_copy(hT[:, k, g * GS:(g + 1) * GS], h32[:, k, :]), reads=["h32_%d" % k], writes=["hT"])
                for tt in range(4):
                    ti = g * 4 + tt
                    for k in range(8):
                        P.mm(lambda e, k=k, tt=tt, ti=ti, l=l: e.matmul(bank(6)[:, ti * 20:(ti + 1) * 20], lhsT=h32[:, k, tt * 128:(tt + 1) * 128], rhs=wr_sb[:, l, k, :], start=(k == 0), stop=(k == 7)),
                             reads=["h32_%d" % k, "wr_sb"], writes=["Rps"])
            for ti in range(16):
                def R(i, n=1):
                    return rt[:, i:i + n]
                P.dve(lambda e, ti=ti, l=l: e.tensor_tensor(out=L[:, ti, :], in0=bank(6)[:, ti * 20:(ti + 1) * 20], in1=rb_sb[:, l, :], op=ALU.add), reads=["Rps", "rb_sb"], writes=["L"])
                gl = L[:, ti, 0:4]
                el = L[:, ti, 4:20]
                P.dve(lambda e, gl=gl: e.reduce_max(out=R(0), in_=gl, axis=X), reads=["L"], writes=["r0"])
                P.dve(lambda e: e.tensor_scalar(out=R(1), in0=R(0), scalar1=-1.0, scalar2=None, op0=ALU.mult), reads=["r0"], writes=["r1"])
                P.act(lambda e, gl=gl: e.activation(out=R(2, 4), in_=gl, func=AF.Exp, bias=R(1)), reads=["L", "r1"], writes=["r2"])
                P.dve(lambda e: e.reduce_sum(out=R(6), in_=R(2, 4), axis=X), reads=["r2"], writes=["r6"])
                P.dve(lambda e: e.reciprocal(R(6), R(6)), reads=["r6"], writes=["r6"])
                P.dve(lambda e, gl=gl: e.tensor_scalar(out=R(7, 4), in0=gl, scalar1=R(0), scalar2=None, op0=ALU.is_equal), reads=["L", "r0"], writes=["r7"])
                P.dve(lambda e: e.tensor_scalar(out=R(7, 4), in0=R(7, 4), scalar1=1.0, scalar2=BIG, op0=ALU.subtract, op1=ALU.mult), reads=["r7"], writes=["r7"])
                for gi in range(4):
                    P.dve(lambda e, gi=gi, el=el: e.tensor_scalar(out=R(12 + 4 * gi, 4), in0=el[:, 4 * gi:4 * gi + 4], scalar1=R(7 + gi), scalar2=None, op0=ALU.add), reads=["L", "r7"], writes=["elm"])
                elm = R(12, 16)
                P.dve(lambda e: e.reduce_max(out=R(28), in_=elm, axis=X), reads=["elm"], writes=["m1"])
                P.dve(lambda e: e.tensor_scalar(out=R(30, 16), in0=elm, scalar1=R(28), scalar2=None, op0=ALU.is_equal), reads=["elm", "m1"], writes=["oh1"])
                P.dve(lambda e: e.scalar_tensor_tensor(out=R(46, 16), in0=R(30, 16), scalar=-BIG, in1=elm, op0=ALU.mult, op1=ALU.add), reads=["oh1", "elm"], writes=["elm2"])
                P.dve(lambda e: e.reduce_max(out=R(29), in_=R(46, 16), axis=X), reads=["elm2"], writes=["m2"])
                P.dve(lambda e: e.tensor_scalar(out=R(46, 16), in0=R(46, 16), scalar1=R(29), scalar2=None, op0=ALU.is_equal), reads=["elm2", "m2"], writes=["elm2"])
                P.dve(lambda e: e.tensor_tensor(out=R(62), in0=R(29), in1=R(28), op=ALU.subtract), reads=["m1", "m2"], writes=["dl"])
                P.act(lambda e: e.activation(out=R(62), in_=R(62), func=AF.Exp), reads=["dl"], writes=["dl"])
                P.dve(lambda e: e.tensor_scalar(out=R(63), in0=R(62), scalar1=1.0, scalar2=None, op0=ALU.add), reads=["dl"], writes=["den"])
                P.dve(lambda e: e.reciprocal(R(63), R(63)), reads=["den"], writes=["den"])
                P.dve(lambda e: e.tensor_tensor(out=R(62), in0=R(62), in1=R(63), op=ALU.mult), reads=["dl", "den"], writes=["dl"])
                P.dve(lambda e: e.tensor_tensor(out=R(63), in0=R(63), in1=R(6), op=ALU.mult), reads=["den", "r6"], writes=["den"])
                P.dve(lambda e: e.tensor_tensor(out=R(62), in0=R(62), in1=R(6), op=ALU.mult), reads=["dl", "r6"], writes=["dl"])
                P.dve(lambda e, ti=ti: e.tensor_scalar(out=gates[:, ti, :], in0=R(30, 16), scalar1=R(63), scalar2=None, op0=ALU.mult), reads=["oh1", "den"], writes=["gates"])
                P.dve(lambda e, ti=ti: e.scalar_tensor_tensor(out=gates[:, ti, :], in0=R(46, 16), scalar=R(62), in1=gates[:, ti, :], op0=ALU.mult, op1=ALU.add), reads=["elm2", "dl", "gates"], writes=["gates"])
                P.mm(lambda e, ti=ti: e.transpose(out=bank(5)[0:16, (ti % 4) * 128:(ti % 4 + 1) * 128], in_=gates[:, ti, :], identity=id_sb[:]), reads=["gates", "id_sb"], writes=["gTps%d" % (ti % 4)])
                P.act(lambda e, ti=ti: e.activation(out=gT[0:16, ti * 128:(ti + 1) * 128], in_=bank(5)[0:16, (ti % 4) * 128:(ti % 4 + 1) * 128], func=AF.Copy), reads=["gTps%d" % (ti % 4)], writes=["gT"])
            stage2 = [None]

            def emit_stage2(e_, g, ab, l=l):
                wg_, wu_, wd_ = wbuf[e_ % 2]
                nm = "w%d" % (e_ % 2)
                for oc in range(8):
                    yb = 4 + (oc % 2)
                    for fc in range(4):
                        P.mm(lambda e, fc=fc, oc=oc, yb=yb, wd_=wd_, ab=ab: e.matmul(bank(yb), lhsT=wd_[:, fc, oc * 128:(oc + 1) * 128], rhs=actb[ab][:, fc, :], start=(fc == 0), stop=(fc == 3)),
                             reads=[nm + "d", "act%d" % ab], writes=["y%d" % yb])
                    P.dve(lambda e, oc=oc, g=g, yb=yb: e.scalar_tensor_tensor(out=x_sb[:, oc, g * GS:(g + 1) * GS], in0=bank(yb), scalar=mod[:, l, 40 + oc:41 + oc], in1=x_sb[:, oc, g * GS:(g + 1) * GS], op0=ALU.mult, op1=ALU.add),
                          reads=["y%d" % yb, "mod", "x"], writes=["x"])
            it = 0
            for e_ in range(16):
                wg_, wu_, wd_ = wbuf[e_ % 2]
                nm = "w%d" % (e_ % 2)
                for g in range(4):
                    ab = it % 2
                    it += 1
                    P.mm(lambda e, e_=e_, g=g: e.matmul(bank(6), lhsT=sel_sb[0:16, e_, :], rhs=gT[0:16, g * GS:(g + 1) * GS], start=True, stop=True), reads=["gT", "sel_sb"], writes=["G"])
                    P.act(lambda e, ab=ab: e.activation(out=gbs[ab], in_=bank(6), func=AF.Copy), reads=["G"], writes=["gbs%d" % ab])
                    for fc in range(4):
                        hb_ = 2 * (fc % 2)
                        for k in range(8):
                            P.mm(lambda e, k=k, fc=fc, g=g, hb_=hb_, wg_=wg_: e.matmul(bank(hb_), lhsT=wg_[:, k, fc * 128:(fc + 1) * 128], rhs=hT[:, k, g * GS:(g + 1) * GS], start=(k == 0), stop=(k == 7)),
                                 reads=[nm + "g", "hT"], writes=["hg%d" % hb_])
                        for k in range(8):
                            P.mm(lambda e, k=k, fc=fc, g=g, hb_=hb_, wu_=wu_: e.matmul(bank(hb_ + 1), lhsT=wu_[:, k, fc * 128:(fc + 1) * 128], rhs=hT[:, k, g * GS:(g + 1) * GS], start=(k == 0), stop=(k == 7)),
                                 reads=[nm + "u", "hT"], writes=["hu%d" % hb_])
                        sbi = fc % 2
                        P.act(lambda e, hb_=hb_, sbi=sbi: e.activation(out=s_sb[sbi], in_=bank(hb_), func=AF.Silu), reads=["hg%d" % hb_], writes=["s%d" % sbi])
                        P.dve(lambda e, hb_=hb_, sbi=sbi: e.tensor_tensor(out=t_sb[sbi], in0=bank(hb_ + 1), in1=s_sb[sbi], op=ALU.mult), reads=["hu%d" % hb_, "s%d" % sbi], writes=["t%d" % sbi])
                        P.pool(lambda e, fc=fc, sbi=sbi, ab=ab: e.tensor_tensor(out=actb[ab][:, fc, :], in0=t_sb[sbi], in1=gbs[ab], op=ALU.mult), reads=["t%d" % sbi, "gbs%d" % ab], writes=["act%d" % ab])
                    if stage2[0] is not None:
                        emit_stage2(*stage2[0])
                    stage2[0] = (e_, g, ab)
                    if g == 0 and e_ + 1 < 16:
                        load_expert(e_ + 1)
            emit_stage2(*stage2[0])
            P.barrier()
        out_ops = []
        for g in range(4):
            P.act(lambda e, g=g: e.activation(out=sq[:], in_=x_sb[:, :, g * GS:(g + 1) * GS], func=AF.Square), reads=["x"], writes=["sq"])
            for k in range(8):
                P.mm(lambda e, k=k: e.matmul(bank(7), lhsT=ones[:], rhs=sq[:, k, :], start=(k == 0), stop=(k == 7)), reads=["sq", "ones"], writes=["ssps"])
            P.act(lambda e: e.activation(out=rstd, in_=bank(7), func=AF.Sqrt, scale=1.0 / 1024, bias=epsb[:]), reads=["ssps", "epsb"], writes=["rstd"])
            P.dve(lambda e: e.reciprocal(rstd, rstd), reads=["rstd"], writes=["rstd"])
            for k in range(8):
                hb = hn[k % 2]
                hbn = "hn%d" % (k % 2)
                P.dve(lambda e, k=k, hb=hb, g=g: e.tensor_tensor(out=hb, in0=x_sb[:, k, g * GS:(g + 1) * GS], in1=rstd, op=ALU.mult), reads=["x", "rstd"], writes=[hbn])
                P.act(lambda e, k=k, hb=hb, g=g: e.activation(out=x_sb[:, k, g * GS:(g + 1) * GS], in_=hb, func=AF.Copy, scale=fngs[:, k:k + 1]), reads=[hbn, "fngs"], writes=["xf"])
        out_ops.append(P.dma("sync", xo, x_sb[:], reads=["xf"]))
        P.emit(final_wait_ops=out_ops)
    return nc


from concourse.bass_utils import run_bass_kernel_spmd
import ml_dtypes

_BF = ml_dtypes.bfloat16
_CACHE = {}


def _fm(a):
    Tn = a.shape[0]
    return np.ascontiguousarray(a.T.reshape(8, 128, Tn).transpose(1, 0, 2))


def _pk(v):
    return np.ascontiguousarray(v.reshape(-1, 128).T)


def _pkl(v):
    return np.ascontiguousarray(np.stack([_pk(v[l]) for l in range(v.shape[0])], axis=1))


def _bc(v):
    return np.ascontiguousarray(np.broadcast_to(v, (128,) + v.shape)).astype(np.float32)


def _mkcst():
    p = np.arange(128)
    c = np.zeros((128, 8), np.float32)
    c[:, 0] = (10000.0 ** (-(2 * (p % 32)).astype(np.float32) / 64)).astype(np.float32)
    c[:, 1] = (10000.0 ** (-(2 * (p % 16)).astype(np.float32) / 32)).astype(np.float32)
    c[:, 2] = np.where((p % 64) < 32, -1.0, 1.0)
    c[:, 3] = np.where((p % 32) < 16, -1.0, 1.0)
    return c


def _mkmasks(r):
    k = np.arange(128)[:, None]
    q = np.arange(128)[None, :]
    own = {}
    oth = {}
    own['A'] = (k <= q)
    oth['A'] = (k < q) if r == 0 else (k <= q)
    own['Bp'] = (k >= q + 65)
    own['Bd'] = ((q - k) >= 0) & ((q - k) <= 63)
    oth['Bp'] = (k >= q + 64) if r == 0 else (k >= q + 65)
    oth['Bd'] = (((q - k) >= 1) & ((q - k) <= 64)) if r == 0 else (((q - k) >= 0) & ((q - k) <= 63))
    own['Cp'] = (k >= q + 64)
    own['Cd'] = ((q - k) >= 0) & ((q - k) <= 64)
    oth['Cp'] = (k >= q + 64) if r == 0 else (k >= q + 65)
    oth['Cd'] = (((q - k) >= 1) & ((q - k) <= 64)) if r == 0 else (((q - k) >= 0) & ((q - k) <= 63))

    def pm(a, key):
        return own[key] if a == r else oth[key]
    m = np.zeros((14, 128, 128), np.float32)
    m[0] = pm(0, 'A'); m[1] = pm(1, 'A')
    m[2] = pm(0, 'Bp'); m[3] = pm(0, 'Bd'); m[4] = pm(1, 'Bp'); m[5] = pm(1, 'Bd')
    m[6] = pm(0, 'Cp'); m[7] = pm(0, 'Cd'); m[8] = pm(1, 'Cp'); m[9] = pm(1, 'Cd')
    m[10] = (k >= q)
    m[11] = (k <= q)
    m[12] = m[10]
    m[13] = m[11]
    return np.ascontiguousarray(m.transpose(1, 0, 2)).astype(_BF)


def kernel(x, c, positions, ada_w, ada_b, norm_mix_g, norm_ffn_g, w_in, w_out,
           diff_lambda_q1, diff_lambda_k1, diff_lambda_q2, diff_lambda_k2, diff_subln_g,
           swa_sinks, router_group_w, router_group_b, router_expert_w, router_expert_b,
           expert_w_gate, expert_w_up, expert_w_down, final_norm_g):
    f32 = np.float32
    D = DEPTH
    A = lambda v: np.ascontiguousarray(np.asarray(v, f32))
    x = A(x)
    c = A(c)
    positions = np.asarray(positions)
    if "nc" not in _CACHE:
        _CACHE["nc"] = build_fused(D)
    sel = np.zeros((16, 16, 128), f32)
    for e in range(16):
        sel[e, e, :] = 1.0
    small = np.zeros((128, D, 16), f32)
    for l in range(D):
        small[:, l, 0] = 0.8 - 0.6 * math.exp(-0.3 * l)
        small[0:64, l, 1] = A(diff_subln_g)[l]
        small[:, l, 8:16] = A(swa_sinks)[l][None, :]
    lamv = _bc(np.stack([np.stack([A(diff_lambda_q1)[l], A(diff_lambda_k1)[l], A(diff_lambda_q2)[l], A(diff_lambda_k2)[l]]) for l in range(D)]))
    wrc = np.concatenate([A(router_group_w), A(router_expert_w)], axis=2)
    wr = np.ascontiguousarray(wrc.reshape(D, 8, 128, 20).transpose(2, 0, 1, 3))
    rb = _bc(np.concatenate([A(router_group_b), A(router_expert_b)], axis=1))
    common = dict(cst=_mkcst(), ada_w=A(ada_w), ada_b=_pkl(A(ada_b)), ngm=_pkl(A(norm_mix_g)), ngf=_pkl(A(norm_ffn_g)),
                  w_in=A(w_in), w_out=A(w_out), lamv=lamv, small=small, wr=wr, rbias=rb, ident=np.eye(128, dtype=f32), sel=sel,
                  wgate=A(expert_w_gate), wup=A(expert_w_up), wdown=A(expert_w_down), fng=_pk(A(final_norm_g)))
    in_maps = []
    for core in range(8):
        b, r = core // 2, core % 2
        m = dict(common)
        m.update(xT=_fm(x[b, r::2, :]), cT=_pk(c[b]),
                 posb=np.ascontiguousarray(np.broadcast_to(positions[b, r::2][None, :], (128, 2048))).astype(np.int32),
                 masks=_mkmasks(r))
        in_maps.append(m)
    res = run_bass_kernel_spmd(_CACHE["nc"], in_maps, core_ids=list(range(8))).results
    out = np.zeros((4, 4096, 1024), f32)
    for i in range(8):
        b, r = i // 2, i % 2
        out[b, r::2, :] = np.asarray(res[i]["xo"], f32).transpose(1, 0, 2).reshape(1024, 2048).T
    return out
```

```python
import math
import contextlib
import numpy as np
import concourse.bass as bass
import concourse.mybir as mybir

F32 = mybir.dt.float32
BF16 = mybir.dt.bfloat16
I32 = mybir.dt.int32
AF = mybir.ActivationFunctionType
ALU = mybir.AluOpType

ENGS = ("tensor", "vector", "scalar", "gpsimd", "sync")
NDMASEM = 12


class Prog:
    def __init__(self, nc):
        self.nc = nc
        self.ops = []
        self.last_writer = {}
        self.readers = {}

    def op(self, eng, fn, reads=(), writes=(), dma=False):
        idx = len(self.ops)
        deps = set()
        for r in reads:
            lw = self.last_writer.get(r)
            if lw is not None:
                deps.add(lw)
        for w in writes:
            lw = self.last_writer.get(w)
            if lw is not None:
                deps.add(lw)
            for rd in self.readers.get(w, ()):
                deps.add(rd)
        deps.discard(idx)
        for w in writes:
            self.last_writer[w] = idx
            self.readers[w] = []
        for r in reads:
            if r not in writes:
                self.readers.setdefault(r, []).append(idx)
        self.ops.append(dict(eng=eng, fn=fn, deps=deps, dma=dma, idx=idx, cc=False))
        return idx

    def cc(self, fn, reads=(), writes=()):
        idx = self.op("gpsimd", fn, reads, writes)
        self.ops[idx]["cc"] = True
        return idx

    def mm(self, fn, reads=(), writes=()):
        return self.op("tensor", fn, reads, writes)

    def dve(self, fn, reads=(), writes=()):
        return self.op("vector", fn, reads, writes)

    def act(self, fn, reads=(), writes=()):
        return self.op("scalar", fn, reads, writes)

    def pool(self, fn, reads=(), writes=()):
        return self.op("gpsimd", fn, reads, writes)

    def dma(self, eng, out, in_, reads=(), writes=()):
        return self.op(eng, lambda e: e.dma_start(out=out, in_=in_), reads, writes, dma=True)

    def emit(self, final_wait_ops=()):
        nc = self.nc
        ops = self.ops
        has_dep = [False] * len(ops)
        for o in ops:
            for d in o["deps"]:
                if ops[d]["eng"] == "tensor" and o["eng"] == "tensor" and not ops[d]["dma"]:
                    continue
                has_dep[d] = True
        for d in final_wait_ops:
            has_dep[d] = True
        eng_cnt = {e: 0 for e in ENGS}
        dma_cnt = {e: 0 for e in ENGS}
        dma_semval = {}
        for o in ops:
            e = o["eng"]
            if o["cc"]:
                o["sig"] = ("cc", o["idx"])
            elif o["dma"]:
                k = dma_cnt[e] % NDMASEM
                dma_cnt[e] += 1
                key = (e, k)
                dma_semval[key] = dma_semval.get(key, 0) + 16
                o["sig"] = ("dma", e, k, dma_semval[key])
            elif has_dep[o["idx"]]:
                eng_cnt[e] += 1
                o["sig"] = ("eng", e, eng_cnt[e])
            else:
                o["sig"] = None
        import contextlib
        with contextlib.ExitStack() as es:
            esem = {e: es.enter_context(nc.semaphore("s_" + e)) for e in ENGS}
            dsem = {}
            for e in ("sync", "gpsimd", "scalar"):
                for k in range(NDMASEM):
                    dsem[(e, k)] = es.enter_context(nc.semaphore("d_%s_%d" % (e, k)))
            ccsem = {o["idx"]: es.enter_context(nc.semaphore("cc_%d" % o["idx"])) for o in ops if o["cc"]}
            block = es.enter_context(nc.Block())

            def make(engname):
                def body(eng):
                    waited = {}
                    for o in ops:
                        if o["eng"] != engname:
                            continue
                        need = {}
                        for d in o["deps"]:
                            od = ops[d]
                            if od["eng"] == "tensor" and engname == "tensor" and not od["dma"]:
                                continue
                            s = od["sig"]
                            if s is None:
                                continue
                            if s[0] == "dma":
                                key = ("dma", s[1], s[2])
                                val = s[3]
                            elif s[0] == "cc":
                                key = ("cc", s[1])
                                val = 1
                            else:
                                key = ("eng", s[1])
                                val = s[2]
                            if need.get(key, 0) < val:
                                need[key] = val
                        if o["dma"]:
                            s = o["sig"]
                            if s[3] > 16:
                                key = ("dma", s[1], s[2])
                                if need.get(key, 0) < s[3] - 16:
                                    need[key] = s[3] - 16
                        for key, val in need.items():
                            if waited.get(key, 0) >= val:
                                continue
                            waited[key] = val
                            if key[0] == "dma":
                                sem = dsem[(key[1], key[2])]
                            elif key[0] == "cc":
                                sem = ccsem[key[1]]
                            else:
                                sem = esem[key[1]]
                            eng.wait_ge(sem, val)
                        ins = o["fn"](eng)
                        s = o["sig"]
                        if s is not None:
                            if s[0] == "dma":
                                ins.then_inc(dsem[(s[1], s[2])], 16)
                            elif s[0] == "cc":
                                ins.then_inc(ccsem[s[1]])
                            else:
                                ins.then_inc(esem[s[1]], 1)
                    if engname == "sync":
                        for d in final_wait_ops:
                            s = ops[d]["sig"]
                            if s[0] == "dma":
                                eng.wait_ge(dsem[(s[1], s[2])], s[3])
                            else:
                                eng.wait_ge(esem[s[1]], s[2])
                return body

            block.tensor(make("tensor"))
            block.vector(make("vector"))
            block.scalar(make("scalar"))
            block.gpsimd(make("gpsimd"))
            block.sync(make("sync"))


def _barrier(self):
    last = {}
    dmas = {}
    for o in self.ops:
        if o["dma"]:
            dmas.setdefault(o["eng"], []).append(o["idx"])
        elif o["cc"]:
            pass
        else:
            last[o["eng"]] = o["idx"]
    s = set(last.values())
    for e, lst in dmas.items():
        s.update(lst[-NDMASEM:])
    for o in self.ops:
        if o["cc"]:
            s.add(o["idx"])
    self.pending_barrier = s
    self.barrier_done = set()


_orig_op = Prog.op


def _op(self, eng, fn, reads=(), writes=(), dma=False):
    idx = _orig_op(self, eng, fn, reads, writes, dma)
    pb = getattr(self, "pending_barrier", None)
    if pb and eng not in self.barrier_done:
        self.ops[idx]["deps"] |= set(pb)
        self.barrier_done.add(eng)
    return idx


Prog.op = _op
Prog.barrier = _barrier


T = 2048
GS = 512
NM = 14
LOOK = 2
NST = 3
NPT = 4
NUB = 4
MKINDS = ['A0', 'A1', 'B0p', 'B0d', 'B1p', 'B1d', 'C0p', 'C0d', 'C1p', 'C1d', 'Dp', 'Dd', 'Dp', 'Dd']
BIG = 1.0e9
PI = math.pi
DEPTH = 4
X = mybir.AxisListType.X


def build_fused(depth=DEPTH):
    nc = bass.Bass("TRN2", target_bir_lowering=False)
    dt = nc.dram_tensor

    def inp(name, shape, dtype=F32):
        return dt(name, shape, dtype, kind="ExternalInput").ap()
    xT = inp("xT", [128, 8, T])
    cT = inp("cT", [128, 8])
    posb = inp("posb", [128, T], I32)
    cst = inp("cst", [128, 8])
    masks = inp("masks", [128, NM, 128], BF16)
    ada_w = inp("ada_w", [depth, 1024, 6144])
    ada_b = inp("ada_b", [128, depth, 48])
    ngm = inp("ngm", [128, depth, 8])
    ngf = inp("ngf", [128, depth, 8])
    w_in = inp("w_in", [depth, 1024, 2304])
    w_out = inp("w_out", [depth, 1024, 1024])
    lamv = inp("lamv", [128, depth, 4, 32])
    small = inp("small", [128, depth, 16])
    wr = inp("wr", [128, depth, 8, 20])
    rbias = inp("rbias", [128, depth, 20])
    ident = inp("ident", [128, 128])
    sel = inp("sel", [16, 16, 128])
    wgate = inp("wgate", [depth, 16, 1024, 512])
    wup = inp("wup", [depth, 16, 1024, 512])
    wdown = inp("wdown", [depth, 16, 512, 1024])
    fng = inp("fng", [128, 8])
    xo = dt("xo", [128, 8, T], F32, kind="ExternalOutput").ap()
    tabD = dt("tabD", [128, 4, T], F32).ap()
    QTd = dt("QTd", [128, 8, T], BF16).ap()
    KTC = (3, 2)
    KTd_h = [dt("KTd%d" % i, [128, KTC[i] * T], BF16) for i in range(2)]
    KTg_h = [dt("KTg%d" % i, [256, KTC[i] * T], BF16) for i in range(2)]
    Vd_h = [dt("Vd%d" % i, [T // 2, 640], BF16) for i in range(2)]
    Vg_h = [dt("Vg%d" % i, [T, 640], BF16) for i in range(2)]
    KTd3 = [h.ap().rearrange("p (c n) -> p c n", c=KTC[i]) for i, h in enumerate(KTd_h)]
    KTg4 = [h.ap().rearrange("(a p) (c n) -> a p c n", a=2, c=KTC[i]) for i, h in enumerate(KTg_h)]
    Vdh = [h.ap() for h in Vd_h]
    Vgh = [h.ap() for h in Vg_h]

    def ktd(c):
        return KTd3[c // 3][:, c % 3, :]

    def ktg(a, c):
        return KTg4[c // 3][a][:, c % 3, :]

    def vd_rows(n0, cnt, step=1):
        hh = n0 // 1024
        r = n0 % 1024
        return Vdh[hh][r:r + step * (cnt - 1) + 1:step, :]

    def vg_tile(a, t):
        hh = t // 8
        r = a * 1024 + (t % 8) * 128
        return Vgh[hh][r:r + 128, :]
    groups = [[0, 1], [2, 3], [4, 5], [6, 7]]

    P = Prog(nc)
    with contextlib.ExitStack() as es:
        def sb(name, shape, dtype):
            return es.enter_context(nc.sbuf_tensor(name, shape, dtype))
        x_sb = sb("x_sb", [128, 8, T], F32)
        ARN = 50432
        arena = sb("arena", [128, ARN], BF16)
        tar = sb("tar", [128, 9, 512], F32)
        off = [0]

        def carve(n):
            a = arena[:, off[0]:off[0] + n]
            off[0] += n
            assert off[0] <= ARN, off[0]
            return a
        off[0] = 0
        Wqk = carve(8 * 1664).rearrange("p (k n) -> p k n", k=8)
        Wsqk = carve(8 * 1664).rearrange("p (k n) -> p k n", k=8)
        Wkb = carve(2048).rearrange("p (k g n) -> p k g n", k=8, g=2)
        Wskb = carve(2048).rearrange("p (k g n) -> p k g n", k=8, g=2)
        Wv = carve(8 * 640).rearrange("p (k n) -> p k n", k=8)
        sqP = carve(4096).rearrange("p (k n) -> p k n", k=8)
        hTP = carve(4096).rearrange("p (k n) -> p k n", k=8)
        tab = carve(4096).bitcast(F32).rearrange("p (k n) -> p k n", k=4)
        qkst = [carve(512) for i in range(2)]
        vst = [carve(640) for i in range(2)]
        WadaP = [arena[:, i * 8192:(i + 1) * 8192].rearrange("p (k n) -> p k n", k=8) for i in range(2)]
        posi = arena[:, 16384:16384 + 1024].bitcast(I32)
        posf = arena[:, 17408:17408 + 1024].bitcast(F32)
        ang = arena[:, 18432:18432 + 1024].bitcast(F32)
        mi = arena[:, 19456:19456 + 1024].bitcast(I32)
        off[0] = 0
        Qreg = carve(4 * T)
        slotA = [Qreg[:, i * 4096:(i + 1) * 4096].rearrange("p (t n) -> p t n", t=2) for i in range(2)]
        slotH = [Qreg[:, i * 2048:(i + 1) * 2048] for i in range(2)]
        kown = Qreg[:, 4096:8192].rearrange("p (c n) -> p c n", c=2)
        k_sb = carve(4 * T).rearrange("p (a c n) -> p a c n", a=2, c=2)
        v_sb = carve(2 * 16 * 260).rearrange("p (a t c) -> p a t c", a=2, t=16)
        v2_sb = carve(16 * 260).rearrange("p (t c) -> p t c", t=16)
        v8_sb = carve(16 * 260).rearrange("p (t c) -> p t c", t=16)
        mix = carve(8 * T).rearrange("p (c n) -> p c n", c=8)
        V_OFF = 8 * T
        V2_OFF = V_OFF + 2 * 16 * 260
        V8_OFF = V2_OFF + 16 * 260

        def vwide(base, tile_idx, col):
            o = base + tile_idx * 260 + col
            return arena[:, o:o + 128]
        off[0] = 0
        hT = carve(8 * T).rearrange("p (c n) -> p c n", c=8)
        wbuf = []
        for i in range(2):
            wg_ = carve(8 * 512).rearrange("p (k f) -> p k f", k=8)
            wu_ = carve(8 * 512).rearrange("p (k f) -> p k f", k=8)
            wd_ = carve(4 * 1024).rearrange("p (c d) -> p c d", c=4)
            wbuf.append((wg_, wu_, wd_))
        actb = [carve(4 * 512).rearrange("p (c n) -> p c n", c=4) for i in range(2)]
        gT = carve(2 * T).bitcast(F32)
        w1base = 8 * T + 3 * 4096
        h32 = arena[:, w1base:w1base + 8192].bitcast(F32).rearrange("p (k n) -> p k n", k=8)
        sq = arena[:, w1base + 8192:w1base + 8192 + 4096].rearrange("p (k n) -> p k n", k=8)
        Wo = arena[:, 0:8192].rearrange("p (k n) -> p k n", k=8)
        rr = tar[:, 0, :]; bcs = tar[:, 1, :]; tmpA = [tar[:, 2 + i, :] for i in range(3)]
        rstd = tar[:, 0, :]; hn = [tar[:, 1 + i, :] for i in range(2)]
        gbs = [tar[:, 3 + i, :] for i in range(2)]; s_sb = [tar[:, 5 + i, :] for i in range(2)]; t_sb = [tar[:, 7 + i, :] for i in range(2)]
        t1 = [tar[:, 3 + i, :] for i in range(2)]; t2 = [tar[:, 5 + i, :] for i in range(2)]
        rrs = [tar[:, 0, :], tar[:, 7, :]]
        rrh = [tar[:, 5 + i, :].bitcast(BF16)[:, 0:512] for i in range(2)]
        rrl = [tar[:, 5 + i, :].bitcast(BF16)[:, 512:1024] for i in range(2)]
        sqb = tar[:, 8, :].bitcast(BF16)[:, 0:512]

        PT = [sb("PT%d" % i, [128, 512], BF16) for i in range(NPT)]
        msk = sb("msk", [128, NM, 128], BF16)
        c_sb = sb("c_sb", [128, 8], F32)
        cact = sb("cact", [128, 8], BF16)
        adab = sb("adab", [128, depth, 48], F32)
        ngm_sb = sb("ngm_sb", [128, depth, 8], F32)
        ngf_sb = sb("ngf_sb", [128, depth, 8], F32)
        fngs = sb("fngs", [128, 8], F32)
        cs = sb("cs", [128, 8], F32)
        mod = sb("mod", [128, depth, 48], F32)
        gs1 = sb("gs1", [128, 8], F32)
        gs2 = sb("gs2", [128, 8], F32)
        ones = sb("ones", [128, 128], BF16)
        onesf = sb("onesf", [128, 128], F32)
        epsb = sb("epsb", [128, 1], F32)
        lam_sb = sb("lam_sb", [128, depth, 4, 32], F32)
        lamt = sb("lamt", [128, 2, 32], F32)
        lams = sb("lams", [128, 8], F32)
        sm = sb("sm", [128, depth, 16], F32)
        sinke = sb("sinke", [128, 8], F32)
        wr_sb = sb("wr_sb", [128, depth, 8, 20], F32)
        rb_sb = sb("rb_sb", [128, depth, 20], F32)
        id_sb = sb("id_sb", [128, 128], F32)
        sel_sb = sb("sel_sb", [16, 16, 128], F32)
        L = sb("L", [128, 16, 20], F32)
        gates = sb("gates", [128, 16, 16], F32)
        rt = sb("rt", [128, 64], F32)
        ps = es.enter_context(nc.psum_tensor("ps", [128, 8 * 512], F32))

        def bank(i):
            return ps[:, i * 512:(i + 1) * 512]

        P.dma("sync", x_sb[:], xT, writes=["x"])
        for (d_, s_, n_) in ((c_sb, cT, "c_sb"), (adab, ada_b, "adab"), (ngm_sb, ngm, "ngm"), (ngf_sb, ngf, "ngf"), (fngs, fng, "fngs"), (lam_sb, lamv, "lam_sb"), (sm, small, "sm"),
                             (wr_sb, wr, "wr_sb"), (rb_sb, rbias, "rb_sb"), (id_sb, ident, "id_sb"), (sel_sb, sel, "sel_sb"), (msk, masks, "msk"), (cs, cst, "cs")):
            P.dma("sync", d_[:], s_, writes=[n_])
        P.pool(lambda e: e.memset(ones[:], 1.0), writes=["ones"])
        P.pool(lambda e: e.memset(onesf[:], 1.0), writes=["onesf"])
        P.pool(lambda e: e.memset(epsb[:], 1e-6), writes=["epsb"])
        P.act(lambda e: e.activation(out=cact[:], in_=c_sb[:], func=AF.Silu), reads=["c_sb"], writes=["cact"])
        for tg in range(4):
            tsl = slice(tg * GS, (tg + 1) * GS)
            P.dma("sync", posi, posb[:, tsl], writes=["posi"])
            P.dve(lambda e: e.tensor_copy(posf, posi), reads=["posi"], writes=["posf"])
            for ti, (fcol, scol) in enumerate(((0, 2), (1, 3))):
                P.dve(lambda e, fcol=fcol: e.tensor_scalar(out=ang, in0=posf, scalar1=cs[:, fcol:fcol + 1], scalar2=None, op0=ALU.mult), reads=["posf", "cs"], writes=["ang"])
                for which in range(2):
                    if which == 0:
                        P.dve(lambda e: e.tensor_scalar(out=t1[0], in0=ang, scalar1=0.5 * PI, scalar2=None, op0=ALU.add), reads=["ang"], writes=["t1_0"])
                    else:
                        P.dve(lambda e: e.tensor_copy(t1[0], ang), reads=["ang"], writes=["t1_0"])
                    P.dve(lambda e: e.tensor_scalar(out=t2[0], in0=t1[0], scalar1=1.0 / (2 * PI), scalar2=None, op0=ALU.mult), reads=["t1_0"], writes=["t2_0"])
                    P.dve(lambda e: e.tensor_copy(mi, t2[0]), reads=["t2_0"], writes=["mi"])
                    P.dve(lambda e: e.tensor_copy(t2[0], mi), reads=["mi"], writes=["t2_0"])
                    P.dve(lambda e: e.scalar_tensor_tensor(out=t1[0], in0=t2[0], scalar=-2 * PI, in1=t1[0], op0=ALU.mult, op1=ALU.add), reads=["t2_0", "t1_0"], writes=["t1_0"])
                    P.dve(lambda e: e.tensor_scalar(out=t2[0], in0=t1[0], scalar1=PI, scalar2=2 * PI, op0=ALU.is_gt, op1=ALU.mult), reads=["t1_0"], writes=["t2_0"])
                    P.dve(lambda e: e.tensor_tensor(out=t1[0], in0=t1[0], in1=t2[0], op=ALU.subtract), reads=["t1_0", "t2_0"], writes=["t1_0"])
                    P.dve(lambda e: e.tensor_scalar(out=t1[0], in0=t1[0], scalar1=PI, scalar2=-PI, op0=ALU.min, op1=ALU.max), reads=["t1_0"], writes=["t1_0"])
                    if which == 0:
                        P.act(lambda e, ti=ti: e.activation(out=tab[:, 2 * ti, :], in_=t1[0], func=AF.Sin), reads=["t1_0"], writes=["tab"])
                    else:
                        P.act(lambda e, ti=ti, scol=scol: e.activation(out=tab[:, 2 * ti + 1, :], in_=t1[0], func=AF.Sin, scale=cs[:, scol:scol + 1]), reads=["t1_0", "cs"], writes=["tab"])
            P.dma("sync", tabD[:, :, tsl], tab, reads=["tab"], writes=["tabD"])
        cnt = 0
        for l in range(depth):
            ada_v = ada_w[l].rearrange("(k p) n -> p k n", p=128)
            for which in range(6):
                Wa = WadaP[cnt % 2]
                wn = "Wada%d" % (cnt % 2)
                cnt += 1
                P.dma("gpsimd", Wa, ada_v[:, :, which * 1024:(which + 1) * 1024], writes=[wn])
                for j in range(8):
                    for k in range(8):
                        P.mm(lambda e, j=j, k=k, which=which, Wa=Wa: e.matmul(bank(0)[:, which * 8 + j: which * 8 + j + 1], lhsT=Wa[:, k, j * 128:(j + 1) * 128], rhs=cact[:, k:k + 1], start=(k == 0), stop=(k == 7)),
                             reads=[wn, "cact"], writes=["modps"])
            P.dve(lambda e, l=l: e.tensor_tensor(out=mod[:, l, :], in0=bank(0)[:, 0:48], in1=adab[:, l, :], op=ALU.add), reads=["modps", "adab"], writes=["mod"])
        P.barrier()

        st = dict(i=0, pend=[], mi=0, ubank_started=set())

        def run_batch(units, scale, rowgrp):
            b = st["i"] % NST
            pi = st["i"] % NPT
            st["i"] += 1
            ptn = "PT%d" % pi
            stn = "ST%d" % b
            o = 0
            for u in units:
                u["off"] = o
                P.mm(lambda e, u=u, o=o, b=b: e.matmul(bank(b)[:, o:o + u["n"]], lhsT=u["kt"], rhs=u["q"], start=True, stop=True),
                     reads=u["kres"] + u["qres"], writes=[stn])
                o += u["n"]
            tot = o
            P.act(lambda e, b=b, pi=pi, tot=tot: e.activation(out=PT[pi][:, 0:tot], in_=bank(b)[:, 0:tot], func=AF.Exp, scale=scale), reads=[stn], writes=[ptn])
            merged = False
            if len(units) > 1 and all(u["mask"] is not None and u["mask"][1] == 128 and u["n"] == 128 for u in units):
                kinds = [MKINDS[u["mask"][2]] for u in units]
                for s0 in range(NM - len(units) + 1):
                    if MKINDS[s0:s0 + len(units)] == kinds:
                        eng = "vector"
                        nn = len(units)
                        P.op(eng, lambda e, pi=pi, s0=s0, nn=nn: e.tensor_tensor(out=PT[pi][:, 0:nn * 128], in0=PT[pi][:, 0:nn * 128], in1=msk[:, s0:s0 + nn, :].rearrange("p a b -> p (a b)"), op=ALU.mult),
                             reads=[ptn, "msk"], writes=[ptn])
                        merged = True
                        break
            if not merged:
                for u in units:
                    if u["mask"] is not None:
                        eng = "gpsimd" if st["mi"] % 2 == 0 else "vector"
                        st["mi"] += 1
                        o = u["off"]
                        m = u["mask"]
                        P.op(eng, lambda e, pi=pi, o=o, m=m: e.tensor_tensor(out=PT[pi][:, o:o + m[1]], in0=PT[pi][:, o:o + m[1]], in1=m[0], op=ALU.mult), reads=[ptn, "msk"], writes=[ptn])
            st["pend"].append((units, pi))
            while len(st["pend"]) > LOOK:
                emit_pv(*st["pend"].pop(0))
            for it_ in fin_q:
                it_[1] -= 1
            while fin_q and fin_q[0][1] <= 0:
                fin_q.pop(0)[0]()

        def emit_pv(units, b):
            ptn = "PT%d" % b
            for u in units:
                first = u["ubank"] not in st["ubank_started"]
                st["ubank_started"].add(u["ubank"])
                o = u["off"]
                P.mm(lambda e, u=u, o=o, b=b, first=first: e.matmul(u["u"], lhsT=u["v"], rhs=PT[b][:, o:o + u["n"]], start=first, stop=False, skip_group_check=True),
                     reads=[ptn] + u["vres"], writes=[u["ures"]])

        def flush():
            while st["pend"]:
                emit_pv(*st["pend"].pop(0))
        fin_q = []

        def schedule_fin(stage1, stage2):
            fin_q.append([stage1, LOOK + 1])
            fin_q.append([stage2, LOOK + 4])
            fin_q.sort(key=lambda t: t[1])

        def drain_fins():
            flush()
            while fin_q:
                fin_q.pop(0)[0]()
        ucount = [0]

        def new_ubank():
            ub = NST + (ucount[0] % NUB)
            ucount[0] += 1
            st["ubank_started"].discard(ub)
            return ub

        def rr_chain(ub, ri, sink_col):
            ures = "U%d" % ub
            rr_ = rrs[ri]
            rn = "rr%d" % ri
            if sink_col is not None:
                P.act(lambda e: e.activation(out=rr_[64:65, :], in_=bank(ub)[64:65, :], func=AF.Ln, bias=sinke[64:65, sink_col:sink_col + 1]), reads=[ures, "sinke"], writes=[rn])
            else:
                P.act(lambda e: e.activation(out=rr_[64:65, :], in_=bank(ub)[64:65, :], func=AF.Ln), reads=[ures], writes=[rn])
            P.act(lambda e: e.activation(out=rr_[64:65, :], in_=rr_[64:65, :], func=AF.Exp, scale=-1.0), reads=[rn], writes=[rn])

        def bcast(ri):
            rn = "rr%d" % ri
            rr_ = rrs[ri]
            P.mm(lambda e: e.matmul(bank(7)[0:64, :], lhsT=onesf[64:65, 0:64], rhs=rr_[64:65, :], start=True, stop=True), reads=[rn, "onesf"], writes=["BC"])
            P.act(lambda e: e.activation(out=bcs[0:64, :], in_=bank(7)[0:64, :], func=AF.Copy), reads=["BC"], writes=["bcs"])

        def finalize_simple(ub, head_feat, g, sink_col):
            ures = "U%d" % ub
            ri = ucount[0] % 2

            def stage1():
                rr_chain(ub, ri, sink_col)

            def stage2():
                bcast(ri)
                ch, po = head_feat // 128, head_feat % 128
                if po == 0:
                    P.dve(lambda e: e.tensor_tensor(out=mix[0:64, ch, g * GS:(g + 1) * GS], in0=bank(ub)[0:64, :], in1=bcs[0:64, :], op=ALU.mult), reads=[ures, "bcs"], writes=["mix"])
                else:
                    P.dve(lambda e: e.tensor_tensor(out=tmpA[0][0:64, :], in0=bank(ub)[0:64, :], in1=bcs[0:64, :], op=ALU.mult), reads=[ures, "bcs"], writes=["tmpA0"])
                    P.act(lambda e: e.activation(out=mix[po:po + 64, ch, g * GS:(g + 1) * GS], in_=tmpA[0][0:64, :], func=AF.Copy), reads=["tmpA0"], writes=["mix"])
            schedule_fin(stage1, stage2)

        def load_v1(dst, src_rows_ap, nheads, resname, extra_reads):
            P.dma("sync", dst.rearrange("p (h c) -> p h c", c=65)[:, :, 0:64], src_rows_ap.rearrange("p (h c) -> p h c", c=64), reads=extra_reads, writes=[resname])

        chunks = []
        chunks += [("w", 0, 32, ("q", 0)), ("w", 128, 32, ("q", 1)), ("w", 256, 32, ("k", 0)), ("w", 384, 32, ("k", 1))]
        chunks += [("w", 512 + 128 * i, 64, ("q", 2 + i)) for i in range(4)]
        chunks += [("w", 1024, 64, ("k", 2))]
        chunks += [("w", 1152 + 128 * i, 64, ("q", 6 + i)) for i in range(2)]
        chunks += [("w", 1408 + 128 * i, 64, ("k", 3 + i)) for i in range(2)]

        for l in range(depth):
            last = (l == depth - 1)
            lam_init_col = sm[:, l, 0:1]
            P.dve(lambda e, l=l: e.scalar_tensor_tensor(out=gs1[:], in0=mod[:, l, 8:16], scalar=1.0, in1=ngm_sb[:, l, :], op0=ALU.add, op1=ALU.mult), reads=["mod", "ngm"], writes=["gs1"])
            P.dve(lambda e, l=l: e.scalar_tensor_tensor(out=gs2[:], in0=mod[:, l, 32:40], scalar=1.0, in1=ngf_sb[:, l, :], op0=ALU.add, op1=ALU.mult), reads=["mod", "ngf"], writes=["gs2"])
            for i in range(2):
                P.dve(lambda e, i=i, l=l: e.tensor_tensor(out=lamt[:, i, :], in0=lam_sb[:, l, 2 * i, :], in1=lam_sb[:, l, 2 * i + 1, :], op=ALU.mult), reads=["lam_sb"], writes=["lamt"])
                P.dve(lambda e, i=i: e.reduce_sum(out=lams[:, i:i + 1], in_=lamt[:, i, :], axis=X), reads=["lamt"], writes=["lams"])
            P.act(lambda e: e.activation(out=lams[:, 2:4], in_=lams[:, 0:2], func=AF.Exp), reads=["lams"], writes=["lams"])
            P.dve(lambda e: e.tensor_tensor(out=lams[:, 4:5], in0=lams[:, 3:4], in1=lams[:, 2:3], op=ALU.subtract), reads=["lams"], writes=["lams"])
            P.dve(lambda e, l=l: e.tensor_tensor(out=lams[:, 4:5], in0=lams[:, 4:5], in1=sm[:, l, 0:1], op=ALU.subtract), reads=["lams", "sm"], writes=["lams"])
            P.dve(lambda e, l=l: e.tensor_scalar(out=lams[:, 5:6], in0=sm[:, l, 0:1], scalar1=-1.0, scalar2=1.0, op0=ALU.mult, op1=ALU.add), reads=["sm"], writes=["lams"])
            P.dve(lambda e, l=l: e.tensor_tensor(out=lams[:, 6:7], in0=lams[:, 5:6], in1=sm[:, l, 1:2], op=ALU.mult), reads=["lams", "sm"], writes=["lams"])
            P.act(lambda e, l=l: e.activation(out=sinke[:], in_=sm[:, l, 8:16], func=AF.Exp), reads=["sm"], writes=["sinke"])

            w_in_v = w_in[l].rearrange("(k p) n -> p k n", p=128)
            for k in range(8):
                for (d0, s0, n) in ((0, 0, 512), (512, 768, 640), (1152, 1536, 512)):
                    P.dma("gpsimd", Wqk[:, k, d0:d0 + n], w_in_v[:, k, s0:s0 + n], writes=["W%d" % k])
                for (d0, s0, n) in ((0, 512, 256), (256, 1408, 128), (384, 2048, 256)):
                    P.dma("gpsimd", Wv[:, k, d0:d0 + n], w_in_v[:, k, s0:s0 + n], writes=["Wv%d" % k])

            def swapcopy(k, c0, nheads, dh):
                h = dh // 2
                d = Wsqk[:, k, c0:c0 + nheads * dh].rearrange("p (a t c) -> p a t c", t=2, c=h)
                s = Wqk[:, k, c0:c0 + nheads * dh].rearrange("p (a t c) -> p a t c", t=2, c=h)
                P.pool(lambda e: e.tensor_copy(d[:, :, 0, :], s[:, :, 1, :]), reads=["W%d" % k], writes=["Ws%d" % k])
                P.pool(lambda e: e.tensor_copy(d[:, :, 1, :], s[:, :, 0, :]), reads=["W%d" % k], writes=["Ws%d" % k])
            for k in range(8):
                swapcopy(k, 0, 16, 32)
                swapcopy(k, 512, 10, 64)
                swapcopy(k, 1152, 8, 64)
            qi = 0
            for tg in range(4):
                tsl = slice(tg * GS, (tg + 1) * GS)
                P.dma("sync", tab, tabD[:, :, tsl], reads=["tabD"], writes=["tab"])
                P.act(lambda e, tg=tg: e.activation(out=sqP[:], in_=x_sb[:, :, tg * GS:(tg + 1) * GS], func=AF.Square), reads=["x"], writes=["sqP"])
                for k in range(8):
                    P.mm(lambda e, k=k: e.matmul(bank(7), lhsT=ones[:], rhs=sqP[:, k, :], start=(k == 0), stop=(k == 7)), reads=["sqP", "ones"], writes=["ssps"])
                P.act(lambda e: e.activation(out=rstd, in_=bank(7), func=AF.Ln, scale=1.0 / 1024, bias=epsb[:]), reads=["ssps", "epsb"], writes=["rstd"])
                P.act(lambda e: e.activation(out=rstd, in_=rstd, func=AF.Exp, scale=-0.5), reads=["rstd"], writes=["rstd"])
                for k in range(8):
                    hb = hn[k % 2]
                    hbn = "hn%d" % (k % 2)
                    P.dve(lambda e, k=k, hb=hb, tg=tg: e.tensor_tensor(out=hb, in0=x_sb[:, k, tg * GS:(tg + 1) * GS], in1=rstd, op=ALU.mult), reads=["x", "rstd"], writes=[hbn])
                    P.act(lambda e, k=k, hb=hb, l=l: e.activation(out=hTP[:, k, :], in_=hb, func=AF.Identity, scale=gs1[:, k:k + 1], bias=mod[:, l, k:k + 1]), reads=[hbn, "gs1", "mod"], writes=["hTP%d" % k])
                for ci, (kind, src, tt, dest) in enumerate(chunks):
                    b1 = 1 + 2 * (ci % 2)
                    b2 = b1 + 1
                    ti = 0 if tt == 64 else 1
                    for k in range(8):
                        if kind == "w":
                            l1 = Wqk[:, k, src:src + 128]; r1 = "W%d" % k
                        else:
                            l1 = Wkb[:, k, src, :]; r1 = "Wkb%d" % k
                        P.mm(lambda e, k=k, l1=l1, b1=b1: e.matmul(bank(b1), lhsT=l1, rhs=hTP[:, k, :], start=(k == 0), stop=(k == 7)), reads=[r1, "hTP%d" % k], writes=["bank%d" % b1])
                    for k in range(8):
                        if kind == "w":
                            l2 = Wsqk[:, k, src:src + 128]; r2 = "Ws%d" % k
                        else:
                            l2 = Wskb[:, k, src, :]; r2 = "Wskb%d" % k
                        P.mm(lambda e, k=k, l2=l2, b2=b2: e.matmul(bank(b2), lhsT=l2, rhs=hTP[:, k, :], start=(k == 0), stop=(k == 7)), reads=[r2, "hTP%d" % k], writes=["bank%d" % b2])
                    a = t1[ci % 2]; an = "t1_%d" % (ci % 2)
                    bb = t2[ci % 2]; bn = "t2_%d" % (ci % 2)
                    P.dve(lambda e, a=a, b1=b1, ti=ti: e.tensor_tensor(out=a, in0=bank(b1), in1=tab[:, 2 * ti, :], op=ALU.mult), reads=["bank%d" % b1, "tab"], writes=[an])
                    P.dve(lambda e, bb=bb, b2=b2, ti=ti: e.tensor_tensor(out=bb, in0=bank(b2), in1=tab[:, 2 * ti + 1, :], op=ALU.mult), reads=["bank%d" % b2, "tab"], writes=[bn])
                    so = qkst[qi % 2]; son = "qkst%d" % (qi % 2)
                    qi += 1
                    P.pool(lambda e, a=a, bb=bb, so=so: e.tensor_tensor(out=so, in0=a, in1=bb, op=ALU.add), reads=[an, bn], writes=[son])
                    if dest[0] == "q":
                        P.dma("sync", QTd[:, dest[1], tsl], so, reads=[son], writes=["QTd"])
                    else:
                        P.dma("sync", ktd(dest[1])[:, tsl], so, reads=[son], writes=["KTd"])
                for tt4 in range(4):
                    vb = vst[tt4 % 2]; vn = "vst%d" % (tt4 % 2)
                    for k in range(8):
                        P.mm(lambda e, k=k, tt4=tt4: e.matmul(bank(5), lhsT=hTP[:, k, tt4 * 128:(tt4 + 1) * 128], rhs=Wv[:, k, 0:512], start=(k == 0), stop=(k == 7)), reads=["Wv%d" % k, "hTP%d" % k], writes=["bank5"])
                    for k in range(8):
                        P.mm(lambda e, k=k, tt4=tt4: e.matmul(bank(6)[:, 0:128], lhsT=hTP[:, k, tt4 * 128:(tt4 + 1) * 128], rhs=Wv[:, k, 512:640], start=(k == 0), stop=(k == 7)), reads=["Wv%d" % k, "hTP%d" % k], writes=["bank6"])
                    P.act(lambda e, vb=vb: e.activation(out=vb[:, 0:512], in_=bank(5), func=AF.Copy), reads=["bank5"], writes=[vn])
                    P.act(lambda e, vb=vb: e.activation(out=vb[:, 512:640], in_=bank(6)[:, 0:128], func=AF.Copy), reads=["bank6"], writes=[vn])
                    r0 = tg * GS + tt4 * 128
                    P.dma("sync", vd_rows(r0, 128), vb, reads=[vn], writes=["Vd"])
            P.barrier()
            for i in range(2):
                P.cc(lambda e, i=i: e.collective_compute("AllGather", ALU.bypass, replica_groups=groups, ins=[KTd_h[i].ap().opt()], outs=[KTg_h[i].ap().opt()]), reads=["KTd"], writes=["KTg"])
            for i in range(2):
                P.cc(lambda e, i=i: e.collective_compute("AllGather", ALU.bypass, replica_groups=groups, ins=[Vd_h[i].ap().opt()], outs=[Vg_h[i].ap().opt()]), reads=["Vd"], writes=["Vg"])

            for a in range(2):
                for ci in range(2):
                    P.dma("sync", k_sb[:, a, ci, :], ktg(a, 0 + ci), reads=["KTg"], writes=["k_sb"])
            P.pool(lambda e: e.memset(v_sb[:], 1.0), writes=["v_sb"])
            for a in range(2):
                for t in range(16):
                    load_v1(v_sb[:, a, t, :], vg_tile(a, t)[:, 0:256], 4, "v_sb", ["Vg"])
            hcnt = [0]

            def prepA(h):
                si = hcnt[0] % 2
                hcnt[0] += 1
                sl = slotA[si]
                P.pool(lambda e: e.memset(sl, 0.0), writes=["qs%d" % si])
                for tau in range(2):
                    P.dma("sync", sl[32 * h:32 * h + 32, tau, :], QTd[32 * h:32 * h + 32, tau, :], reads=["QTd"], writes=["qs%d" % si])
                return sl, "qs%d" % si
            slotsA_ = {0: prepA(0)}
            scaleA = 32 ** -0.5
            for h in range(4):
                if h + 1 < 4:
                    slotsA_[h + 1] = prepA(h + 1)
                qsl, qsn = slotsA_[h]
                for g in range(4):
                    ubs = []
                    for tau in range(2):
                        ub = new_ubank()
                        ubs.append(ub)
                        for jp in range(4 * g + 4):
                            for a in range(2):
                                if jp < 4 * g:
                                    q0, n, m = g * GS, GS, None
                                else:
                                    q0 = jp * 128
                                    n = (g + 1) * GS - q0
                                    m = (msk[:, a, :], 128, a)
                                u = dict(kt=k_sb[:, a, tau, jp * 128:(jp + 1) * 128], q=qsl[:, tau, q0:q0 + n], n=n, mask=m,
                                         v=vwide(V_OFF, a * 16 + jp, 65 * h), u=bank(ub)[:, q0 - g * GS:q0 - g * GS + n], ures="U%d" % ub, ubank=ub,
                                         kres=["k_sb"], qres=[qsn], vres=["v_sb"])
                                run_batch([u], scaleA, 0)
                    def mkfin(ubs=ubs, h=h, g=g):
                        def stage1():
                            for tau in range(2):
                                rr_chain(ubs[tau], tau, None)

                        def stage2():
                            for tau in range(2):
                                ub = ubs[tau]
                                bcast(tau)
                                P.dve(lambda e, ub=ub, tau=tau: e.tensor_tensor(out=tmpA[tau][0:64, :], in0=bank(ub)[0:64, :], in1=bcs[0:64, :], op=ALU.mult), reads=["U%d" % ub, "bcs"], writes=["tmpA%d" % tau])
                            P.dve(lambda e: e.scalar_tensor_tensor(out=tmpA[0][0:64, :], in0=tmpA[1][0:64, :], scalar=lams[0:64, 4:5], in1=tmpA[0][0:64, :], op0=ALU.mult, op1=ALU.add), reads=["tmpA0", "tmpA1", "lams"], writes=["tmpA0"])
                            P.act(lambda e: e.activation(out=sqb[0:64, :], in_=tmpA[0][0:64, :], func=AF.Square), reads=["tmpA0"], writes=["sqb"])
                            P.mm(lambda e: e.matmul(bank(7)[0:64, :], lhsT=ones[0:64, 0:64], rhs=sqb[0:64, :], start=True, stop=True), reads=["sqb", "ones"], writes=["BC"])
                            P.act(lambda e: e.activation(out=bcs[0:64, :], in_=bank(7)[0:64, :], func=AF.Ln, scale=1.0 / 64, bias=epsb[0:64, :]), reads=["BC", "epsb"], writes=["bcs"])
                            P.act(lambda e: e.activation(out=bcs[0:64, :], in_=bcs[0:64, :], func=AF.Exp, scale=-0.5), reads=["bcs"], writes=["bcs"])
                            P.dve(lambda e: e.tensor_tensor(out=tmpA[0][0:64, :], in0=tmpA[0][0:64, :], in1=bcs[0:64, :], op=ALU.mult), reads=["tmpA0", "bcs"], writes=["tmpA0"])
                            ch, po = (64 * h) // 128, (64 * h) % 128
                            P.act(lambda e: e.activation(out=mix[po:po + 64, ch, g * GS:(g + 1) * GS], in_=tmpA[0][0:64, :], func=AF.Copy, scale=lams[0:64, 6:7]), reads=["tmpA0", "lams"], writes=["mix"])
                        return stage1, stage2
                    schedule_fin(*mkfin())
            drain_fins()

            for a in range(2):
                P.dma("sync", k_sb[:, a, 0, :], ktg(a, 2), reads=["KTg"], writes=["k_sb"])
            for a in range(2):
                for t in range(16):
                    load_v1(v_sb[:, a, t, 0:130], vg_tile(a, t)[:, 256:384], 2, "v_sb", ["Vg"])

            def prepH(dst_row, src_chunk, src_row):
                si = hcnt[0] % 2
                hcnt[0] += 1
                sl = slotH[si]
                P.pool(lambda e: e.memset(sl, 0.0), writes=["qs%d" % si])
                P.dma("sync", sl[dst_row:dst_row + 64, :], QTd[src_row:src_row + 64, src_chunk, :], reads=["QTd"], writes=["qs%d" % si])
                return sl, "qs%d" % si
            slotsB_ = {0: prepH(0, 2, 0)}
            scaleB = 64 ** -0.5
            for h in range(8):
                kv = h // 4
                if h + 1 < 8:
                    slotsB_[h + 1] = prepH(64 * ((h + 1) // 4), 2 + (h + 1) // 2, 64 * ((h + 1) % 2))
                qsl, qsn = slotsB_[h]
                for g in range(4):
                    ub = new_ubank()
                    for j in range(4 * g, 4 * g + 4):
                        units = []
                        for (a, jj, mid) in ((0, j - 1, 2), (0, j, 3), (1, j - 1, 4), (1, j, 5)):
                            if jj < 0:
                                continue
                            units.append(dict(kt=k_sb[:, a, 0, jj * 128:(jj + 1) * 128], q=qsl[:, j * 128:(j + 1) * 128], n=128, mask=(msk[:, mid, :], 128, mid),
                                              v=vwide(V_OFF, a * 16 + jj, 65 * kv), u=bank(ub)[:, (j - 4 * g) * 128:(j - 4 * g + 1) * 128], ures="U%d" % ub, ubank=ub,
                                              kres=["k_sb"], qres=[qsn], vres=["v_sb"]))
                        run_batch(units, scaleB, 0)
                    finalize_simple(ub, 256 + 64 * h, g, h)
            drain_fins()

            for a in range(2):
                for ci in range(2):
                    P.dma("sync", k_sb[:, a, ci, :], ktg(a, 3 + ci), reads=["KTg"], writes=["k_sb"])
            for a in range(2):
                for t in range(16):
                    load_v1(v_sb[:, a, t, :], vg_tile(a, t)[:, 384:640], 4, "v_sb", ["Vg"])
            P.barrier()
            for ci in range(2):
                P.dma("sync", kown[:, ci, :], ktd(3 + ci), reads=["KTd"], writes=["kown"])
            slotsC_ = {0: prepH(0, 6, 0)}
            P.pool(lambda e: e.memset(v2_sb[:], 1.0), writes=["v2_sb"])
            P.pool(lambda e: e.memset(v8_sb[:], 1.0), writes=["v8_sb"])
            for cp in range(2):
                for bt in range(8):
                    n0 = cp + 2 * 128 * bt
                    load_v1(v2_sb[:, cp * 8 + bt, :], vd_rows(n0, 128, 2)[:, 384:640], 4, "v2_sb", ["Vd"])
            for cp in range(8):
                for bt in range(2):
                    n0 = cp + 8 * 128 * bt
                    load_v1(v8_sb[:, cp * 2 + bt, :], vd_rows(n0, 128, 8)[:, 384:640], 4, "v8_sb", ["Vd"])
            for h in range(4):
                rg = 64 * (h % 2)
                qc = h // 2
                if h + 1 < 4:
                    slotsC_[h + 1] = prepH(64 * ((h + 1) % 2), 6 + (h + 1) // 2, 64 * ((h + 1) % 2))
                qsl, qsn = slotsC_[h]
                for g in range(4):
                    ub = new_ubank()
                    for j in range(4 * g, 4 * g + 4):
                        units = []
                        for (a, jj, mid) in ((0, j - 1, 6), (0, j, 7), (1, j - 1, 8), (1, j, 9)):
                            if jj < 0:
                                continue
                            units.append(dict(kt=k_sb[:, a, qc, jj * 128:(jj + 1) * 128], q=qsl[:, j * 128:(j + 1) * 128], n=128, mask=(msk[:, mid, :], 128, mid),
                                              v=vwide(V_OFF, a * 16 + jj, 65 * h), u=bank(ub)[:, (j - 4 * g) * 128:(j - 4 * g + 1) * 128], ures="U%d" % ub, ubank=ub,
                                              kres=["k_sb"], qres=[qsn], vres=["v_sb"]))
                        run_batch(units, scaleB, 0)
                    units = []
                    for cp in range(2):
                        for bt in (2 * g, 2 * g + 1):
                            qn0 = cp + 256 * bt
                            for (bb_, mid) in ((bt - 1, 10), (bt, 11)):
                                if bb_ < 0:
                                    continue
                                kn0 = cp + 256 * bb_
                                units.append(dict(kt=kown[:, qc, kn0:kn0 + 255:2], q=qsl[:, qn0:qn0 + 255:2], n=128, mask=(msk[:, mid, :], 128, mid),
                                                  v=vwide(V2_OFF, cp * 8 + bb_, 65 * h), u=bank(ub)[:, qn0 - g * GS:qn0 - g * GS + 255:2], ures="U%d" % ub, ubank=ub,
                                                  kres=["kown"], qres=[qsn], vres=["v2_sb"]))
                    for i in range(0, len(units), 4):
                        run_batch(units[i:i + 4], scaleB, 0)
                    units = []
                    bt = g // 2
                    half = g % 2
                    for cp in range(8):
                        qn0 = cp + 1024 * bt + 8 * 64 * half
                        for (bb_, mid) in ((bt - 1, 10), (bt, 11)):
                            if bb_ < 0:
                                continue
                            kn0 = cp + 1024 * bb_
                            units.append(dict(kt=kown[:, qc, kn0:kn0 + 1017:8], q=qsl[:, qn0:qn0 + 505:8], n=64, mask=(msk[:, mid, 64 * half:64 * half + 64], 64, mid),
                                              v=vwide(V8_OFF, cp * 2 + bb_, 65 * h), u=bank(ub)[:, qn0 - g * GS:qn0 - g * GS + 505:8], ures="U%d" % ub, ubank=ub,
                                              kres=["kown"], qres=[qsn], vres=["v8_sb"]))
                    for i in range(0, len(units), 8):
                        run_batch(units[i:i + 8], scaleB, 0)
                    finalize_simple(ub, 768 + 64 * h, g, None)
            drain_fins()
            P.barrier()
            wo_v = w_out[l].rearrange("(k p) n -> p k n", p=128)
            for k in range(8):
                P.dma("gpsimd", Wo[:, k, :], wo_v[:, k, :], writes=["Wo%d" % k])
            cnt = 0
            for g in range(4):
                for oc in range(8):
                    b = cnt % 2
                    cnt += 1
                    for k in range(8):
                        P.mm(lambda e, k=k, oc=oc, g=g, b=b: e.matmul(bank(b), lhsT=Wo[:, k, oc * 128:(oc + 1) * 128], rhs=mix[:, k, g * GS:(g + 1) * GS], start=(k == 0), stop=(k == 7)),
                             reads=["Wo%d" % k, "mix"], writes=["ob%d" % b])
                    P.dve(lambda e, oc=oc, g=g, b=b, l=l: e.scalar_tensor_tensor(out=x_sb[:, oc, g * GS:(g + 1) * GS], in0=bank(b), scalar=mod[:, l, 16 + oc:17 + oc], in1=x_sb[:, oc, g * GS:(g + 1) * GS], op0=ALU.mult, op1=ALU.add),
                          reads=["ob%d" % b, "mod", "x"], writes=["x"])
            P.barrier()
            def load_expert(e_, l=l):
                wg_, wu_, wd_ = wbuf[e_ % 2]
                nm = "w%d" % (e_ % 2)
                extra = (["h32_%d" % k for k in range(8)] + ["sq"]) if e_ == 1 else []
                P.dma("gpsimd", wg_, wgate[l, e_].rearrange("(k p) f -> p k f", p=128), writes=[nm + "g"] + extra)
                P.dma("gpsimd", wu_, wup[l, e_].rearrange("(k p) f -> p k f", p=128), writes=[nm + "u"] + extra)
                P.dma("gpsimd", wd_, wdown[l, e_].rearrange("(c p) d -> p c d", p=128), writes=[nm + "d"] + extra)
            load_expert(0)
            for g in range(4):
                P.act(lambda e, g=g: e.activation(out=sq[:], in_=x_sb[:, :, g * GS:(g + 1) * GS], func=AF.Square), reads=["x"], writes=["sq"])
                for k in range(8):
                    P.mm(lambda e, k=k: e.matmul(bank(7), lhsT=ones[:], rhs=sq[:, k, :], start=(k == 0), stop=(k == 7)), reads=["sq", "ones"], writes=["ssps"])
                P.act(lambda e: e.activation(out=rstd, in_=bank(7), func=AF.Ln, scale=1.0 / 1024, bias=epsb[:]), reads=["ssps", "epsb"], writes=["rstd"])
                P.act(lambda e: e.activation(out=rstd, in_=rstd, func=AF.Exp, scale=-0.5), reads=["rstd"], writes=["rstd"])
                for k in range(8):
                    hb = hn[k % 2]
                    hbn = "hn%d" % (k % 2)
                    P.dve(lambda e, k=k, hb=hb, g=g: e.tensor_tensor(out=hb, in0=x_sb[:, k, g * GS:(g + 1) * GS], in1=rstd, op=ALU.mult), reads=["x", "rstd"], writes=[hbn])
                    P.act(lambda e, k=k, hb=hb, l=l: e.activation(out=h32[:, k, :], in_=hb, func=AF.Identity, scale=gs2[:, k:k + 1], bias=mod[:, l, 24 + k:25 + k]), reads=[hbn, "gs2", "mod"], writes=["h32_%d" % k])
                    P.pool(lambda e, k=k, g=g: e.tensor_copy(hT[:, k, g * GS:(g + 1) * GS], h32[:, k, :]), reads=["h32_%d" % k], writes=["hT"])
                for tt in range(4):
                    ti = g * 4 + tt
                    for k in range(8):
                        P.mm(lambda e, k=k, tt=tt, ti=ti, l=l: e.matmul(bank(6)[:, ti * 20:(ti + 1) * 20], lhsT=h32[:, k, tt * 128:(tt + 1) * 128], rhs=wr_sb[:, l, k, :], start=(k == 0), stop=(k == 7)),
                             reads=["h32_%d" % k, "wr_sb"], writes=["Rps"])
            for ti in range(16):
                def R(i, n=1):
                    return rt[:, i:i + n]
                P.dve(lambda e, ti=ti, l=l: e.tensor_tensor(out=L[:, ti, :], in0=bank(6)[:, ti * 20:(ti + 1) * 20], in1=rb_sb[:, l, :], op=ALU.add), reads=["Rps", "rb_sb"], writes=["L"])
                gl = L[:, ti, 0:4]
                el = L[:, ti, 4:20]
                P.dve(lambda e, gl=gl: e.reduce_max(out=R(0), in_=gl, axis=X), reads=["L"], writes=["r0"])
                P.dve(lambda e: e.tensor_scalar(out=R(1), in0=R(0), scalar1=-1.0, scalar2=None, op0=ALU.mult), reads=["r0"], writes=["r1"])
                P.act(lambda e, gl=gl: e.activation(out=R(2, 4), in_=gl, func=AF.Exp, bias=R(1)), reads=["L", "r1"], writes=["r2"])
                P.dve(lambda e: e.reduce_sum(out=R(6), in_=R(2, 4), axis=X), reads=["r2"], writes=["r6"])
                P.dve(lambda e: e.reciprocal(R(6), R(6)), reads=["r6"], writes=["r6"])
                P.dve(lambda e, gl=gl: e.tensor_scalar(out=R(7, 4), in0=gl, scalar1=R(0), scalar2=None, op0=ALU.is_equal), reads=["L", "r0"], writes=["r7"])
                P.dve(lambda e: e.tensor_scalar(out=R(7, 4), in0=R(7, 4), scalar1=1.0, scalar2=BIG, op0=ALU.subtract, op1=ALU.mult), reads=["r7"], writes=["r7"])
                for gi in range(4):
                    P.dve(lambda e, gi=gi, el=el: e.tensor_scalar(out=R(12 + 4 * gi, 4), in0=el[:, 4 * gi:4 * gi + 4], scalar1=R(7 + gi), scalar2=None, op0=ALU.add), reads=["L", "r7"], writes=["elm"])
                elm = R(12, 16)
                P.dve(lambda e: e.reduce_max(out=R(28), in_=elm, axis=X), reads=["elm"], writes=["m1"])
                P.dve(lambda e: e.tensor_scalar(out=R(30, 16), in0=elm, scalar1=R(28), scalar2=None, op0=ALU.is_equal), reads=["elm", "m1"], writes=["oh1"])
                P.dve(lambda e: e.scalar_tensor_tensor(out=R(46, 16), in0=R(30, 16), scalar=-BIG, in1=elm, op0=ALU.mult, op1=ALU.add), reads=["oh1", "elm"], writes=["elm2"])
                P.dve(lambda e: e.reduce_max(out=R(29), in_=R(46, 16), axis=X), reads=["elm2"], writes=["m2"])
                P.dve(lambda e: e.tensor_scalar(out=R(46, 16), in0=R(46, 16), scalar1=R(29), scalar2=None, op0=ALU.is_equal), reads=["elm2", "m2"], writes=["elm2"])
                P.dve(lambda e: e.tensor_tensor(out=R(62), in0=R(29), in1=R(28), op=ALU.subtract), reads=["m1", "m2"], writes=["dl"])
                P.act(lambda e: e.activation(out=R(62), in_=R(62), func=AF.Exp), reads=["dl"], writes=["dl"])
                P.dve(lambda e: e.tensor_scalar(out=R(63), in0=R(62), scalar1=1.0, scalar2=None, op0=ALU.add), reads=["dl"], writes=["den"])
                P.dve(lambda e: e.reciprocal(R(63), R(63)), reads=["den"], writes=["den"])
                P.dve(lambda e: e.tensor_tensor(out=R(62), in0=R(62), in1=R(63), op=ALU.mult), reads=["dl", "den"], writes=["dl"])
                P.dve(lambda e: e.tensor_tensor(out=R(63), in0=R(63), in1=R(6), op=ALU.mult), reads=["den", "r6"], writes=["den"])
                P.dve(lambda e: e.tensor_tensor(out=R(62), in0=R(62), in1=R(6), op=ALU.mult), reads=["dl", "r6"], writes=["dl"])
                P.dve(lambda e, ti=ti: e.tensor_scalar(out=gates[:, ti, :], in0=R(30, 16), scalar1=R(63), scalar2=None, op0=ALU.mult), reads=["oh1", "den"], writes=["gates"])
                P.dve(lambda e, ti=ti: e.scalar_tensor_tensor(out=gates[:, ti, :], in0=R(46, 16), scalar=R(62), in1=gates[:, ti, :], op0=ALU.mult, op1=ALU.add), reads=["elm2", "dl", "gates"], writes=["gates"])
                P.mm(lambda e, ti=ti: e.transpose(out=bank(5)[0:16, (ti % 4) * 128:(ti % 4 + 1) * 128], in_=gates[:, ti, :], identity=id_sb[:]), reads=["gates", "id_sb"], writes=["gTps%d" % (ti % 4)])
                P.act(lambda e, ti=ti: e.activation(out=gT[0:16, ti * 128:(ti + 1) * 128], in_=bank(5)[0:16, (ti % 4) * 128:(ti % 4 + 1) * 128], func=AF.Copy), reads=["gTps%d" % (ti % 4)], writes=["gT"])
            stage2 = [None]

            def emit_stage2(e_, g, ab, l=l):
                wg_, wu_, wd_ = wbuf[e_ % 2]
                nm = "w%d" % (e_ % 2)
                for oc in range(8):
                    yb = 4 + (oc % 2)
                    for fc in range(4):
                        P.mm(lambda e, fc=fc, oc=oc, yb=yb, wd_=wd_, ab=ab: e.matmul(bank(yb), lhsT=wd_[:, fc, oc * 128:(oc + 1) * 128], rhs=actb[ab][:, fc, :], start=(fc == 0), stop=(fc == 3)),
                             reads=[nm + "d", "act%d" % ab], writes=["y%d" % yb])
                    P.dve(lambda e, oc=oc, g=g, yb=yb: e.scalar_tensor_tensor(out=x_sb[:, oc, g * GS:(g + 1) * GS], in0=bank(yb), scalar=mod[:, l, 40 + oc:41 + oc], in1=x_sb[:, oc, g * GS:(g + 1) * GS], op0=ALU.mult, op1=ALU.add),
                          reads=["y%d" % yb, "mod", "x"], writes=["x"])
            it = 0
            for e_ in range(16):
                wg_, wu_, wd_ = wbuf[e_ % 2]
                nm = "w%d" % (e_ % 2)
                for g in range(4):
                    ab = it % 2
                    it += 1
                    P.mm(lambda e, e_=e_, g=g: e.matmul(bank(6), lhsT=sel_sb[0:16, e_, :], rhs=gT[0:16, g * GS:(g + 1) * GS], start=True, stop=True), reads=["gT", "sel_sb"], writes=["G"])
                    P.act(lambda e, ab=ab: e.activation(out=gbs[ab], in_=bank(6), func=AF.Copy), reads=["G"], writes=["gbs%d" % ab])
                    for fc in range(4):
                        hb_ = 2 * (fc % 2)
                        for k in range(8):
                            P.mm(lambda e, k=k, fc=fc, g=g, hb_=hb_, wg_=wg_: e.matmul(bank(hb_), lhsT=wg_[:, k, fc * 128:(fc + 1) * 128], rhs=hT[:, k, g * GS:(g + 1) * GS], start=(k == 0), stop=(k == 7)),
                                 reads=[nm + "g", "hT"], writes=["hg%d" % hb_])
                        for k in range(8):
                            P.mm(lambda e, k=k, fc=fc, g=g, hb_=hb_, wu_=wu_: e.matmul(bank(hb_ + 1), lhsT=wu_[:, k, fc * 128:(fc + 1) * 128], rhs=hT[:, k, g * GS:(g + 1) * GS], start=(k == 0), stop=(k == 7)),
                                 reads=[nm + "u", "hT"], writes=["hu%d" % hb_])
                        sbi = fc % 2
                        P.act(lambda e, hb_=hb_, sbi=sbi: e.activation(out=s_sb[sbi], in_=bank(hb_), func=AF.Silu), reads=["hg%d" % hb_], writes=["s%d" % sbi])
                        P.dve(lambda e, hb_=hb_, sbi=sbi: e.tensor_tensor(out=t_sb[sbi], in0=bank(hb_ + 1), in1=s_sb[sbi], op=ALU.mult), reads=["hu%d" % hb_, "s%d" % sbi], writes=["t%d" % sbi])
                        P.pool(lambda e, fc=fc, sbi=sbi, ab=ab: e.tensor_tensor(out=actb[ab][:, fc, :], in0=t_sb[sbi], in1=gbs[ab], op=ALU.mult), reads=["t%d" % sbi, "gbs%d" % ab], writes=["act%d" % ab])
                    if stage2[0] is not None:
                        emit_stage2(*stage2[0])
                    stage2[0] = (e_, g, ab)
                    if g == 0 and e_ + 1 < 16:
                        load_expert(e_ + 1)
            emit_stage2(*stage2[0])
            P.barrier()
        out_ops = []
        for g in range(4):
            P.act(lambda e, g=g: e.activation(out=sq[:], in_=x_sb[:, :, g * GS:(g + 1) * GS], func=AF.Square), reads=["x"], writes=["sq"])
            for k in range(8):
                P.mm(lambda e, k=k: e.matmul(bank(7), lhsT=ones[:], rhs=sq[:, k, :], start=(k == 0), stop=(k == 7)), reads=["sq", "ones"], writes=["ssps"])
            P.act(lambda e: e.activation(out=rstd, in_=bank(7), func=AF.Ln, scale=1.0 / 1024, bias=epsb[:]), reads=["ssps", "epsb"], writes=["rstd"])
            P.act(lambda e: e.activation(out=rstd, in_=rstd, func=AF.Exp, scale=-0.5), reads=["rstd"], writes=["rstd"])
            for k in range(8):
                hb = hn[k % 2]
                hbn = "hn%d" % (k % 2)
                P.dve(lambda e, k=k, hb=hb, g=g: e.tensor_tensor(out=hb, in0=x_sb[:, k, g * GS:(g + 1) * GS], in1=rstd, op=ALU.mult), reads=["x", "rstd"], writes=[hbn])
                P.act(lambda e, k=k, hb=hb, g=g: e.activation(out=x_sb[:, k, g * GS:(g + 1) * GS], in_=hb, func=AF.Copy, scale=fngs[:, k:k + 1]), reads=[hbn, "fngs"], writes=["xf"])
        out_ops.append(P.dma("sync", xo, x_sb[:], reads=["xf"]))
        P.emit(final_wait_ops=out_ops)
    return nc


from concourse.bass_utils import run_bass_kernel_spmd
import ml_dtypes

_BF = ml_dtypes.bfloat16
_CACHE = {}


def _fm(a):
    Tn = a.shape[0]
    return np.ascontiguousarray(a.T.reshape(8, 128, Tn).transpose(1, 0, 2))


def _pk(v):
    return np.ascontiguousarray(v.reshape(-1, 128).T)


def _pkl(v):
    return np.ascontiguousarray(np.stack([_pk(v[l]) for l in range(v.shape[0])], axis=1))


def _bc(v):
    return np.ascontiguousarray(np.broadcast_to(v, (128,) + v.shape)).astype(np.float32)


def _mkcst():
    p = np.arange(128)
    c = np.zeros((128, 8), np.float32)
    c[:, 0] = (10000.0 ** (-(2 * (p % 32)).astype(np.float32) / 64)).astype(np.float32)
    c[:, 1] = (10000.0 ** (-(2 * (p % 16)).astype(np.float32) / 32)).astype(np.float32)
    c[:, 2] = np.where((p % 64) < 32, -1.0, 1.0)
    c[:, 3] = np.where((p % 32) < 16, -1.0, 1.0)
    return c


def _mkmasks(r):
    k = np.arange(128)[:, None]
    q = np.arange(128)[None, :]
    own = {}
    oth = {}
    own['A'] = (k <= q)
    oth['A'] = (k < q) if r == 0 else (k <= q)
    own['Bp'] = (k >= q + 65)
    own['Bd'] = ((q - k) >= 0) & ((q - k) <= 63)
    oth['Bp'] = (k >= q + 64) if r == 0 else (k >= q + 65)
    oth['Bd'] = (((q - k) >= 1) & ((q - k) <= 64)) if r == 0 else (((q - k) >= 0) & ((q - k) <= 63))
    own['Cp'] = (k >= q + 64)
    own['Cd'] = ((q - k) >= 0) & ((q - k) <= 64)
    oth['Cp'] = (k >= q + 64) if r == 0 else (k >= q + 65)
    oth['Cd'] = (((q - k) >= 1) & ((q - k) <= 64)) if r == 0 else (((q - k) >= 0) & ((q - k) <= 63))

    def pm(a, key):
        return own[key] if a == r else oth[key]
    m = np.zeros((14, 128, 128), np.float32)
    m[0] = pm(0, 'A'); m[1] = pm(1, 'A')
    m[2] = pm(0, 'Bp'); m[3] = pm(0, 'Bd'); m[4] = pm(1, 'Bp'); m[5] = pm(1, 'Bd')
    m[6] = pm(0, 'Cp'); m[7] = pm(0, 'Cd'); m[8] = pm(1, 'Cp'); m[9] = pm(1, 'Cd')
    m[10] = (k >= q)
    m[11] = (k <= q)
    m[12] = m[10]
    m[13] = m[11]
    return np.ascontiguousarray(m.transpose(1, 0, 2)).astype(_BF)


def kernel(x, c, positions, ada_w, ada_b, norm_mix_g, norm_ffn_g, w_in, w_out,
           diff_lambda_q1, diff_lambda_k1, diff_lambda_q2, diff_lambda_k2, diff_subln_g,
           swa_sinks, router_group_w, router_group_b, router_expert_w, router_expert_b,
           expert_w_gate, expert_w_up, expert_w_down, final_norm_g):
    f32 = np.float32
    D = DEPTH
    A = lambda v: np.ascontiguousarray(np.asarray(v, f32))
    x = A(x)
    c = A(c)
    positions = np.asarray(positions)
    if "nc" not in _CACHE:
        _CACHE["nc"] = build_fused(D)
    sel = np.zeros((16, 16, 128), f32)
    for e in range(16):
        sel[e, e, :] = 1.0
    small = np.zeros((128, D, 16), f32)
    for l in range(D):
        small[:, l, 0] = 0.8 - 0.6 * math.exp(-0.3 * l)
        small[0:64, l, 1] = A(diff_subln_g)[l]
        small[:, l, 8:16] = A(swa_sinks)[l][None, :]
    lamv = _bc(np.stack([np.stack([A(diff_lambda_q1)[l], A(diff_lambda_k1)[l], A(diff_lambda_q2)[l], A(diff_lambda_k2)[l]]) for l in range(D)]))
    wrc = np.concatenate([A(router_group_w), A(router_expert_w)], axis=2)
    wr = np.ascontiguousarray(wrc.reshape(D, 8, 128, 20).transpose(2, 0, 1, 3))
    rb = _bc(np.concatenate([A(router_group_b), A(router_expert_b)], axis=1))
    common = dict(cst=_mkcst(), ada_w=A(ada_w), ada_b=_pkl(A(ada_b)), ngm=_pkl(A(norm_mix_g)), ngf=_pkl(A(norm_ffn_g)),
                  w_in=A(w_in), w_out=A(w_out), lamv=lamv, small=small, wr=wr, rbias=rb, ident=np.eye(128, dtype=f32), sel=sel,
                  wgate=A(expert_w_gate), wup=A(expert_w_up), wdown=A(expert_w_down), fng=_pk(A(final_norm_g)))
    in_maps = []
    for core in range(8):
        b, r = core // 2, core % 2
        m = dict(common)
        m.update(xT=_fm(x[b, r::2, :]), cT=_pk(c[b]),
                 posb=np.ascontiguousarray(np.broadcast_to(positions[b, r::2][None, :], (128, 2048))).astype(np.int32),
                 masks=_mkmasks(r))
        in_maps.append(m)
    res = run_bass_kernel_spmd(_CACHE["nc"], in_maps, core_ids=list(range(8))).results
    out = np.zeros((4, 4096, 1024), f32)
    for i in range(8):
        b, r = i // 2, i % 2
        out[b, r::2, :] = np.asarray(res[i]["xo"], f32).transpose(1, 0, 2).reshape(1024, 2048).T
    return out
```

```python
import math
import contextlib
import numpy as np
import concourse.bass as bass
import concourse.mybir as mybir

F32 = mybir.dt.float32
BF16 = mybir.dt.bfloat16
I32 = mybir.dt.int32
AF = mybir.ActivationFunctionType
ALU = mybir.AluOpType

ENGS = ("tensor", "vector", "scalar", "gpsimd", "sync")
NDMASEM = 12


class Prog:
    def __init__(self, nc):
        self.nc = nc
        self.ops = []
        self.last_writer = {}
        self.readers = {}

    def op(self, eng, fn, reads=(), writes=(), dma=False):
        idx = len(self.ops)
        deps = set()
        for r in reads:
            lw = self.last_writer.get(r)
            if lw is not None:
                deps.add(lw)
        for w in writes:
            lw = self.last_writer.get(w)
            if lw is not None:
                deps.add(lw)
            for rd in self.readers.get(w, ()):
                deps.add(rd)
        deps.discard(idx)
        for w in writes:
            self.last_writer[w] = idx
            self.readers[w] = []
        for r in reads:
            if r not in writes:
                self.readers.setdefault(r, []).append(idx)
        self.ops.append(dict(eng=eng, fn=fn, deps=deps, dma=dma, idx=idx, cc=False))
        return idx

    def cc(self, fn, reads=(), writes=()):
        idx = self.op("gpsimd", fn, reads, writes)
        self.ops[idx]["cc"] = True
        return idx

    def mm(self, fn, reads=(), writes=()):
        return self.op("tensor", fn, reads, writes)

    def dve(self, fn, reads=(), writes=()):
        return self.op("vector", fn, reads, writes)

    def act(self, fn, reads=(), writes=()):
        return self.op("scalar", fn, reads, writes)

    def pool(self, fn, reads=(), writes=()):
        return self.op("gpsimd", fn, reads, writes)

    def dma(self, eng, out, in_, reads=(), writes=()):
        return self.op(eng, lambda e: e.dma_start(out=out, in_=in_), reads, writes, dma=True)

    def emit(self, final_wait_ops=()):
        nc = self.nc
        ops = self.ops
        has_dep = [False] * len(ops)
        for o in ops:
            for d in o["deps"]:
                if ops[d]["eng"] == "tensor" and o["eng"] == "tensor" and not ops[d]["dma"]:
                    continue
                has_dep[d] = True
        for d in final_wait_ops:
            has_dep[d] = True
        eng_cnt = {e: 0 for e in ENGS}
        dma_cnt = {e: 0 for e in ENGS}
        dma_semval = {}
        for o in ops:
            e = o["eng"]
            if o["cc"]:
                o["sig"] = ("cc", o["idx"])
            elif o["dma"]:
                k = dma_cnt[e] % NDMASEM
                dma_cnt[e] += 1
                key = (e, k)
                dma_semval[key] = dma_semval.get(key, 0) + 16
                o["sig"] = ("dma", e, k, dma_semval[key])
            elif has_dep[o["idx"]]:
                eng_cnt[e] += 1
                o["sig"] = ("eng", e, eng_cnt[e])
            else:
                o["sig"] = None
        import contextlib
        with contextlib.ExitStack() as es:
            esem = {e: es.enter_context(nc.semaphore("s_" + e)) for e in ENGS}
            dsem = {}
            for e in ("sync", "gpsimd", "scalar"):
                for k in range(NDMASEM):
                    dsem[(e, k)] = es.enter_context(nc.semaphore("d_%s_%d" % (e, k)))
            ccsem = {o["idx"]: es.enter_context(nc.semaphore("cc_%d" % o["idx"])) for o in ops if o["cc"]}
            block = es.enter_context(nc.Block())

            def make(engname):
                def body(eng):
                    waited = {}
                    for o in ops:
                        if o["eng"] != engname:
                            continue
                        need = {}
                        for d in o["deps"]:
                            od = ops[d]
                            if od["eng"] == "tensor" and engname == "tensor" and not od["dma"]:
                                continue
                            s = od["sig"]
                            if s is None:
                                continue
                            if s[0] == "dma":
                                key = ("dma", s[1], s[2])
                                val = s[3]
                            elif s[0] == "cc":
                                key = ("cc", s[1])
                                val = 1
                            else:
                                key = ("eng", s[1])
                                val = s[2]
                            if need.get(key, 0) < val:
                                need[key] = val
                        if o["dma"]:
                            s = o["sig"]
                            if s[3] > 16:
                                key = ("dma", s[1], s[2])
                                if need.get(key, 0) < s[3] - 16:
                                    need[key] = s[3] - 16
                        for key, val in need.items():
                            if waited.get(key, 0) >= val:
                                continue
                            waited[key] = val
                            if key[0] == "dma":
                                sem = dsem[(key[1], key[2])]
                            elif key[0] == "cc":
                                sem = ccsem[key[1]]
                            else:
                                sem = esem[key[1]]
                            eng.wait_ge(sem, val)
                        ins = o["fn"](eng)
                        s = o["sig"]
                        if s is not None:
                            if s[0] == "dma":
                                ins.then_inc(dsem[(s[1], s[2])], 16)
                            elif s[0] == "cc":
                                ins.then_inc(ccsem[s[1]])
                            else:
                                ins.then_inc(esem[s[1]], 1)
                    if engname == "sync":
                        for d in final_wait_ops:
                            s = ops[d]["sig"]
                            if s[0] == "dma":
                                eng.wait_ge(dsem[(s[1], s[2])], s[3])
                            else:
                                eng.wait_ge(esem[s[1]], s[2])
                return body

            block.tensor(make("tensor"))
            block.vector(make("vector"))
            block.scalar(make("scalar"))
            block.gpsimd(make("gpsimd"))
            block.sync(make("sync"))


def _barrier(self):
    last = {}
    dmas = {}
    for o in self.ops:
        if o["dma"]:
            dmas.setdefault(o["eng"], []).append(o["idx"])
        elif o["cc"]:
            pass
        else:
            last[o["eng"]] = o["idx"]
    s = set(last.values())
    for e, lst in dmas.items():
        s.update(lst[-NDMASEM:])
    for o in self.ops:
        if o["cc"]:
            s.add(o["idx"])
    self.pending_barrier = s
    self.barrier_done = set()


_orig_op = Prog.op


def _op(self, eng, fn, reads=(), writes=(), dma=False):
    idx = _orig_op(self, eng, fn, reads, writes, dma)
    pb = getattr(self, "pending_barrier", None)
    if pb and eng not in self.barrier_done:
        self.ops[idx]["deps"] |= set(pb)
        self.barrier_done.add(eng)
    return idx


Prog.op = _op
Prog.barrier = _barrier


T = 2048
GS = 512
NM = 15
LOOK = 2
NST = 3
NPT = 4
NUB = 4
MKINDS = ['A0', 'A1', 'B0p', 'B0d', 'B1p', 'B1d', 'C0p', 'C0d', 'C1p', 'C1d', 'Dp', 'Dd', 'Dp', 'Dd', 'Dd']
BIG = 1.0e9
PI = math.pi
DEPTH = 4
X = mybir.AxisListType.X


def build_fused(depth=DEPTH):
    nc = bass.Bass("TRN2", target_bir_lowering=False)
    dt = nc.dram_tensor

    def inp(name, shape, dtype=F32):
        return dt(name, shape, dtype, kind="ExternalInput").ap()
    xT = inp("xT", [128, 8, T])
    cT = inp("cT", [128, 8])
    posb = inp("posb", [128, T], I32)
    cst = inp("cst", [128, 8])
    masks = inp("masks", [128, NM, 128], BF16)
    masks8 = inp("masks8", [128, 4, 512], BF16)
    ada_w = inp("ada_w", [depth, 1024, 6144])
    ada_b = inp("ada_b", [128, depth, 48])
    ngm = inp("ngm", [128, depth, 8])
    ngf = inp("ngf", [128, depth, 8])
    w_in = inp("w_in", [depth, 1024, 2304])
    w_out = inp("w_out", [depth, 1024, 1024])
    lamv = inp("lamv", [128, depth, 4, 32])
    small = inp("small", [128, depth, 16])
    wr = inp("wr", [128, depth, 8, 20])
    rbias = inp("rbias", [128, depth, 20])
    ident = inp("ident", [128, 128])
    sel = inp("sel", [16, 16, 128])
    wgate = inp("wgate", [depth, 16, 1024, 512])
    wup = inp("wup", [depth, 16, 1024, 512])
    wdown = inp("wdown", [depth, 16, 512, 1024])
    fng = inp("fng", [128, 8])
    xo = dt("xo", [128, 8, T], F32, kind="ExternalOutput").ap()
    tabD = dt("tabD", [128, 4, T], F32).ap()
    QTd = dt("QTd", [128, 8, T], BF16).ap()
    KTC = (3, 2)
    KTd_h = [dt("KTd%d" % i, [128, KTC[i] * T], BF16) for i in range(2)]
    KTg_h = [dt("KTg%d" % i, [256, KTC[i] * T], BF16) for i in range(2)]
    Vd_h = [dt("Vd%d" % i, [T // 2, 640], BF16) for i in range(2)]
    Vg_h = [dt("Vg%d" % i, [T, 640], BF16) for i in range(2)]
    KTd3 = [h.ap().rearrange("p (c n) -> p c n", c=KTC[i]) for i, h in enumerate(KTd_h)]
    KTg4 = [h.ap().rearrange("(a p) (c n) -> a p c n", a=2, c=KTC[i]) for i, h in enumerate(KTg_h)]
    Vdh = [h.ap() for h in Vd_h]
    Vgh = [h.ap() for h in Vg_h]

    def ktd(c):
        return KTd3[c // 3][:, c % 3, :]

    def ktg(a, c):
        return KTg4[c // 3][a][:, c % 3, :]

    def vd_rows(n0, cnt, step=1):
        hh = n0 // 1024
        r = n0 % 1024
        return Vdh[hh][r:r + step * (cnt - 1) + 1:step, :]

    def vg_tile(a, t):
        hh = t // 8
        r = a * 1024 + (t % 8) * 128
        return Vgh[hh][r:r + 128, :]
    groups = [[0, 1], [2, 3], [4, 5], [6, 7]]

    P = Prog(nc)
    with contextlib.ExitStack() as es:
        def sb(name, shape, dtype):
            return es.enter_context(nc.sbuf_tensor(name, shape, dtype))
        x_sb = sb("x_sb", [128, 8, T], F32)
        ARN = 50432
        arena = sb("arena", [128, ARN], BF16)
        tar = sb("tar", [128, 9, 512], F32)
        off = [0]

        def carve(n):
            a = arena[:, off[0]:off[0] + n]
            off[0] += n
            assert off[0] <= ARN, off[0]
            return a
        off[0] = 0
        Wqk = carve(8 * 1664).rearrange("p (k n) -> p k n", k=8)
        Wsqk = carve(8 * 1664).rearrange("p (k n) -> p k n", k=8)
        Wkb = carve(2048).rearrange("p (k g n) -> p k g n", k=8, g=2)
        Wskb = carve(2048).rearrange("p (k g n) -> p k g n", k=8, g=2)
        Wv = carve(8 * 640).rearrange("p (k n) -> p k n", k=8)
        sqP = carve(4096).rearrange("p (k n) -> p k n", k=8)
        hTP = carve(4096).rearrange("p (k n) -> p k n", k=8)
        tab = carve(4096).bitcast(F32).rearrange("p (k n) -> p k n", k=4)
        qkst = [carve(512) for i in range(2)]
        vst = [carve(640) for i in range(2)]
        WadaP = [arena[:, i * 8192:(i + 1) * 8192].rearrange("p (k n) -> p k n", k=8) for i in range(2)]
        posi = arena[:, 16384:16384 + 1024].bitcast(I32)
        posf = arena[:, 17408:17408 + 1024].bitcast(F32)
        ang = arena[:, 18432:18432 + 1024].bitcast(F32)
        mi = arena[:, 19456:19456 + 1024].bitcast(I32)
        off[0] = 0
        Qreg = carve(4 * T)
        slotA = [Qreg[:, i * 4096:(i + 1) * 4096].rearrange("p (t n) -> p t n", t=2) for i in range(2)]
        slotH = [Qreg[:, i * 2048:(i + 1) * 2048] for i in range(2)]
        kown = Qreg[:, 4096:8192].rearrange("p (c n) -> p c n", c=2)
        k_sb = carve(4 * T).rearrange("p (a c n) -> p a c n", a=2, c=2)
        v_sb = carve(2 * 16 * 260).rearrange("p (a t c) -> p a t c", a=2, t=16)
        v2_sb = carve(16 * 260).rearrange("p (t c) -> p t c", t=16)
        v8_sb = carve(16 * 260).rearrange("p (t c) -> p t c", t=16)
        mix = carve(8 * T).rearrange("p (c n) -> p c n", c=8)
        V_OFF = 8 * T
        V2_OFF = V_OFF + 2 * 16 * 260
        V8_OFF = V2_OFF + 16 * 260

        def vwide(base, tile_idx, col):
            o = base + tile_idx * 260 + col
            return arena[:, o:o + 128]
        off[0] = 0
        hT = carve(8 * T).rearrange("p (c n) -> p c n", c=8)
        wbuf = []
        for i in range(2):
            wg_ = carve(8 * 512).rearrange("p (k f) -> p k f", k=8)
            wu_ = carve(8 * 512).rearrange("p (k f) -> p k f", k=8)
            wd_ = carve(4 * 1024).rearrange("p (c d) -> p c d", c=4)
            wbuf.append((wg_, wu_, wd_))
        actb = [carve(4 * 512).rearrange("p (c n) -> p c n", c=4) for i in range(2)]
        gT = carve(2 * T).bitcast(F32)
        w1base = 8 * T + 3 * 4096
        h32 = arena[:, w1base:w1base + 8192].bitcast(F32).rearrange("p (k n) -> p k n", k=8)
        sq = arena[:, w1base + 8192:w1base + 8192 + 4096].rearrange("p (k n) -> p k n", k=8)
        Wo = arena[:, 0:8192].rearrange("p (k n) -> p k n", k=8)
        rr = tar[:, 0, :]; bcs = tar[:, 1, :]; tmpA = [tar[:, 2 + i, :] for i in range(3)]
        rstd = tar[:, 0, :]; hn = [tar[:, 1 + i, :] for i in range(2)]
        gbs = [tar[:, 3 + i, :] for i in range(2)]; s_sb = [tar[:, 5 + i, :] for i in range(2)]; t_sb = [tar[:, 7 + i, :] for i in range(2)]
        t1 = [tar[:, 3 + i, :] for i in range(2)]; t2 = [tar[:, 5 + i, :] for i in range(2)]
        rrs = [tar[:, 0, :], tar[:, 7, :]]
        rrh = [tar[:, 5 + i, :].bitcast(BF16)[:, 0:512] for i in range(2)]
        rrl = [tar[:, 5 + i, :].bitcast(BF16)[:, 512:1024] for i in range(2)]
        sqb = tar[:, 8, :].bitcast(BF16)[:, 0:512]

        PT = [sb("PT%d" % i, [128, 512], BF16) for i in range(NPT)]
        msk = sb("msk", [128, NM, 128], BF16)
        msk8 = sb("msk8", [128, 4, 512], BF16)
        c_sb = sb("c_sb", [128, 8], F32)
        cact = sb("cact", [128, 8], BF16)
        adab = sb("adab", [128, depth, 48], F32)
        ngm_sb = sb("ngm_sb", [128, depth, 8], F32)
        ngf_sb = sb("ngf_sb", [128, depth, 8], F32)
        fngs = sb("fngs", [128, 8], F32)
        cs = sb("cs", [128, 8], F32)
        mod = sb("mod", [128, depth, 48], F32)
        gs1 = sb("gs1", [128, 8], F32)
        gs2 = sb("gs2", [128, 8], F32)
        ones = sb("ones", [128, 128], BF16)
        onesf = sb("onesf", [128, 128], F32)
        epsb = sb("epsb", [128, 1], F32)
        lam_sb = sb("lam_sb", [128, 4, 32], F32)
        lamt = sb("lamt", [128, 2, 32], F32)
        lams = sb("lams", [128, 8], F32)
        sm = sb("sm", [128, depth, 16], F32)
        sinke = sb("sinke", [128, 8], F32)
        wr_sb = sb("wr_sb", [128, depth, 8, 20], F32)
        rb_sb = sb("rb_sb", [128, depth, 20], F32)
        id_sb = sb("id_sb", [128, 128], F32)
        sel_sb = sb("sel_sb", [16, 16, 128], F32)
        L = tar[:, 4, 0:320].rearrange("p (a b) -> p a b", a=16)
        gates = tar[:, 5, 0:256].rearrange("p (a b) -> p a b", a=16)
        rt = tar[:, 3, 0:64]
        ps = es.enter_context(nc.psum_tensor("ps", [128, 8 * 512], F32))

        def bank(i):
            return ps[:, i * 512:(i + 1) * 512]

        P.dma("sync", x_sb[:], xT, writes=["x"])
        for (d_, s_, n_) in ((c_sb, cT, "c_sb"), (adab, ada_b, "adab"), (ngm_sb, ngm, "ngm"), (ngf_sb, ngf, "ngf"), (fngs, fng, "fngs"), (sm, small, "sm"),
                             (wr_sb, wr, "wr_sb"), (rb_sb, rbias, "rb_sb"), (id_sb, ident, "id_sb"), (sel_sb, sel, "sel_sb"), (msk, masks, "msk"), (msk8, masks8, "msk"), (cs, cst, "cs")):
            P.dma("sync", d_[:], s_, writes=[n_])
        P.pool(lambda e: e.memset(ones[:], 1.0), writes=["ones"])
        P.pool(lambda e: e.memset(onesf[:], 1.0), writes=["onesf"])
        P.pool(lambda e: e.memset(epsb[:], 1e-6), writes=["epsb"])
        P.act(lambda e: e.activation(out=cact[:], in_=c_sb[:], func=AF.Silu), reads=["c_sb"], writes=["cact"])
        for tg in range(4):
            tsl = slice(tg * GS, (tg + 1) * GS)
            P.dma("sync", posi, posb[:, tsl], writes=["posi"])
            P.dve(lambda e: e.tensor_copy(posf, posi), reads=["posi"], writes=["posf"])
            for ti, (fcol, scol) in enumerate(((0, 2), (1, 3))):
                P.dve(lambda e, fcol=fcol: e.tensor_scalar(out=ang, in0=posf, scalar1=cs[:, fcol:fcol + 1], scalar2=None, op0=ALU.mult), reads=["posf", "cs"], writes=["ang"])
                for which in range(2):
                    if which == 0:
                        P.dve(lambda e: e.tensor_scalar(out=t1[0], in0=ang, scalar1=0.5 * PI, scalar2=None, op0=ALU.add), reads=["ang"], writes=["t1_0"])
                    else:
                        P.dve(lambda e: e.tensor_copy(t1[0], ang), reads=["ang"], writes=["t1_0"])
                    P.dve(lambda e: e.tensor_scalar(out=t2[0], in0=t1[0], scalar1=1.0 / (2 * PI), scalar2=None, op0=ALU.mult), reads=["t1_0"], writes=["t2_0"])
                    P.dve(lambda e: e.tensor_copy(mi, t2[0]), reads=["t2_0"], writes=["mi"])
                    P.dve(lambda e: e.tensor_copy(t2[0], mi), reads=["mi"], writes=["t2_0"])
                    P.dve(lambda e: e.scalar_tensor_tensor(out=t1[0], in0=t2[0], scalar=-2 * PI, in1=t1[0], op0=ALU.mult, op1=ALU.add), reads=["t2_0", "t1_0"], writes=["t1_0"])
                    P.dve(lambda e: e.tensor_scalar(out=t2[0], in0=t1[0], scalar1=PI, scalar2=2 * PI, op0=ALU.is_gt, op1=ALU.mult), reads=["t1_0"], writes=["t2_0"])
                    P.dve(lambda e: e.tensor_tensor(out=t1[0], in0=t1[0], in1=t2[0], op=ALU.subtract), reads=["t1_0", "t2_0"], writes=["t1_0"])
                    P.dve(lambda e: e.tensor_scalar(out=t1[0], in0=t1[0], scalar1=PI, scalar2=-PI, op0=ALU.min, op1=ALU.max), reads=["t1_0"], writes=["t1_0"])
                    if which == 0:
                        P.act(lambda e, ti=ti: e.activation(out=tab[:, 2 * ti, :], in_=t1[0], func=AF.Sin), reads=["t1_0"], writes=["tab"])
                    else:
                        P.act(lambda e, ti=ti, scol=scol: e.activation(out=tab[:, 2 * ti + 1, :], in_=t1[0], func=AF.Sin, scale=cs[:, scol:scol + 1]), reads=["t1_0", "cs"], writes=["tab"])
            P.dma("sync", tabD[:, :, tsl], tab, reads=["tab"], writes=["tabD"])
        cnt = 0
        for l in range(depth):
            ada_v = ada_w[l].rearrange("(k p) n -> p k n", p=128)
            for which in range(6):
                Wa = WadaP[cnt % 2]
                wn = "Wada%d" % (cnt % 2)
                cnt += 1
                P.dma("gpsimd", Wa, ada_v[:, :, which * 1024:(which + 1) * 1024], writes=[wn])
                for j in range(8):
                    for k in range(8):
                        P.mm(lambda e, j=j, k=k, which=which, Wa=Wa: e.matmul(bank(0)[:, which * 8 + j: which * 8 + j + 1], lhsT=Wa[:, k, j * 128:(j + 1) * 128], rhs=cact[:, k:k + 1], start=(k == 0), stop=(k == 7)),
                             reads=[wn, "cact"], writes=["modps"])
            P.dve(lambda e, l=l: e.tensor_tensor(out=mod[:, l, :], in0=bank(0)[:, 0:48], in1=adab[:, l, :], op=ALU.add), reads=["modps", "adab"], writes=["mod"])
        P.barrier()

        st = dict(i=0, pend=[], mi=0, ubank_started=set())

        def run_batch(units, scale, rowgrp, m8=None):
            b = st["i"] % NST
            pi = st["i"] % NPT
            st["i"] += 1
            ptn = "PT%d" % pi
            stn = "ST%d" % b
            o = 0
            for u in units:
                u["off"] = o
                P.mm(lambda e, u=u, o=o, b=b: e.matmul(bank(b)[:, o:o + u["n"]], lhsT=u["kt"], rhs=u["q"], start=True, stop=True),
                     reads=u["kres"] + u["qres"], writes=[stn])
                o += u["n"]
            tot = o
            ptu = ["%s_%d" % (ptn, i) for i in range(8)]
            P.act(lambda e, b=b, pi=pi, tot=tot: e.activation(out=PT[pi][:, 0:tot], in_=bank(b)[:, 0:tot], func=AF.Exp, scale=scale), reads=[stn], writes=ptu)
            merged = False
            if m8 is not None:
                P.dve(lambda e, pi=pi, tot=tot: e.tensor_tensor(out=PT[pi][:, 0:tot], in0=PT[pi][:, 0:tot], in1=msk8[:, m8, 0:tot], op=ALU.mult), reads=ptu + ["msk"], writes=ptu)
                merged = True
            elif len(units) > 1 and all(u["mask"] is not None and u["mask"][1] == 128 and u["n"] == 128 for u in units):
                kinds = [MKINDS[u["mask"][2]] for u in units]
                for s0 in range(NM - len(units) + 1):
                    if MKINDS[s0:s0 + len(units)] == kinds:
                        nn = len(units)
                        P.dve(lambda e, pi=pi, s0=s0, nn=nn: e.tensor_tensor(out=PT[pi][:, 0:nn * 128], in0=PT[pi][:, 0:nn * 128], in1=msk[:, s0:s0 + nn, :].rearrange("p a b -> p (a b)"), op=ALU.mult),
                              reads=ptu + ["msk"], writes=ptu)
                        merged = True
                        break
            if not merged:
                for ui, u in enumerate(units):
                    if u["mask"] is not None:
                        eng = "gpsimd" if st["mi"] % 2 == 0 else "vector"
                        st["mi"] += 1
                        o = u["off"]
                        m = u["mask"]
                        P.op(eng, lambda e, pi=pi, o=o, m=m: e.tensor_tensor(out=PT[pi][:, o:o + m[1]], in0=PT[pi][:, o:o + m[1]], in1=m[0], op=ALU.mult), reads=[ptu[ui], "msk"], writes=[ptu[ui]])
            st["pend"].append((units, pi))
            while len(st["pend"]) > LOOK:
                emit_pv(*st["pend"].pop(0))
            for it_ in fin_q:
                it_[1] -= 1
            while fin_q and fin_q[0][1] <= 0:
                fin_q.pop(0)[0]()

        def emit_pv(units, b):
            ptn = "PT%d" % b
            for ui, u in enumerate(units):
                first = u["ubank"] not in st["ubank_started"]
                st["ubank_started"].add(u["ubank"])
                o = u["off"]
                P.mm(lambda e, u=u, o=o, b=b, first=first: e.matmul(u["u"], lhsT=u["v"], rhs=PT[b][:, o:o + u["n"]], start=first, stop=False, skip_group_check=True),
                     reads=["%s_%d" % (ptn, ui)] + u["vres"], writes=[u["ures"]])

        def flush():
            while st["pend"]:
                emit_pv(*st["pend"].pop(0))
        fin_q = []

        def schedule_fin(stage1, stage2):
            fin_q.append([stage1, LOOK + 1])
            fin_q.append([stage2, LOOK + 4])
            fin_q.sort(key=lambda t: t[1])

        def drain_fins():
            flush()
            while fin_q:
                fin_q.pop(0)[0]()
        ucount = [0]

        def new_ubank():
            ub = NST + (ucount[0] % NUB)
            ucount[0] += 1
            st["ubank_started"].discard(ub)
            return ub

        def rr_chain(ub, ri, sink_col):
            ures = "U%d" % ub
            rr_ = rrs[ri]
            rn = "rr%d" % ri
            if sink_col is not None:
                P.act(lambda e: e.activation(out=rr_[64:65, :], in_=bank(ub)[64:65, :], func=AF.Ln, bias=sinke[64:65, sink_col:sink_col + 1]), reads=[ures, "sinke"], writes=[rn])
            else:
                P.act(lambda e: e.activation(out=rr_[64:65, :], in_=bank(ub)[64:65, :], func=AF.Ln), reads=[ures], writes=[rn])
            P.act(lambda e: e.activation(out=rr_[64:65, :], in_=rr_[64:65, :], func=AF.Exp, scale=-1.0), reads=[rn], writes=[rn])

        def bcast(ri):
            rn = "rr%d" % ri
            rr_ = rrs[ri]
            P.mm(lambda e: e.matmul(bank(7)[0:64, :], lhsT=onesf[64:65, 0:64], rhs=rr_[64:65, :], start=True, stop=True), reads=[rn, "onesf"], writes=["BC"])
            P.act(lambda e: e.activation(out=bcs[0:64, :], in_=bank(7)[0:64, :], func=AF.Copy), reads=["BC"], writes=["bcs"])

        def finalize_simple(ub, head_feat, g, sink_col):
            ures = "U%d" % ub
            ri = ucount[0] % 2

            def stage1():
                rr_chain(ub, ri, sink_col)

            def stage2():
                bcast(ri)
                ch, po = head_feat // 128, head_feat % 128
                if po == 0:
                    P.dve(lambda e: e.tensor_tensor(out=mix[0:64, ch, g * GS:(g + 1) * GS], in0=bank(ub)[0:64, :], in1=bcs[0:64, :], op=ALU.mult), reads=[ures, "bcs"], writes=["mix"])
                else:
                    P.dve(lambda e: e.tensor_tensor(out=tmpA[0][0:64, :], in0=bank(ub)[0:64, :], in1=bcs[0:64, :], op=ALU.mult), reads=[ures, "bcs"], writes=["tmpA0"])
                    P.act(lambda e: e.activation(out=mix[po:po + 64, ch, g * GS:(g + 1) * GS], in_=tmpA[0][0:64, :], func=AF.Copy), reads=["tmpA0"], writes=["mix"])
            schedule_fin(stage1, stage2)

        vq = [0]

        def load_v1(dst, src_rows_ap, nheads, resname, extra_reads):
            vq[0] += 1
            P.dma("sync", dst.rearrange("p (h c) -> p h c", c=65)[:, :, 0:64], src_rows_ap.rearrange("p (h c) -> p h c", c=64), reads=extra_reads, writes=[resname])

        chunks = []
        chunks += [("w", 0, 32, ("q", 0)), ("w", 128, 32, ("q", 1)), ("w", 256, 32, ("k", 0)), ("w", 384, 32, ("k", 1))]
        chunks += [("w", 512 + 128 * i, 64, ("q", 2 + i)) for i in range(4)]
        chunks += [("w", 1024, 64, ("k", 2))]
        chunks += [("w", 1152 + 128 * i, 64, ("q", 6 + i)) for i in range(2)]
        chunks += [("w", 1408 + 128 * i, 64, ("k", 3 + i)) for i in range(2)]

        for l in range(depth):
            last = (l == depth - 1)
            lam_init_col = sm[:, l, 0:1]
            P.dve(lambda e, l=l: e.scalar_tensor_tensor(out=gs1[:], in0=mod[:, l, 8:16], scalar=1.0, in1=ngm_sb[:, l, :], op0=ALU.add, op1=ALU.mult), reads=["mod", "ngm"], writes=["gs1"])
            P.dve(lambda e, l=l: e.scalar_tensor_tensor(out=gs2[:], in0=mod[:, l, 32:40], scalar=1.0, in1=ngf_sb[:, l, :], op0=ALU.add, op1=ALU.mult), reads=["mod", "ngf"], writes=["gs2"])
            P.dma("sync", lam_sb[:], lamv[:, l, :, :], writes=["lam_sb"])
            for i in range(2):
                P.dve(lambda e, i=i, l=l: e.tensor_tensor(out=lamt[:, i, :], in0=lam_sb[:, 2 * i, :], in1=lam_sb[:, 2 * i + 1, :], op=ALU.mult), reads=["lam_sb"], writes=["lamt"])
                P.dve(lambda e, i=i: e.reduce_sum(out=lams[:, i:i + 1], in_=lamt[:, i, :], axis=X), reads=["lamt"], writes=["lams"])
            P.act(lambda e: e.activation(out=lams[:, 2:4], in_=lams[:, 0:2], func=AF.Exp), reads=["lams"], writes=["lams"])
            P.dve(lambda e: e.tensor_tensor(out=lams[:, 4:5], in0=lams[:, 3:4], in1=lams[:, 2:3], op=ALU.subtract), reads=["lams"], writes=["lams"])
            P.dve(lambda e, l=l: e.tensor_tensor(out=lams[:, 4:5], in0=lams[:, 4:5], in1=sm[:, l, 0:1], op=ALU.subtract), reads=["lams", "sm"], writes=["lams"])
            P.dve(lambda e, l=l: e.tensor_scalar(out=lams[:, 5:6], in0=sm[:, l, 0:1], scalar1=-1.0, scalar2=1.0, op0=ALU.mult, op1=ALU.add), reads=["sm"], writes=["lams"])
            P.dve(lambda e, l=l: e.tensor_tensor(out=lams[:, 6:7], in0=lams[:, 5:6], in1=sm[:, l, 1:2], op=ALU.mult), reads=["lams", "sm"], writes=["lams"])
            P.act(lambda e, l=l: e.activation(out=sinke[:], in_=sm[:, l, 8:16], func=AF.Exp), reads=["sm"], writes=["sinke"])

            w_in_v = w_in[l].rearrange("(k p) n -> p k n", p=128)
            for k in range(8):
                for (d0, s0, n) in ((0, 0, 512), (512, 768, 640), (1152, 1536, 512)):
                    P.dma("gpsimd", Wqk[:, k, d0:d0 + n], w_in_v[:, k, s0:s0 + n], writes=["W%d" % k])
                for (d0, s0, n) in ((0, 512, 256), (256, 1408, 128), (384, 2048, 256)):
                    P.dma("gpsimd", Wv[:, k, d0:d0 + n], w_in_v[:, k, s0:s0 + n], writes=["Wv%d" % k])

            def swapcopy(k, c0, nheads, dh):
                h = dh // 2
                d = Wsqk[:, k, c0:c0 + nheads * dh].rearrange("p (a t c) -> p a t c", t=2, c=h)
                s = Wqk[:, k, c0:c0 + nheads * dh].rearrange("p (a t c) -> p a t c", t=2, c=h)
                P.act(lambda e: e.activation(out=d[:, :, 0, :], in_=s[:, :, 1, :], func=AF.Copy), reads=["W%d" % k], writes=["Ws%d" % k])
                P.act(lambda e: e.activation(out=d[:, :, 1, :], in_=s[:, :, 0, :], func=AF.Copy), reads=["W%d" % k], writes=["Ws%d" % k])
            for k in range(8):
                swapcopy(k, 0, 16, 32)
                swapcopy(k, 512, 10, 64)
                swapcopy(k, 1152, 8, 64)
            qi = 0
            for tg in range(4):
                tsl = slice(tg * GS, (tg + 1) * GS)
                P.dma("sync", tab, tabD[:, :, tsl], reads=["tabD"], writes=["tab"])
                P.act(lambda e, tg=tg: e.activation(out=sqP[:], in_=x_sb[:, :, tg * GS:(tg + 1) * GS], func=AF.Square), reads=["x"], writes=["sqP"])
                for k in range(8):
                    P.mm(lambda e, k=k: e.matmul(bank(7), lhsT=ones[:], rhs=sqP[:, k, :], start=(k == 0), stop=(k == 7)), reads=["sqP", "ones"], writes=["ssps"])
                P.act(lambda e: e.activation(out=rstd, in_=bank(7), func=AF.Ln, scale=1.0 / 1024, bias=epsb[:]), reads=["ssps", "epsb"], writes=["rstd"])
                P.act(lambda e: e.activation(out=rstd, in_=rstd, func=AF.Exp, scale=-0.5), reads=["rstd"], writes=["rstd"])
                for k in range(8):
                    hb = hn[k % 2]
                    hbn = "hn%d" % (k % 2)
                    P.dve(lambda e, k=k, hb=hb, tg=tg: e.tensor_tensor(out=hb, in0=x_sb[:, k, tg * GS:(tg + 1) * GS], in1=rstd, op=ALU.mult), reads=["x", "rstd"], writes=[hbn])
                    P.act(lambda e, k=k, hb=hb, l=l: e.activation(out=hTP[:, k, :], in_=hb, func=AF.Identity, scale=gs1[:, k:k + 1], bias=mod[:, l, k:k + 1]), reads=[hbn, "gs1", "mod"], writes=["hTP%d" % k])
                for ci, (kind, src, tt, dest) in enumerate(chunks):
                    b1 = 1 + 2 * (ci % 2)
                    b2 = b1 + 1
                    ti = 0 if tt == 64 else 1
                    for k in range(8):
                        if kind == "w":
                            l1 = Wqk[:, k, src:src + 128]; r1 = "W%d" % k
                        else:
                            l1 = Wkb[:, k, src, :]; r1 = "Wkb%d" % k
                        P.mm(lambda e, k=k, l1=l1, b1=b1: e.matmul(bank(b1), lhsT=l1, rhs=hTP[:, k, :], start=(k == 0), stop=(k == 7)), reads=[r1, "hTP%d" % k], writes=["bank%d" % b1])
                    for k in range(8):
                        if kind == "w":
                            l2 = Wsqk[:, k, src:src + 128]; r2 = "Ws%d" % k
                        else:
                            l2 = Wskb[:, k, src, :]; r2 = "Wskb%d" % k
                        P.mm(lambda e, k=k, l2=l2, b2=b2: e.matmul(bank(b2), lhsT=l2, rhs=hTP[:, k, :], start=(k == 0), stop=(k == 7)), reads=[r2, "hTP%d" % k], writes=["bank%d" % b2])
                    a = t1[ci % 2]; an = "t1_%d" % (ci % 2)
                    bb = t2[ci % 2]; bn = "t2_%d" % (ci % 2)
                    P.dve(lambda e, a=a, b1=b1, ti=ti: e.tensor_tensor(out=a, in0=bank(b1), in1=tab[:, 2 * ti, :], op=ALU.mult), reads=["bank%d" % b1, "tab"], writes=[an])
                    P.dve(lambda e, bb=bb, b2=b2, ti=ti: e.tensor_tensor(out=bb, in0=bank(b2), in1=tab[:, 2 * ti + 1, :], op=ALU.mult), reads=["bank%d" % b2, "tab"], writes=[bn])
                    so = qkst[qi % 2]; son = "qkst%d" % (qi % 2)
                    qi += 1
                    P.pool(lambda e, a=a, bb=bb, so=so: e.tensor_tensor(out=so, in0=a, in1=bb, op=ALU.add), reads=[an, bn], writes=[son])
                    if dest[0] == "q":
                        P.dma("sync", QTd[:, dest[1], tsl], so, reads=[son], writes=["QTd"])
                    else:
                        P.dma("sync", ktd(dest[1])[:, tsl], so, reads=[son], writes=["KTd"])
                for tt4 in range(4):
                    vb = vst[tt4 % 2]; vn = "vst%d" % (tt4 % 2)
                    for k in range(8):
                        P.mm(lambda e, k=k, tt4=tt4: e.matmul(bank(5), lhsT=hTP[:, k, tt4 * 128:(tt4 + 1) * 128], rhs=Wv[:, k, 0:512], start=(k == 0), stop=(k == 7)), reads=["Wv%d" % k, "hTP%d" % k], writes=["bank5"])
                    for k in range(8):
                        P.mm(lambda e, k=k, tt4=tt4: e.matmul(bank(6)[:, 0:128], lhsT=hTP[:, k, tt4 * 128:(tt4 + 1) * 128], rhs=Wv[:, k, 512:640], start=(k == 0), stop=(k == 7)), reads=["Wv%d" % k, "hTP%d" % k], writes=["bank6"])
                    P.act(lambda e, vb=vb: e.activation(out=vb[:, 0:512], in_=bank(5), func=AF.Copy), reads=["bank5"], writes=[vn])
                    P.act(lambda e, vb=vb: e.activation(out=vb[:, 512:640], in_=bank(6)[:, 0:128], func=AF.Copy), reads=["bank6"], writes=[vn])
                    r0 = tg * GS + tt4 * 128
                    P.dma("sync", vd_rows(r0, 128), vb, reads=[vn], writes=["Vd"])
            P.barrier()
            for i in range(2):
                P.cc(lambda e, i=i: e.collective_compute("AllGather", ALU.bypass, replica_groups=groups, ins=[KTd_h[i].ap().opt()], outs=[KTg_h[i].ap().opt()]), reads=["KTd"], writes=["KTg"])
            for i in range(2):
                P.cc(lambda e, i=i: e.collective_compute("AllGather", ALU.bypass, replica_groups=groups, ins=[Vd_h[i].ap().opt()], outs=[Vg_h[i].ap().opt()]), reads=["Vd"], writes=["Vg"])

            P.pool(lambda e: e.memset(v2_sb[:], 1.0), writes=["v2_sb"])
            P.pool(lambda e: e.memset(v8_sb[:], 1.0), writes=["v8_sb"])
            for cp in range(2):
                for bt in range(8):
                    n0 = cp + 2 * 128 * bt
                    load_v1(v2_sb[:, cp * 8 + bt, :], vd_rows(n0, 128, 2)[:, 384:640], 4, "v2_sb", ["Vd"])
            for cp in range(8):
                for bt in range(2):
                    n0 = cp + 8 * 128 * bt
                    load_v1(v8_sb[:, cp * 2 + bt, :], vd_rows(n0, 128, 8)[:, 384:640], 4, "v8_sb", ["Vd"])
            for a in range(2):
                for ci in range(2):
                    P.dma("sync", k_sb[:, a, ci, :], ktg(a, 0 + ci), reads=["KTg"], writes=["k_sb"])
            P.pool(lambda e: e.memset(v_sb[:], 1.0), writes=["v_sb"])
            for a in range(2):
                for t in range(16):
                    load_v1(v_sb[:, a, t, :], vg_tile(a, t)[:, 0:256], 4, "v_sb", ["Vg"])
            hcnt = [0]

            def prepA(h):
                si = hcnt[0] % 2
                hcnt[0] += 1
                sl = slotA[si]
                P.pool(lambda e: e.memset(sl, 0.0), writes=["qs%d" % si])
                for tau in range(2):
                    P.dma("sync", sl[32 * h:32 * h + 32, tau, :], QTd[32 * h:32 * h + 32, tau, :], reads=["QTd"], writes=["qs%d" % si])
                return sl, "qs%d" % si
            slotsA_ = {0: prepA(0)}
            scaleA = 32 ** -0.5
            for h in range(4):
                if h + 1 < 4:
                    slotsA_[h + 1] = prepA(h + 1)
                qsl, qsn = slotsA_[h]
                for g in range(4):
                    ubs = []
                    for tau in range(2):
                        ub = new_ubank()
                        ubs.append(ub)
                        for jp in range(4 * g + 4):
                            for a in range(2):
                                if jp < 4 * g:
                                    q0, n, m = g * GS, GS, None
                                else:
                                    q0 = jp * 128
                                    n = (g + 1) * GS - q0
                                    m = (msk[:, a, :], 128, a)
                                u = dict(kt=k_sb[:, a, tau, jp * 128:(jp + 1) * 128], q=qsl[:, tau, q0:q0 + n], n=n, mask=m,
                                         v=vwide(V_OFF, a * 16 + jp, 65 * h), u=bank(ub)[:, q0 - g * GS:q0 - g * GS + n], ures="U%d" % ub, ubank=ub,
                                         kres=["k_sb"], qres=[qsn], vres=["v_sb"])
                                run_batch([u], scaleA, 0)
                    def mkfin(ubs=ubs, h=h, g=g):
                        def stage1():
                            for tau in range(2):
                                rr_chain(ubs[tau], tau, None)

                        def stage2():
                            for tau in range(2):
                                ub = ubs[tau]
                                bcast(tau)
                                P.dve(lambda e, ub=ub, tau=tau: e.tensor_tensor(out=tmpA[tau][0:64, :], in0=bank(ub)[0:64, :], in1=bcs[0:64, :], op=ALU.mult), reads=["U%d" % ub, "bcs"], writes=["tmpA%d" % tau])
                            P.dve(lambda e: e.scalar_tensor_tensor(out=tmpA[0][0:64, :], in0=tmpA[1][0:64, :], scalar=lams[0:64, 4:5], in1=tmpA[0][0:64, :], op0=ALU.mult, op1=ALU.add), reads=["tmpA0", "tmpA1", "lams"], writes=["tmpA0"])
                            P.act(lambda e: e.activation(out=sqb[0:64, :], in_=tmpA[0][0:64, :], func=AF.Square), reads=["tmpA0"], writes=["sqb"])
                            P.mm(lambda e: e.matmul(bank(7)[0:64, :], lhsT=ones[0:64, 0:64], rhs=sqb[0:64, :], start=True, stop=True), reads=["sqb", "ones"], writes=["BC"])
                            P.act(lambda e: e.activation(out=bcs[0:64, :], in_=bank(7)[0:64, :], func=AF.Ln, scale=1.0 / 64, bias=epsb[0:64, :]), reads=["BC", "epsb"], writes=["bcs"])
                            P.act(lambda e: e.activation(out=bcs[0:64, :], in_=bcs[0:64, :], func=AF.Exp, scale=-0.5), reads=["bcs"], writes=["bcs"])
                            P.dve(lambda e: e.tensor_tensor(out=tmpA[0][0:64, :], in0=tmpA[0][0:64, :], in1=bcs[0:64, :], op=ALU.mult), reads=["tmpA0", "bcs"], writes=["tmpA0"])
                            ch, po = (64 * h) // 128, (64 * h) % 128
                            P.act(lambda e: e.activation(out=mix[po:po + 64, ch, g * GS:(g + 1) * GS], in_=tmpA[0][0:64, :], func=AF.Copy, scale=lams[0:64, 6:7]), reads=["tmpA0", "lams"], writes=["mix"])
                        return stage1, stage2
                    schedule_fin(*mkfin())
            drain_fins()

            for ci in range(2):
                P.dma("sync", kown[:, ci, :], ktd(3 + ci), reads=["KTd"], writes=["kown", "qs1"])
            for a in range(2):
                P.dma("sync", k_sb[:, a, 0, :], ktg(a, 2), reads=["KTg"], writes=["k_sb"])
            for a in range(2):
                for t in range(16):
                    load_v1(v_sb[:, a, t, 0:130], vg_tile(a, t)[:, 256:384], 2, "v_sb", ["Vg"])

            def prepH(dst_row, src_chunk, src_row):
                si = hcnt[0] % 2
                hcnt[0] += 1
                sl = slotH[si]
                P.pool(lambda e: e.memset(sl, 0.0), writes=["qs%d" % si])
                P.dma("sync", sl[dst_row:dst_row + 64, :], QTd[src_row:src_row + 64, src_chunk, :], reads=["QTd"], writes=["qs%d" % si])
                return sl, "qs%d" % si
            slotsB_ = {0: prepH(0, 2, 0)}
            scaleB = 64 ** -0.5
            for h in range(8):
                kv = h // 4
                if h + 1 < 8:
                    slotsB_[h + 1] = prepH(64 * ((h + 1) // 4), 2 + (h + 1) // 2, 64 * ((h + 1) % 2))
                qsl, qsn = slotsB_[h]
                for g in range(4):
                    ub = new_ubank()
                    for j in range(4 * g, 4 * g + 4):
                        units = []
                        for (a, jj, mid) in ((0, j - 1, 2), (0, j, 3), (1, j - 1, 4), (1, j, 5)):
                            if jj < 0:
                                continue
                            units.append(dict(kt=k_sb[:, a, 0, jj * 128:(jj + 1) * 128], q=qsl[:, j * 128:(j + 1) * 128], n=128, mask=(msk[:, mid, :], 128, mid),
                                              v=vwide(V_OFF, a * 16 + jj, 65 * kv), u=bank(ub)[:, (j - 4 * g) * 128:(j - 4 * g + 1) * 128], ures="U%d" % ub, ubank=ub,
                                              kres=["k_sb"], qres=[qsn], vres=["v_sb"]))
                        run_batch(units, scaleB, 0)
                    finalize_simple(ub, 256 + 64 * h, g, h)
            drain_fins()

            for a in range(2):
                for ci in range(2):
                    P.dma("sync", k_sb[:, a, ci, :], ktg(a, 3 + ci), reads=["KTg"], writes=["k_sb"])
            for a in range(2):
                for t in range(16):
                    load_v1(v_sb[:, a, t, :], vg_tile(a, t)[:, 384:640], 4, "v_sb", ["Vg"])
            slotsC_ = {0: prepH(0, 6, 0)}
            for h in range(4):
                rg = 64 * (h % 2)
                qc = h // 2
                if h + 1 < 4:
                    slotsC_[h + 1] = prepH(64 * ((h + 1) % 2), 6 + (h + 1) // 2, 64 * ((h + 1) % 2))
                qsl, qsn = slotsC_[h]
                for g in range(4):
                    ub = new_ubank()
                    for j in range(4 * g, 4 * g + 4):
                        units = []
                        for (a, jj, mid) in ((0, j - 1, 6), (0, j, 7), (1, j - 1, 8), (1, j, 9)):
                            if jj < 0:
                                continue
                            units.append(dict(kt=k_sb[:, a, qc, jj * 128:(jj + 1) * 128], q=qsl[:, j * 128:(j + 1) * 128], n=128, mask=(msk[:, mid, :], 128, mid),
                                              v=vwide(V_OFF, a * 16 + jj, 65 * h), u=bank(ub)[:, (j - 4 * g) * 128:(j - 4 * g + 1) * 128], ures="U%d" % ub, ubank=ub,
                                              kres=["k_sb"], qres=[qsn], vres=["v_sb"]))
                        run_batch(units, scaleB, 0)
                    units = []
                    for cp in range(2):
                        for bt in (2 * g, 2 * g + 1):
                            qn0 = cp + 256 * bt
                            for (bb_, mid) in ((bt - 1, 10), (bt, 11)):
                                if bb_ < 0:
                                    continue
                                kn0 = cp + 256 * bb_
                                units.append(dict(kt=kown[:, qc, kn0:kn0 + 255:2], q=qsl[:, qn0:qn0 + 255:2], n=128, mask=(msk[:, mid, :], 128, mid),
                                                  v=vwide(V2_OFF, cp * 8 + bb_, 65 * h), u=bank(ub)[:, qn0 - g * GS:qn0 - g * GS + 255:2], ures="U%d" % ub, ubank=ub,
                                                  kres=["kown"], qres=[qsn], vres=["v2_sb"], pair=(bt > 0)))
                    units.sort(key=lambda u: 0 if u["pair"] else 1)
                    for i in range(0, len(units), 4):
                        run_batch(units[i:i + 4], scaleB, 0)
                    units = []
                    bt = g // 2
                    half = g % 2
                    for cp in range(8):
                        qn0 = cp + 1024 * bt + 8 * 64 * half
                        for (bb_, mid) in ((bt - 1, 10), (bt, 11)):
                            if bb_ < 0:
                                continue
                            kn0 = cp + 1024 * bb_
                            units.append(dict(kt=kown[:, qc, kn0:kn0 + 1017:8], q=qsl[:, qn0:qn0 + 505:8], n=64, mask=(msk[:, mid, 64 * half:64 * half + 64], 64, mid),
                                              v=vwide(V8_OFF, cp * 2 + bb_, 65 * h), u=bank(ub)[:, qn0 - g * GS:qn0 - g * GS + 505:8], ures="U%d" % ub, ubank=ub,
                                              kres=["kown"], qres=[qsn], vres=["v8_sb"]))
                    for i in range(0, len(units), 8):
                        run_batch(units[i:i + 8], scaleB, 0, m8=(half if bt > 0 else 2 + half))
                    finalize_simple(ub, 768 + 64 * h, g, None)
            drain_fins()
            P.barrier()
            wo_v = w_out[l].rearrange("(k p) n -> p k n", p=128)
            for k in range(8):
                P.dma("gpsimd", Wo[:, k, :], wo_v[:, k, :], writes=["Wo%d" % k])
            cnt = 0
            for g in range(4):
                for oc in range(8):
                    b = cnt % 2
                    cnt += 1
                    for k in range(8):
                        P.mm(lambda e, k=k, oc=oc, g=g, b=b: e.matmul(bank(b), lhsT=Wo[:, k, oc * 128:(oc + 1) * 128], rhs=mix[:, k, g * GS:(g + 1) * GS], start=(k == 0), stop=(k == 7)),
                             reads=["Wo%d" % k, "mix"], writes=["ob%d" % b])
                    P.dve(lambda e, oc=oc, g=g, b=b, l=l: e.scalar_tensor_tensor(out=x_sb[:, oc, g * GS:(g + 1) * GS], in0=bank(b), scalar=mod[:, l, 16 + oc:17 + oc], in1=x_sb[:, oc, g * GS:(g + 1) * GS], op0=ALU.mult, op1=ALU.add),
                          reads=["ob%d" % b, "mod", "x"], writes=["x"])
            P.barrier()
            def load_expert(e_, l=l):
                wg_, wu_, wd_ = wbuf[e_ % 2]
                nm = "w%d" % (e_ % 2)
                extra = (["h32_%d" % k for k in range(8)] + ["sq"]) if e_ == 1 else []
                P.dma("gpsimd", wg_, wgate[l, e_].rearrange("(k p) f -> p k f", p=128), writes=[nm + "g"] + extra)
                P.dma("gpsimd", wu_, wup[l, e_].rearrange("(k p) f -> p k f", p=128), writes=[nm + "u"] + extra)
                P.dma("gpsimd", wd_, wdown[l, e_].rearrange("(c p) d -> p c d", p=128), writes=[nm + "d"] + extra)
            load_expert(0)
            for g in range(4):
                P.act(lambda e, g=g: e.activation(out=sq[:], in_=x_sb[:, :, g * GS:(g + 1) * GS], func=AF.Square), reads=["x"], writes=["sq"])
                for k in range(8):
                    P.mm(lambda e, k=k: e.matmul(bank(7), lhsT=ones[:], rhs=sq[:, k, :], start=(k == 0), stop=(k == 7)), reads=["sq", "ones"], writes=["ssps"])
                P.act(lambda e: e.activation(out=rstd, in_=bank(7), func=AF.Ln, scale=1.0 / 1024, bias=epsb[:]), reads=["ssps", "epsb"], writes=["rstd"])
                P.act(lambda e: e.activation(out=rstd, in_=rstd, func=AF.Exp, scale=-0.5), reads=["rstd"], writes=["rstd"])
                for k in range(8):
                    hb = hn[k % 2]
                    hbn = "hn%d" % (k % 2)
                    P.dve(lambda e, k=k, hb=hb, g=g: e.tensor_tensor(out=hb, in0=x_sb[:, k, g * GS:(g + 1) * GS], in1=rstd, op=ALU.mult), reads=["x", "rstd"], writes=[hbn])
                    P.act(lambda e, k=k, hb=hb, l=l: e.activation(out=h32[:, k, :], in_=hb, func=AF.Identity, scale=gs2[:, k:k + 1], bias=mod[:, l, 24 + k:25 + k]), reads=[hbn, "gs2", "mod"], writes=["h32_%d" % k])
                    P.pool(lambda e, k=k, g=g: e.tensor_copy(hT[:, k, g * GS:(g + 1) * GS], h32[:, k, :]), reads=["h32_%d" % k], writes=["hT"])
                for tt in range(4):
                    ti = g * 4 + tt
                    for k in range(8):
                        P.mm(lambda e, k=k, tt=tt, ti=ti, l=l: e.matmul(bank(6)[:, ti * 20:(ti + 1) * 20], lhsT=h32[:, k, tt * 128:(tt + 1) * 128], rhs=wr_sb[:, l, k, :], start=(k == 0), stop=(k == 7)),
                             reads=["h32_%d" % k, "wr_sb"], writes=["Rps"])
            for ti in range(16):
                def R(i, n=1):
                    return rt[:, i:i + n]
                P.dve(lambda e, ti=ti, l=l: e.tensor_tensor(out=L[:, ti, :], in0=bank(6)[:, ti * 20:(ti + 1) * 20], in1=rb_sb[:, l, :], op=ALU.add), reads=["Rps", "rb_sb"], writes=["L"])
                gl = L[:, ti, 0:4]
                el = L[:, ti, 4:20]
                P.dve(lambda e, gl=gl: e.reduce_max(out=R(0), in_=gl, axis=X), reads=["L"], writes=["r0"])
                P.dve(lambda e: e.tensor_scalar(out=R(1), in0=R(0), scalar1=-1.0, scalar2=None, op0=ALU.mult), reads=["r0"], writes=["r1"])
                P.act(lambda e, gl=gl: e.activation(out=R(2, 4), in_=gl, func=AF.Exp, bias=R(1)), reads=["L", "r1"], writes=["r2"])
                P.dve(lambda e: e.reduce_sum(out=R(6), in_=R(2, 4), axis=X), reads=["r2"], writes=["r6"])
                P.dve(lambda e: e.reciprocal(R(6), R(6)), reads=["r6"], writes=["r6"])
                P.dve(lambda e, gl=gl: e.tensor_scalar(out=R(7, 4), in0=gl, scalar1=R(0), scalar2=None, op0=ALU.is_equal), reads=["L", "r0"], writes=["r7"])
                P.dve(lambda e: e.tensor_scalar(out=R(7, 4), in0=R(7, 4), scalar1=1.0, scalar2=BIG, op0=ALU.subtract, op1=ALU.mult), reads=["r7"], writes=["r7"])
                for gi in range(4):
                    P.dve(lambda e, gi=gi, el=el: e.tensor_scalar(out=R(12 + 4 * gi, 4), in0=el[:, 4 * gi:4 * gi + 4], scalar1=R(7 + gi), scalar2=None, op0=ALU.add), reads=["L", "r7"], writes=["elm"])
                elm = R(12, 16)
                P.dve(lambda e: e.reduce_max(out=R(28), in_=elm, axis=X), reads=["elm"], writes=["m1"])
                P.dve(lambda e: e.tensor_scalar(out=R(30, 16), in0=elm, scalar1=R(28), scalar2=None, op0=ALU.is_equal), reads=["elm", "m1"], writes=["oh1"])
                P.dve(lambda e: e.scalar_tensor_tensor(out=R(46, 16), in0=R(30, 16), scalar=-BIG, in1=elm, op0=ALU.mult, op1=ALU.add), reads=["oh1", "elm"], writes=["elm2"])
                P.dve(lambda e: e.reduce_max(out=R(29), in_=R(46, 16), axis=X), reads=["elm2"], writes=["m2"])
                P.dve(lambda e: e.tensor_scalar(out=R(46, 16), in0=R(46, 16), scalar1=R(29), scalar2=None, op0=ALU.is_equal), reads=["elm2", "m2"], writes=["elm2"])
                P.dve(lambda e: e.tensor_tensor(out=R(62), in0=R(29), in1=R(28), op=ALU.subtract), reads=["m1", "m2"], writes=["dl"])
                P.act(lambda e: e.activation(out=R(62), in_=R(62), func=AF.Exp), reads=["dl"], writes=["dl"])
                P.dve(lambda e: e.tensor_scalar(out=R(63), in0=R(62), scalar1=1.0, scalar2=None, op0=ALU.add), reads=["dl"], writes=["den"])
                P.dve(lambda e: e.reciprocal(R(63), R(63)), reads=["den"], writes=["den"])
                P.dve(lambda e: e.tensor_tensor(out=R(62), in0=R(62), in1=R(63), op=ALU.mult), reads=["dl", "den"], writes=["dl"])
                P.dve(lambda e: e.tensor_tensor(out=R(63), in0=R(63), in1=R(6), op=ALU.mult), reads=["den", "r6"], writes=["den"])
                P.dve(lambda e: e.tensor_tensor(out=R(62), in0=R(62), in1=R(6), op=ALU.mult), reads=["dl", "r6"], writes=["dl"])
                P.dve(lambda e, ti=ti: e.tensor_scalar(out=gates[:, ti, :], in0=R(30, 16), scalar1=R(63), scalar2=None, op0=ALU.mult), reads=["oh1", "den"], writes=["gates"])
                P.dve(lambda e, ti=ti: e.scalar_tensor_tensor(out=gates[:, ti, :], in0=R(46, 16), scalar=R(62), in1=gates[:, ti, :], op0=ALU.mult, op1=ALU.add), reads=["elm2", "dl", "gates"], writes=["gates"])
                P.mm(lambda e, ti=ti: e.transpose(out=bank(5)[0:16, (ti % 4) * 128:(ti % 4 + 1) * 128], in_=gates[:, ti, :], identity=id_sb[:]), reads=["gates", "id_sb"], writes=["gTps%d" % (ti % 4)])
                P.act(lambda e, ti=ti: e.activation(out=gT[0:16, ti * 128:(ti + 1) * 128], in_=bank(5)[0:16, (ti % 4) * 128:(ti % 4 + 1) * 128], func=AF.Copy), reads=["gTps%d" % (ti % 4)], writes=["gT"])
            stage2 = [None]

            def emit_stage2(e_, g, ab, l=l):
                wg_, wu_, wd_ = wbuf[e_ % 2]
                nm = "w%d" % (e_ % 2)
                for oc in range(8):
                    yb = 4 + (oc % 2)
                    for fc in range(4):
                        P.mm(lambda e, fc=fc, oc=oc, yb=yb, wd_=wd_, ab=ab: e.matmul(bank(yb), lhsT=wd_[:, fc, oc * 128:(oc + 1) * 128], rhs=actb[ab][:, fc, :], start=(fc == 0), stop=(fc == 3)),
                             reads=[nm + "d", "act%d" % ab], writes=["y%d" % yb])
                    P.dve(lambda e, oc=oc, g=g, yb=yb: e.scalar_tensor_tensor(out=x_sb[:, oc, g * GS:(g + 1) * GS], in0=bank(yb), scalar=mod[:, l, 40 + oc:41 + oc], in1=x_sb[:, oc, g * GS:(g + 1) * GS], op0=ALU.mult, op1=ALU.add),
                          reads=["y%d" % yb, "mod", "x"], writes=["x"])
            it = 0
            for e_ in range(16):
                wg_, wu_, wd_ = wbuf[e_ % 2]
                nm = "w%d" % (e_ % 2)
                for g in range(4):
                    ab = it % 2
                    it += 1
                    P.mm(lambda e, e_=e_, g=g: e.matmul(bank(6), lhsT=sel_sb[0:16, e_, :], rhs=gT[0:16, g * GS:(g + 1) * GS], start=True, stop=True), reads=["gT", "sel_sb"], writes=["G"])
                    P.act(lambda e, ab=ab: e.activation(out=gbs[ab], in_=bank(6), func=AF.Copy), reads=["G"], writes=["gbs%d" % ab])
                    for fc in range(4):
                        hb_ = 2 * (fc % 2)
                        for k in range(8):
                            P.mm(lambda e, k=k, fc=fc, g=g, hb_=hb_, wg_=wg_: e.matmul(bank(hb_), lhsT=wg_[:, k, fc * 128:(fc + 1) * 128], rhs=hT[:, k, g * GS:(g + 1) * GS], start=(k == 0), stop=(k == 7)),
                                 reads=[nm + "g", "hT"], writes=["hg%d" % hb_])
                        for k in range(8):
                            P.mm(lambda e, k=k, fc=fc, g=g, hb_=hb_, wu_=wu_: e.matmul(bank(hb_ + 1), lhsT=wu_[:, k, fc * 128:(fc + 1) * 128], rhs=hT[:, k, g * GS:(g + 1) * GS], start=(k == 0), stop=(k == 7)),
                                 reads=[nm + "u", "hT"], writes=["hu%d" % hb_])
                        sbi = fc % 2
                        P.act(lambda e, hb_=hb_, sbi=sbi: e.activation(out=s_sb[sbi], in_=bank(hb_), func=AF.Silu), reads=["hg%d" % hb_], writes=["s%d" % sbi])
                        P.dve(lambda e, hb_=hb_, sbi=sbi: e.tensor_tensor(out=t_sb[sbi], in0=bank(hb_ + 1), in1=s_sb[sbi], op=ALU.mult), reads=["hu%d" % hb_, "s%d" % sbi], writes=["t%d" % sbi])
                        P.pool(lambda e, fc=fc, sbi=sbi, ab=ab: e.tensor_tensor(out=actb[ab][:, fc, :], in0=t_sb[sbi], in1=gbs[ab], op=ALU.mult), reads=["t%d" % sbi, "gbs%d" % ab], writes=["act%d" % ab])
                    if stage2[0] is not None:
                        emit_stage2(*stage2[0])
                    stage2[0] = (e_, g, ab)
                    if g == 0 and e_ + 1 < 16:
                        load_expert(e_ + 1)
            emit_stage2(*stage2[0])
            P.barrier()
        out_ops = []
        for g in range(4):
            P.act(lambda e, g=g: e.activation(out=sq[:], in_=x_sb[:, :, g * GS:(g + 1) * GS], func=AF.Square), reads=["x"], writes=["sq"])
            for k in range(8):
                P.mm(lambda e, k=k: e.matmul(bank(7), lhsT=ones[:], rhs=sq[:, k, :], start=(k == 0), stop=(k == 7)), reads=["sq", "ones"], writes=["ssps"])
            P.act(lambda e: e.activation(out=rstd, in_=bank(7), func=AF.Ln, scale=1.0 / 1024, bias=epsb[:]), reads=["ssps", "epsb"], writes=["rstd"])
            P.act(lambda e: e.activation(out=rstd, in_=rstd, func=AF.Exp, scale=-0.5), reads=["rstd"], writes=["rstd"])
            for k in range(8):
                hb = hn[k % 2]
                hbn = "hn%d" % (k % 2)
                P.dve(lambda e, k=k, hb=hb, g=g: e.tensor_tensor(out=hb, in0=x_sb[:, k, g * GS:(g + 1) * GS], in1=rstd, op=ALU.mult), reads=["x", "rstd"], writes=[hbn])
                P.act(lambda e, k=k, hb=hb, g=g: e.activation(out=x_sb[:, k, g * GS:(g + 1) * GS], in_=hb, func=AF.Copy, scale=fngs[:, k:k + 1]), reads=[hbn, "fngs"], writes=["xf"])
        out_ops.append(P.dma("sync", xo, x_sb[:], reads=["xf"]))
        P.emit(final_wait_ops=out_ops)
    return nc


from concourse.bass_utils import run_bass_kernel_spmd
import ml_dtypes

_BF = ml_dtypes.bfloat16
_CACHE = {}


def _fm(a):
    Tn = a.shape[0]
    return np.ascontiguousarray(a.T.reshape(8, 128, Tn).transpose(1, 0, 2))


def _pk(v):
    return np.ascontiguousarray(v.reshape(-1, 128).T)


def _pkl(v):
    return np.ascontiguousarray(np.stack([_pk(v[l]) for l in range(v.shape[0])], axis=1))


def _bc(v):
    return np.ascontiguousarray(np.broadcast_to(v, (128,) + v.shape)).astype(np.float32)


def _mkcst():
    p = np.arange(128)
    c = np.zeros((128, 8), np.float32)
    c[:, 0] = (10000.0 ** (-(2 * (p % 32)).astype(np.float32) / 64)).astype(np.float32)
    c[:, 1] = (10000.0 ** (-(2 * (p % 16)).astype(np.float32) / 32)).astype(np.float32)
    c[:, 2] = np.where((p % 64) < 32, -1.0, 1.0)
    c[:, 3] = np.where((p % 32) < 16, -1.0, 1.0)
    return c


def _mkmasks(r):
    k = np.arange(128)[:, None]
    q = np.arange(128)[None, :]
    own = {}
    oth = {}
    own['A'] = (k <= q)
    oth['A'] = (k < q) if r == 0 else (k <= q)
    own['Bp'] = (k >= q + 65)
    own['Bd'] = ((q - k) >= 0) & ((q - k) <= 63)
    oth['Bp'] = (k >= q + 64) if r == 0 else (k >= q + 65)
    oth['Bd'] = (((q - k) >= 1) & ((q - k) <= 64)) if r == 0 else (((q - k) >= 0) & ((q - k) <= 63))
    own['Cp'] = (k >= q + 64)
    own['Cd'] = ((q - k) >= 0) & ((q - k) <= 64)
    oth['Cp'] = (k >= q + 64) if r == 0 else (k >= q + 65)
    oth['Cd'] = (((q - k) >= 1) & ((q - k) <= 64)) if r == 0 else (((q - k) >= 0) & ((q - k) <= 63))

    def pm(a, key):
        return own[key] if a == r else oth[key]
    m = np.zeros((15, 128, 128), np.float32)
    m[0] = pm(0, 'A'); m[1] = pm(1, 'A')
    m[2] = pm(0, 'Bp'); m[3] = pm(0, 'Bd'); m[4] = pm(1, 'Bp'); m[5] = pm(1, 'Bd')
    m[6] = pm(0, 'Cp'); m[7] = pm(0, 'Cd'); m[8] = pm(1, 'Cp'); m[9] = pm(1, 'Cd')
    m[10] = (k >= q)
    m[11] = (k <= q)
    m[12] = m[10]
    m[13] = m[11]
    m[14] = m[11]
    return np.ascontiguousarray(m.transpose(1, 0, 2)).astype(_BF)


def _mkmasks8():
    k = np.arange(128)[:, None]
    q = np.arange(128)[None, :]
    dp = (k >= q).astype(np.float32)
    dd = (k <= q).astype(np.float32)
    m = np.zeros((4, 128, 512), np.float32)
    for half in range(2):
        sl = slice(64 * half, 64 * half + 64)
        m[half] = np.concatenate([dp[:, sl], dd[:, sl]] * 4, axis=1)
        m[2 + half] = np.concatenate([dd[:, sl]] * 8, axis=1)
    return np.ascontiguousarray(m.transpose(1, 0, 2)).astype(_BF)


def kernel(x, c, positions, ada_w, ada_b, norm_mix_g, norm_ffn_g, w_in, w_out,
           diff_lambda_q1, diff_lambda_k1, diff_lambda_q2, diff_lambda_k2, diff_subln_g,
           swa_sinks, router_group_w, router_group_b, router_expert_w, router_expert_b,
           expert_w_gate, expert_w_up, expert_w_down, final_norm_g):
    f32 = np.float32
    D = DEPTH
    A = lambda v: np.ascontiguousarray(np.asarray(v, f32))
    x = A(x)
    c = A(c)
    positions = np.asarray(positions)
    if "nc" not in _CACHE:
        _CACHE["nc"] = build_fused(D)
    sel = np.zeros((16, 16, 128), f32)
    for e in range(16):
        sel[e, e, :] = 1.0
    small = np.zeros((128, D, 16), f32)
    for l in range(D):
        small[:, l, 0] = 0.8 - 0.6 * math.exp(-0.3 * l)
        small[0:64, l, 1] = A(diff_subln_g)[l]
        small[:, l, 8:16] = A(swa_sinks)[l][None, :]
    lamv = _bc(np.stack([np.stack([A(diff_lambda_q1)[l], A(diff_lambda_k1)[l], A(diff_lambda_q2)[l], A(diff_lambda_k2)[l]]) for l in range(D)]))
    wrc = np.concatenate([A(router_group_w), A(router_expert_w)], axis=2)
    wr = np.ascontiguousarray(wrc.reshape(D, 8, 128, 20).transpose(2, 0, 1, 3))
    rb = _bc(np.concatenate([A(router_group_b), A(router_expert_b)], axis=1))
    common = dict(cst=_mkcst(), ada_w=A(ada_w), ada_b=_pkl(A(ada_b)), ngm=_pkl(A(norm_mix_g)), ngf=_pkl(A(norm_ffn_g)),
                  w_in=A(w_in), w_out=A(w_out), lamv=lamv, small=small, wr=wr, rbias=rb, ident=np.eye(128, dtype=f32), sel=sel,
                  wgate=A(expert_w_gate), wup=A(expert_w_up), wdown=A(expert_w_down), fng=_pk(A(final_norm_g)))
    in_maps = []
    for core in range(8):
        b, r = core // 2, core % 2
        m = dict(common)
        m.update(xT=_fm(x[b, r::2, :]), cT=_pk(c[b]),
                 posb=np.ascontiguousarray(np.broadcast_to(positions[b, r::2][None, :], (128, 2048))).astype(np.int32),
                 masks=_mkmasks(r), masks8=_mkmasks8())
        in_maps.append(m)
    res = run_bass_kernel_spmd(_CACHE["nc"], in_maps, core_ids=list(range(8))).results
    out = np.zeros((4, 4096, 1024), f32)
    for i in range(8):
        b, r = i // 2, i % 2
        out[b, r::2, :] = np.asarray(res[i]["xo"], f32).transpose(1, 0, 2).reshape(1024, 2048).T
    return out
```

```python
import math
import contextlib
import numpy as np
import concourse.bass as bass
import concourse.mybir as mybir

F32 = mybir.dt.float32
BF16 = mybir.dt.bfloat16
I32 = mybir.dt.int32
AF = mybir.ActivationFunctionType
ALU = mybir.AluOpType

ENGS = ("tensor", "vector", "scalar", "gpsimd", "sync")
NDMASEM = 12


class Prog:
    def __init__(self, nc):
        self.nc = nc
        self.ops = []
        self.last_writer = {}
        self.readers = {}

    def op(self, eng, fn, reads=(), writes=(), dma=False):
        idx = len(self.ops)
        deps = set()
        for r in reads:
            lw = self.last_writer.get(r)
            if lw is not None:
                deps.add(lw)
        for w in writes:
            lw = self.last_writer.get(w)
            if lw is not None:
                deps.add(lw)
            for rd in self.readers.get(w, ()):
                deps.add(rd)
        deps.discard(idx)
        for w in writes:
            self.last_writer[w] = idx
            self.readers[w] = []
        for r in reads:
            if r not in writes:
                self.readers.setdefault(r, []).append(idx)
        self.ops.append(dict(eng=eng, fn=fn, deps=deps, dma=dma, idx=idx, cc=False))
        return idx

    def cc(self, fn, reads=(), writes=()):
        idx = self.op("gpsimd", fn, reads, writes)
        self.ops[idx]["cc"] = True
        return idx

    def mm(self, fn, reads=(), writes=()):
        return self.op("tensor", fn, reads, writes)

    def dve(self, fn, reads=(), writes=()):
        return self.op("vector", fn, reads, writes)

    def act(self, fn, reads=(), writes=()):
        return self.op("scalar", fn, reads, writes)

    def pool(self, fn, reads=(), writes=()):
        return self.op("gpsimd", fn, reads, writes)

    def dma(self, eng, out, in_, reads=(), writes=()):
        return self.op(eng, lambda e: e.dma_start(out=out, in_=in_), reads, writes, dma=True)

    def emit(self, final_wait_ops=()):
        nc = self.nc
        ops = self.ops
        has_dep = [False] * len(ops)
        for o in ops:
            for d in o["deps"]:
                if ops[d]["eng"] == "tensor" and o["eng"] == "tensor" and not ops[d]["dma"]:
                    continue
                has_dep[d] = True
        for d in final_wait_ops:
            has_dep[d] = True
        eng_cnt = {e: 0 for e in ENGS}
        dma_cnt = {e: 0 for e in ENGS}
        dma_semval = {}
        for o in ops:
            e = o["eng"]
            if o["cc"]:
                o["sig"] = ("cc", o["idx"])
            elif o["dma"]:
                k = dma_cnt[e] % NDMASEM
                dma_cnt[e] += 1
                key = (e, k)
                dma_semval[key] = dma_semval.get(key, 0) + 16
                o["sig"] = ("dma", e, k, dma_semval[key])
            elif has_dep[o["idx"]]:
                eng_cnt[e] += 1
                o["sig"] = ("eng", e, eng_cnt[e])
            else:
                o["sig"] = None
        import contextlib
        with contextlib.ExitStack() as es:
            esem = {e: es.enter_context(nc.semaphore("s_" + e)) for e in ENGS}
            dsem = {}
            for e in ("sync", "gpsimd", "scalar"):
                for k in range(NDMASEM):
                    dsem[(e, k)] = es.enter_context(nc.semaphore("d_%s_%d" % (e, k)))
            ccsem = {o["idx"]: es.enter_context(nc.semaphore("cc_%d" % o["idx"])) for o in ops if o["cc"]}
            block = es.enter_context(nc.Block())

            def make(engname):
                def body(eng):
                    waited = {}
                    for o in ops:
                        if o["eng"] != engname:
                            continue
                        need = {}
                        for d in o["deps"]:
                            od = ops[d]
                            if od["eng"] == "tensor" and engname == "tensor" and not od["dma"]:
                                continue
                            s = od["sig"]
                            if s is None:
                                continue
                            if s[0] == "dma":
                                key = ("dma", s[1], s[2])
                                val = s[3]
                            elif s[0] == "cc":
                                key = ("cc", s[1])
                                val = 1
                            else:
                                key = ("eng", s[1])
                                val = s[2]
                            if need.get(key, 0) < val:
                                need[key] = val
                        if o["dma"]:
                            s = o["sig"]
                            if s[3] > 16:
                                key = ("dma", s[1], s[2])
                                if need.get(key, 0) < s[3] - 16:
                                    need[key] = s[3] - 16
                        for key, val in need.items():
                            if waited.get(key, 0) >= val:
                                continue
                            waited[key] = val
                            if key[0] == "dma":
                                sem = dsem[(key[1], key[2])]
                            elif key[0] == "cc":
                                sem = ccsem[key[1]]
                            else:
                                sem = esem[key[1]]
                            eng.wait_ge(sem, val)
                        ins = o["fn"](eng)
                        s = o["sig"]
                        if s is not None:
                            if s[0] == "dma":
                                ins.then_inc(dsem[(s[1], s[2])], 16)
                            elif s[0] == "cc":
                                ins.then_inc(ccsem[s[1]])
                            else:
                                ins.then_inc(esem[s[1]], 1)
                    if engname == "sync":
                        for d in final_wait_ops:
                            s = ops[d]["sig"]
                            if s[0] == "dma":
                                eng.wait_ge(dsem[(s[1], s[2])], s[3])
                            else:
                                eng.wait_ge(esem[s[1]], s[2])
                return body

            block.tensor(make("tensor"))
            block.vector(make("vector"))
            block.scalar(make("scalar"))
            block.gpsimd(make("gpsimd"))
            block.sync(make("sync"))


def _barrier(self):
    last = {}
    dmas = {}
    for o in self.ops:
        if o["dma"]:
            dmas.setdefault(o["eng"], []).append(o["idx"])
        elif o["cc"]:
            pass
        else:
            last[o["eng"]] = o["idx"]
    s = set(last.values())
    for e, lst in dmas.items():
        s.update(lst[-NDMASEM:])
    for o in self.ops:
        if o["cc"]:
            s.add(o["idx"])
    self.pending_barrier = s
    self.barrier_done = set()


_orig_op = Prog.op


def _op(self, eng, fn, reads=(), writes=(), dma=False):
    idx = _orig_op(self, eng, fn, reads, writes, dma)
    pb = getattr(self, "pending_barrier", None)
    if pb and eng not in self.barrier_done:
        self.ops[idx]["deps"] |= set(pb)
        self.barrier_done.add(eng)
    return idx


Prog.op = _op
Prog.barrier = _barrier


T = 2048
GS = 512
NM = 15
LOOK = 2
NST = 3
NPT = 4
NUB = 4
MKINDS = ['A0', 'A1', 'B0p', 'B0d', 'B1p', 'B1d', 'C0p', 'C0d', 'C1p', 'C1d', 'Dp', 'Dd', 'Dp', 'Dd', 'Dd']
BIG = 1.0e9
PI = math.pi
DEPTH = 4
X = mybir.AxisListType.X


def build_fused(depth=DEPTH):
    nc = bass.Bass("TRN2", target_bir_lowering=False)
    dt = nc.dram_tensor

    def inp(name, shape, dtype=F32):
        return dt(name, shape, dtype, kind="ExternalInput").ap()
    xT = inp("xT", [128, 8, T])
    cT = inp("cT", [128, 8])
    posb = inp("posb", [128, T], I32)
    cst = inp("cst", [128, 8])
    masks = inp("masks", [128, NM, 128], BF16)
    masks8 = inp("masks8", [128, 4, 512], BF16)
    ada_w = inp("ada_w", [depth, 1024, 6144])
    ada_b = inp("ada_b", [128, depth, 48])
    ngm = inp("ngm", [128, depth, 8])
    ngf = inp("ngf", [128, depth, 8])
    w_in = inp("w_in", [depth, 1024, 2304])
    w_out = inp("w_out", [depth, 1024, 1024])
    lamv = inp("lamv", [128, depth, 4, 32])
    small = inp("small", [128, depth, 16])
    wr = inp("wr", [128, depth, 8, 20])
    rbias = inp("rbias", [128, depth, 20])
    ident = inp("ident", [128, 128])
    sel = inp("sel", [16, 16, 128])
    wgate = inp("wgate", [depth, 16, 1024, 512])
    wup = inp("wup", [depth, 16, 1024, 512])
    wdown = inp("wdown", [depth, 16, 512, 1024])
    fng = inp("fng", [128, 8])
    xo = dt("xo", [128, 8, T], F32, kind="ExternalOutput").ap()
    tabD = dt("tabD", [128, 4, T], F32).ap()
    QTd = dt("QTd", [128, 8, T], BF16).ap()
    TH = T // 2
    KTd_h = [dt("KTd%d" % i, [128, 5 * TH], BF16) for i in range(2)]
    KTg_h = [dt("KTg%d" % i, [256, 5 * TH], BF16) for i in range(2)]
    Vd_h = [dt("Vd%d" % i, [T // 2, 650], BF16) for i in range(2)]
    Vg_h = [dt("Vg%d" % i, [T, 650], BF16) for i in range(2)]
    KTd3 = [h.ap().rearrange("p (c n) -> p c n", c=5) for i, h in enumerate(KTd_h)]
    KTg4 = [h.ap().rearrange("(a p) (c n) -> a p c n", a=2, c=5) for i, h in enumerate(KTg_h)]
    Vdh = [h.ap() for h in Vd_h]
    Vgh = [h.ap() for h in Vg_h]

    def ktd(c, hh):
        return KTd3[hh][:, c, :]

    def ktg(a, c, hh):
        return KTg4[hh][a][:, c, :]

    def vd_rows(n0, cnt, step=1):
        hh = n0 // 1024
        r = n0 % 1024
        return Vdh[hh][r:r + step * (cnt - 1) + 1:step, :]

    def vg_tile(a, t):
        hh = t // 8
        r = a * 1024 + (t % 8) * 128
        return Vgh[hh][r:r + 128, :]
    groups = [[0, 1], [2, 3], [4, 5], [6, 7]]

    P = Prog(nc)
    with contextlib.ExitStack() as es:
        def sb(name, shape, dtype):
            return es.enter_context(nc.sbuf_tensor(name, shape, dtype))
        x_sb = sb("x_sb", [128, 8, T], F32)
        ARN = 50496
        arena = sb("arena", [128, ARN], BF16)
        tar = sb("tar", [128, 9, 512], F32)
        off = [0]

        def carve(n):
            a = arena[:, off[0]:off[0] + n]
            off[0] += n
            assert off[0] <= ARN, off[0]
            return a
        off[0] = 0
        Wqk = carve(8 * 1664).rearrange("p (k n) -> p k n", k=8)
        Wsqk = carve(8 * 1664).rearrange("p (k n) -> p k n", k=8)
        Wkb = carve(2048).rearrange("p (k g n) -> p k g n", k=8, g=2)
        Wskb = carve(2048).rearrange("p (k g n) -> p k g n", k=8, g=2)
        Wv = carve(8 * 640).rearrange("p (k n) -> p k n", k=8)
        sqP = carve(4096).rearrange("p (k n) -> p k n", k=8)
        hTP = carve(4096).rearrange("p (k n) -> p k n", k=8)
        tab = carve(4096).bitcast(F32).rearrange("p (k n) -> p k n", k=4)
        qkst = [carve(512) for i in range(2)]
        vst = [carve(650) for i in range(2)]
        WadaP = [arena[:, i * 8192:(i + 1) * 8192].rearrange("p (k n) -> p k n", k=8) for i in range(2)]
        posi = arena[:, 16384:16384 + 1024].bitcast(I32)
        posf = arena[:, 17408:17408 + 1024].bitcast(F32)
        ang = arena[:, 18432:18432 + 1024].bitcast(F32)
        mi = arena[:, 19456:19456 + 1024].bitcast(I32)
        off[0] = 0
        Qreg = carve(4 * T)
        slotA = [Qreg[:, i * 4096:(i + 1) * 4096].rearrange("p (t n) -> p t n", t=2) for i in range(2)]
        slotH = [Qreg[:, i * 2048:(i + 1) * 2048] for i in range(2)]
        kown = Qreg[:, 4096:8192].rearrange("p (c n) -> p c n", c=2)
        k_sb = carve(4 * T).rearrange("p (a c n) -> p a c n", a=2, c=2)
        v_sb = carve(2 * 16 * 260).rearrange("p (a t c) -> p a t c", a=2, t=16)
        v2_sb = carve(16 * 260).rearrange("p (t c) -> p t c", t=16)
        v8_sb = carve(16 * 260).rearrange("p (t c) -> p t c", t=16)
        mix = carve(8 * T).rearrange("p (c n) -> p c n", c=8)
        V_OFF = 8 * T
        V2_OFF = V_OFF + 2 * 16 * 260
        V8_OFF = V2_OFF + 16 * 260

        def vwide(base, tile_idx, col):
            o = base + tile_idx * 260 + col
            return arena[:, o:o + 128]
        off[0] = 0
        hT = carve(8 * T).rearrange("p (c n) -> p c n", c=8)
        wbuf = []
        for i in range(2):
            wg_ = carve(8 * 512).rearrange("p (k f) -> p k f", k=8)
            wu_ = carve(8 * 512).rearrange("p (k f) -> p k f", k=8)
            wd_ = carve(4 * 1024).rearrange("p (c d) -> p c d", c=4)
            wbuf.append((wg_, wu_, wd_))
        actb = [carve(4 * 512).rearrange("p (c n) -> p c n", c=4) for i in range(2)]
        gT = carve(2 * T).bitcast(F32)
        w1base = 8 * T + 3 * 4096
        h32 = arena[:, w1base:w1base + 8192].bitcast(F32).rearrange("p (k n) -> p k n", k=8)
        sq = arena[:, w1base + 8192:w1base + 8192 + 4096].rearrange("p (k n) -> p k n", k=8)
        Wo = arena[:, 0:8192].rearrange("p (k n) -> p k n", k=8)
        rr = tar[:, 0, :]; bcs = tar[:, 1, :]; tmpA = [tar[:, 2 + i, :] for i in range(3)]
        rstd = tar[:, 0, :]; hn = [tar[:, 1 + i, :] for i in range(2)]
        gbs = [tar[:, 3 + i, :] for i in range(2)]; s_sb = [tar[:, 5 + i, :] for i in range(2)]; t_sb = [tar[:, 7 + i, :] for i in range(2)]
        t1 = [tar[:, 3 + i, :] for i in range(2)]; t2 = [tar[:, 5 + i, :] for i in range(2)]
        rrs = [tar[:, 0, :], tar[:, 7, :]]
        rrh = [tar[:, 5 + i, :].bitcast(BF16)[:, 0:512] for i in range(2)]
        rrl = [tar[:, 5 + i, :].bitcast(BF16)[:, 512:1024] for i in range(2)]
        sqb = tar[:, 8, :].bitcast(BF16)[:, 0:512]

        PT = [sb("PT%d" % i, [128, 512], BF16) for i in range(NPT)]
        msk = sb("msk", [128, NM, 128], BF16)
        msk8 = sb("msk8", [128, 4, 512], BF16)
        c_sb = sb("c_sb", [128, 8], F32)
        cact = sb("cact", [128, 8], BF16)
        adab = sb("adab", [128, depth, 48], F32)
        ngm_sb = sb("ngm_sb", [128, depth, 8], F32)
        ngf_sb = sb("ngf_sb", [128, depth, 8], F32)
        fngs = sb("fngs", [128, 8], F32)
        cs = sb("cs", [128, 8], F32)
        mod = sb("mod", [128, depth, 48], F32)
        gs1 = sb("gs1", [128, 8], F32)
        gs2 = sb("gs2", [128, 8], F32)
        ones = sb("ones", [128, 128], BF16)
        onesf = sb("onesf", [128, 128], F32)
        epsb = sb("epsb", [128, 1], F32)
        lam_sb = sb("lam_sb", [128, 4, 32], F32)
        lamt = sb("lamt", [128, 2, 32], F32)
        lams = sb("lams", [128, 8], F32)
        sm = sb("sm", [128, depth, 16], F32)
        sinke = sb("sinke", [128, 8], F32)
        wr_sb = sb("wr_sb", [128, depth, 8, 20], F32)
        rb_sb = sb("rb_sb", [128, depth, 20], F32)
        id_sb = sb("id_sb", [128, 128], F32)
        sel_sb = sb("sel_sb", [16, 16, 128], F32)
        L = tar[:, 4, 0:320].rearrange("p (a b) -> p a b", a=16)
        gates = tar[:, 5, 0:256].rearrange("p (a b) -> p a b", a=16)
        rt = tar[:, 3, 0:64]
        ps = es.enter_context(nc.psum_tensor("ps", [128, 8 * 512], F32))

        def bank(i):
            return ps[:, i * 512:(i + 1) * 512]

        P.dma("sync", x_sb[:], xT, writes=["x"])
        for (d_, s_, n_) in ((c_sb, cT, "c_sb"), (adab, ada_b, "adab"), (ngm_sb, ngm, "ngm"), (ngf_sb, ngf, "ngf"), (fngs, fng, "fngs"), (sm, small, "sm"),
                             (wr_sb, wr, "wr_sb"), (rb_sb, rbias, "rb_sb"), (id_sb, ident, "id_sb"), (sel_sb, sel, "sel_sb"), (msk, masks, "msk"), (msk8, masks8, "msk"), (cs, cst, "cs")):
            P.dma("sync", d_[:], s_, writes=[n_])
        P.pool(lambda e: e.memset(ones[:], 1.0), writes=["ones"])
        P.pool(lambda e: e.memset(onesf[:], 1.0), writes=["onesf"])
        P.pool(lambda e: e.memset(epsb[:], 1e-6), writes=["epsb"])
        P.act(lambda e: e.activation(out=cact[:], in_=c_sb[:], func=AF.Silu), reads=["c_sb"], writes=["cact"])
        for tg in range(4):
            tsl = slice(tg * GS, (tg + 1) * GS)
            P.dma("sync", posi, posb[:, tsl], writes=["posi"])
            P.dve(lambda e: e.tensor_copy(posf, posi), reads=["posi"], writes=["posf"])
            for ti, (fcol, scol) in enumerate(((0, 2), (1, 3))):
                P.dve(lambda e, fcol=fcol: e.tensor_scalar(out=ang, in0=posf, scalar1=cs[:, fcol:fcol + 1], scalar2=None, op0=ALU.mult), reads=["posf", "cs"], writes=["ang"])
                for which in range(2):
                    if which == 0:
                        P.dve(lambda e: e.tensor_scalar(out=t1[0], in0=ang, scalar1=0.5 * PI, scalar2=None, op0=ALU.add), reads=["ang"], writes=["t1_0"])
                    else:
                        P.dve(lambda e: e.tensor_copy(t1[0], ang), reads=["ang"], writes=["t1_0"])
                    P.dve(lambda e: e.tensor_scalar(out=t2[0], in0=t1[0], scalar1=1.0 / (2 * PI), scalar2=None, op0=ALU.mult), reads=["t1_0"], writes=["t2_0"])
                    P.dve(lambda e: e.tensor_copy(mi, t2[0]), reads=["t2_0"], writes=["mi"])
                    P.dve(lambda e: e.tensor_copy(t2[0], mi), reads=["mi"], writes=["t2_0"])
                    P.dve(lambda e: e.scalar_tensor_tensor(out=t1[0], in0=t2[0], scalar=-2 * PI, in1=t1[0], op0=ALU.mult, op1=ALU.add), reads=["t2_0", "t1_0"], writes=["t1_0"])
                    P.dve(lambda e: e.tensor_scalar(out=t2[0], in0=t1[0], scalar1=PI, scalar2=2 * PI, op0=ALU.is_gt, op1=ALU.mult), reads=["t1_0"], writes=["t2_0"])
                    P.dve(lambda e: e.tensor_tensor(out=t1[0], in0=t1[0], in1=t2[0], op=ALU.subtract), reads=["t1_0", "t2_0"], writes=["t1_0"])
                    P.dve(lambda e: e.tensor_scalar(out=t1[0], in0=t1[0], scalar1=PI, scalar2=-PI, op0=ALU.min, op1=ALU.max), reads=["t1_0"], writes=["t1_0"])
                    if which == 0:
                        P.act(lambda e, ti=ti: e.activation(out=tab[:, 2 * ti, :], in_=t1[0], func=AF.Sin), reads=["t1_0"], writes=["tab"])
                    else:
                        P.act(lambda e, ti=ti, scol=scol: e.activation(out=tab[:, 2 * ti + 1, :], in_=t1[0], func=AF.Sin, scale=cs[:, scol:scol + 1]), reads=["t1_0", "cs"], writes=["tab"])
            P.dma("sync", tabD[:, :, tsl], tab, reads=["tab"], writes=["tabD"])
        cnt = 0
        for l in range(depth):
            ada_v = ada_w[l].rearrange("(k p) n -> p k n", p=128)
            for which in range(6):
                Wa = WadaP[cnt % 2]
                wn = "Wada%d" % (cnt % 2)
                cnt += 1
                P.dma("gpsimd", Wa, ada_v[:, :, which * 1024:(which + 1) * 1024], writes=[wn])
                for j in range(8):
                    for k in range(8):
                        P.mm(lambda e, j=j, k=k, which=which, Wa=Wa: e.matmul(bank(0)[:, which * 8 + j: which * 8 + j + 1], lhsT=Wa[:, k, j * 128:(j + 1) * 128], rhs=cact[:, k:k + 1], start=(k == 0), stop=(k == 7)),
                             reads=[wn, "cact"], writes=["modps"])
            P.dve(lambda e, l=l: e.tensor_tensor(out=mod[:, l, :], in0=bank(0)[:, 0:48], in1=adab[:, l, :], op=ALU.add), reads=["modps", "adab"], writes=["mod"])
        P.barrier()

        st = dict(i=0, pend=[], mi=0, ubank_started=set())

        def run_batch(units, scale, rowgrp, m8=None):
            b = st["i"] % NST
            pi = st["i"] % NPT
            st["i"] += 1
            ptn = "PT%d" % pi
            stn = "ST%d" % b
            o = 0
            for u in units:
                u["off"] = o
                P.mm(lambda e, u=u, o=o, b=b: e.matmul(bank(b)[:, o:o + u["n"]], lhsT=u["kt"], rhs=u["q"], start=True, stop=True),
                     reads=u["kres"] + u["qres"], writes=[stn])
                o += u["n"]
            tot = o
            ptu = ["%s_%d" % (ptn, i) for i in range(8)]
            P.act(lambda e, b=b, pi=pi, tot=tot: e.activation(out=PT[pi][:, 0:tot], in_=bank(b)[:, 0:tot], func=AF.Exp, scale=scale), reads=[stn], writes=ptu)
            merged = False
            if m8 is not None:
                P.dve(lambda e, pi=pi, tot=tot: e.tensor_tensor(out=PT[pi][:, 0:tot], in0=PT[pi][:, 0:tot], in1=msk8[:, m8, 0:tot], op=ALU.mult), reads=ptu + ["msk"], writes=ptu)
                merged = True
            elif len(units) > 1 and all(u["mask"] is not None and u["mask"][1] == 128 and u["n"] == 128 for u in units):
                kinds = [MKINDS[u["mask"][2]] for u in units]
                for s0 in range(NM - len(units) + 1):
                    if MKINDS[s0:s0 + len(units)] == kinds:
                        nn = len(units)
                        P.dve(lambda e, pi=pi, s0=s0, nn=nn: e.tensor_tensor(out=PT[pi][:, 0:nn * 128], in0=PT[pi][:, 0:nn * 128], in1=msk[:, s0:s0 + nn, :].rearrange("p a b -> p (a b)"), op=ALU.mult),
                              reads=ptu + ["msk"], writes=ptu)
                        merged = True
                        break
            if not merged:
                for ui, u in enumerate(units):
                    if u["mask"] is not None:
                        if u["mask"][1] >= 128:
                            eng = "vector"
                        else:
                            eng = "gpsimd" if st["mi"] % 2 == 0 else "vector"
                            st["mi"] += 1
                        o = u["off"]
                        m = u["mask"]
                        P.op(eng, lambda e, pi=pi, o=o, m=m: e.tensor_tensor(out=PT[pi][:, o:o + m[1]], in0=PT[pi][:, o:o + m[1]], in1=m[0], op=ALU.mult), reads=[ptu[ui], "msk"], writes=[ptu[ui]])
            st["pend"].append((units, pi))
            while len(st["pend"]) > LOOK:
                emit_pv(*st["pend"].pop(0))
            for it_ in fin_q:
                it_[1] -= 1
            while fin_q and fin_q[0][1] <= 0:
                fin_q.pop(0)[0]()

        def emit_pv(units, b):
            ptn = "PT%d" % b
            for ui, u in enumerate(units):
                first = u["ubank"] not in st["ubank_started"]
                st["ubank_started"].add(u["ubank"])
                o = u["off"]
                P.mm(lambda e, u=u, o=o, b=b, first=first: e.matmul(u["u"], lhsT=u["v"], rhs=PT[b][:, o:o + u["n"]], start=first, stop=False, skip_group_check=True),
                     reads=["%s_%d" % (ptn, ui)] + u["vres"], writes=[u["ures"]])

        def flush():
            while st["pend"]:
                emit_pv(*st["pend"].pop(0))
        fin_q = []

        def schedule_fin(stage1, stage2):
            fin_q.append([stage1, LOOK + 1])
            fin_q.append([stage2, LOOK + 4])
            fin_q.sort(key=lambda t: t[1])

        def drain_fins():
            flush()
            while fin_q:
                fin_q.pop(0)[0]()
        ucount = [0]

        def new_ubank():
            ub = NST + (ucount[0] % NUB)
            ucount[0] += 1
            st["ubank_started"].discard(ub)
            return ub

        def rr_chain(ub, ri, sink_col):
            ures = "U%d" % ub
            rr_ = rrs[ri]
            rn = "rr%d" % ri
            if sink_col is not None:
                P.act(lambda e: e.activation(out=rr_[64:65, :], in_=bank(ub)[64:65, :], func=AF.Ln, bias=sinke[64:65, sink_col:sink_col + 1]), reads=[ures, "sinke"], writes=[rn])
            else:
                P.act(lambda e: e.activation(out=rr_[64:65, :], in_=bank(ub)[64:65, :], func=AF.Ln), reads=[ures], writes=[rn])
            P.act(lambda e: e.activation(out=rr_[64:65, :], in_=rr_[64:65, :], func=AF.Exp, scale=-1.0), reads=[rn], writes=[rn])

        def bcast(ri):
            rn = "rr%d" % ri
            rr_ = rrs[ri]
            P.mm(lambda e: e.matmul(bank(7)[0:64, :], lhsT=onesf[64:65, 0:64], rhs=rr_[64:65, :], start=True, stop=True), reads=[rn, "onesf"], writes=["BC"])
            P.act(lambda e: e.activation(out=bcs[0:64, :], in_=bank(7)[0:64, :], func=AF.Copy), reads=["BC"], writes=["bcs"])

        def finalize_simple(ub, head_feat, g, sink_col):
            ures = "U%d" % ub
            ri = ucount[0] % 2

            def stage1():
                rr_chain(ub, ri, sink_col)

            def stage2():
                bcast(ri)
                ch, po = head_feat // 128, head_feat % 128
                if po == 0:
                    P.dve(lambda e: e.tensor_tensor(out=mix[0:64, ch, g * GS:(g + 1) * GS], in0=bank(ub)[0:64, :], in1=bcs[0:64, :], op=ALU.mult), reads=[ures, "bcs"], writes=["mix"])
                else:
                    P.dve(lambda e: e.tensor_tensor(out=tmpA[0][0:64, :], in0=bank(ub)[0:64, :], in1=bcs[0:64, :], op=ALU.mult), reads=[ures, "bcs"], writes=["tmpA0"])
                    P.act(lambda e: e.activation(out=mix[po:po + 64, ch, g * GS:(g + 1) * GS], in_=tmpA[0][0:64, :], func=AF.Copy), reads=["tmpA0"], writes=["mix"])
            schedule_fin(stage1, stage2)

        def load_kv(c0, nc_, vc0, vw):
            for hh in range(2):
                for a in range(2):
                    P.dma("sync", k_sb[:, a, 0:nc_, hh * TH:(hh + 1) * TH], KTg4[hh][a][:, c0:c0 + nc_, :], reads=["KTg%d" % hh], writes=["k_sb%d" % hh])
                    P.dma("sync", v_sb[:, a, 8 * hh:8 * hh + 8, 0:vw], Vgh[hh][a * TH:(a + 1) * TH, vc0:vc0 + vw].rearrange("(t p) w -> p t w", p=128),
                          reads=["Vg%d" % hh], writes=["v_sb%d" % hh])
        vq = [0]

        def load_v1(dst, src_rows_ap, nheads, resname, extra_reads):
            vq[0] += 1
            P.dma("sync", dst.rearrange("p (h c) -> p h c", c=65)[:, :, 0:64], src_rows_ap.rearrange("p (h c) -> p h c", c=64), reads=extra_reads, writes=[resname])

        chunks = []
        chunks += [("w", 0, 32, ("q", 0)), ("w", 128, 32, ("q", 1)), ("w", 256, 32, ("k", 0)), ("w", 384, 32, ("k", 1))]
        chunks += [("w", 512 + 128 * i, 64, ("q", 2 + i)) for i in range(4)]
        chunks += [("w", 1024, 64, ("k", 2))]
        chunks += [("w", 1152 + 128 * i, 64, ("q", 6 + i)) for i in range(2)]
        chunks += [("w", 1408 + 128 * i, 64, ("k", 3 + i)) for i in range(2)]

        for l in range(depth):
            last = (l == depth - 1)
            lam_init_col = sm[:, l, 0:1]
            P.dve(lambda e, l=l: e.scalar_tensor_tensor(out=gs1[:], in0=mod[:, l, 8:16], scalar=1.0, in1=ngm_sb[:, l, :], op0=ALU.add, op1=ALU.mult), reads=["mod", "ngm"], writes=["gs1"])
            P.dve(lambda e, l=l: e.scalar_tensor_tensor(out=gs2[:], in0=mod[:, l, 32:40], scalar=1.0, in1=ngf_sb[:, l, :], op0=ALU.add, op1=ALU.mult), reads=["mod", "ngf"], writes=["gs2"])
            P.dma("sync", lam_sb[:], lamv[:, l, :, :], writes=["lam_sb"])
            for i in range(2):
                P.dve(lambda e, i=i, l=l: e.tensor_tensor(out=lamt[:, i, :], in0=lam_sb[:, 2 * i, :], in1=lam_sb[:, 2 * i + 1, :], op=ALU.mult), reads=["lam_sb"], writes=["lamt"])
                P.dve(lambda e, i=i: e.reduce_sum(out=lams[:, i:i + 1], in_=lamt[:, i, :], axis=X), reads=["lamt"], writes=["lams"])
            P.act(lambda e: e.activation(out=lams[:, 2:4], in_=lams[:, 0:2], func=AF.Exp), reads=["lams"], writes=["lams"])
            P.dve(lambda e: e.tensor_tensor(out=lams[:, 4:5], in0=lams[:, 3:4], in1=lams[:, 2:3], op=ALU.subtract), reads=["lams"], writes=["lams"])
            P.dve(lambda e, l=l: e.tensor_tensor(out=lams[:, 4:5], in0=lams[:, 4:5], in1=sm[:, l, 0:1], op=ALU.subtract), reads=["lams", "sm"], writes=["lams"])
            P.dve(lambda e, l=l: e.tensor_scalar(out=lams[:, 5:6], in0=sm[:, l, 0:1], scalar1=-1.0, scalar2=1.0, op0=ALU.mult, op1=ALU.add), reads=["sm"], writes=["lams"])
            P.dve(lambda e, l=l: e.tensor_tensor(out=lams[:, 6:7], in0=lams[:, 5:6], in1=sm[:, l, 1:2], op=ALU.mult), reads=["lams", "sm"], writes=["lams"])
            P.act(lambda e, l=l: e.activation(out=sinke[:], in_=sm[:, l, 8:16], func=AF.Exp), reads=["sm"], writes=["sinke"])

            w_in_v = w_in[l].rearrange("(k p) n -> p k n", p=128)
            for k in range(8):
                for (d0, s0, n) in ((0, 0, 512), (512, 768, 640), (1152, 1536, 512)):
                    P.dma("gpsimd", Wqk[:, k, d0:d0 + n], w_in_v[:, k, s0:s0 + n], writes=["W%d" % k])
                for (d0, s0, n) in ((0, 512, 256), (256, 1408, 128), (384, 2048, 256)):
                    P.dma("gpsimd", Wv[:, k, d0:d0 + n], w_in_v[:, k, s0:s0 + n], writes=["Wv%d" % k])

            def swapcopy(k, c0, nheads, dh):
                h = dh // 2
                d = Wsqk[:, k, c0:c0 + nheads * dh].rearrange("p (a t c) -> p a t c", t=2, c=h)
                s = Wqk[:, k, c0:c0 + nheads * dh].rearrange("p (a t c) -> p a t c", t=2, c=h)
                P.act(lambda e: e.activation(out=d[:, :, 0, :], in_=s[:, :, 1, :], func=AF.Copy), reads=["W%d" % k], writes=["Ws%d" % k])
                P.act(lambda e: e.activation(out=d[:, :, 1, :], in_=s[:, :, 0, :], func=AF.Copy), reads=["W%d" % k], writes=["Ws%d" % k])
            for k in range(8):
                swapcopy(k, 0, 16, 32)
                swapcopy(k, 512, 10, 64)
                swapcopy(k, 1152, 8, 64)
            def exchange(hh):
                P.cc(lambda e: e.collective_compute("AllGather", ALU.bypass, replica_groups=groups, ins=[KTd_h[hh].ap().opt()], outs=[KTg_h[hh].ap().opt()]), reads=["KTd%d" % hh], writes=["KTg%d" % hh])
                P.cc(lambda e: e.collective_compute("AllGather", ALU.bypass, replica_groups=groups, ins=[Vd_h[hh].ap().opt()], outs=[Vg_h[hh].ap().opt()]), reads=["Vd%d" % hh], writes=["Vg%d" % hh])
            for i in range(2):
                P.pool(lambda e, i=i: e.memset(vst[i], 1.0), writes=["vst%d" % i])
            qi = 0
            for tg in range(4):
                tsl = slice(tg * GS, (tg + 1) * GS)
                P.dma("sync", tab, tabD[:, :, tsl], reads=["tabD"], writes=["tab"])
                P.act(lambda e, tg=tg: e.activation(out=sqP[:], in_=x_sb[:, :, tg * GS:(tg + 1) * GS], func=AF.Square), reads=["x"], writes=["sqP"])
                for k in range(8):
                    P.mm(lambda e, k=k: e.matmul(bank(7), lhsT=ones[:], rhs=sqP[:, k, :], start=(k == 0), stop=(k == 7)), reads=["sqP", "ones"], writes=["ssps"])
                P.act(lambda e: e.activation(out=rstd, in_=bank(7), func=AF.Ln, scale=1.0 / 1024, bias=epsb[:]), reads=["ssps", "epsb"], writes=["rstd"])
                P.act(lambda e: e.activation(out=bank(7), in_=rstd, func=AF.Exp, scale=-0.5), reads=["rstd"], writes=["ssps"])
                for k in range(8):
                    hb = hn[k % 2]
                    hbn = "hn%d" % (k % 2)
                    P.dve(lambda e, k=k, hb=hb, tg=tg: e.tensor_tensor(out=hb, in0=x_sb[:, k, tg * GS:(tg + 1) * GS], in1=bank(7), op=ALU.mult), reads=["x", "ssps"], writes=[hbn])
                    P.act(lambda e, k=k, hb=hb, l=l: e.activation(out=hTP[:, k, :], in_=hb, func=AF.Identity, scale=gs1[:, k:k + 1], bias=mod[:, l, k:k + 1]), reads=[hbn, "gs1", "mod"], writes=["hTP%d" % k])
                for ci, (kind, src, tt, dest) in enumerate(chunks):
                    b1 = 1 + 2 * (ci % 2)
                    b2 = b1 + 1
                    ti = 0 if tt == 64 else 1
                    for k in range(8):
                        if kind == "w":
                            l1 = Wqk[:, k, src:src + 128]; r1 = "W%d" % k
                        else:
                            l1 = Wkb[:, k, src, :]; r1 = "Wkb%d" % k
                        P.mm(lambda e, k=k, l1=l1, b1=b1: e.matmul(bank(b1), lhsT=l1, rhs=hTP[:, k, :], start=(k == 0), stop=(k == 7)), reads=[r1, "hTP%d" % k], writes=["bank%d" % b1])
                    for k in range(8):
                        if kind == "w":
                            l2 = Wsqk[:, k, src:src + 128]; r2 = "Ws%d" % k
                        else:
                            l2 = Wskb[:, k, src, :]; r2 = "Wskb%d" % k
                        P.mm(lambda e, k=k, l2=l2, b2=b2: e.matmul(bank(b2), lhsT=l2, rhs=hTP[:, k, :], start=(k == 0), stop=(k == 7)), reads=[r2, "hTP%d" % k], writes=["bank%d" % b2])
                    a = t1[ci % 2]; an = "t1_%d" % (ci % 2)
                    bb = t2[ci % 2]; bn = "t2_%d" % (ci % 2)
                    P.dve(lambda e, a=a, b1=b1, ti=ti: e.tensor_tensor(out=a, in0=bank(b1), in1=tab[:, 2 * ti, :], op=ALU.mult), reads=["bank%d" % b1, "tab"], writes=[an])
                    P.dve(lambda e, bb=bb, b2=b2, ti=ti: e.tensor_tensor(out=bb, in0=bank(b2), in1=tab[:, 2 * ti + 1, :], op=ALU.mult), reads=["bank%d" % b2, "tab"], writes=[bn])
                    so = qkst[qi % 2]; son = "qkst%d" % (qi % 2)
                    qi += 1
                    P.pool(lambda e, a=a, bb=bb, so=so: e.tensor_tensor(out=so, in0=a, in1=bb, op=ALU.add), reads=[an, bn], writes=[son])
                    if dest[0] == "q":
                        P.dma("sync", QTd[:, dest[1], tsl], so, reads=[son], writes=["QTd"])
                    else:
                        P.dma("sync", ktd(dest[1], tg // 2)[:, (tg % 2) * GS:(tg % 2 + 1) * GS], so, reads=[son], writes=["KTd%d" % (tg // 2)])
                for tt4 in range(4):
                    vb = vst[tt4 % 2]; vn = "vst%d" % (tt4 % 2)
                    for k in range(8):
                        P.mm(lambda e, k=k, tt4=tt4: e.matmul(bank(5), lhsT=hTP[:, k, tt4 * 128:(tt4 + 1) * 128], rhs=Wv[:, k, 0:512], start=(k == 0), stop=(k == 7)), reads=["Wv%d" % k, "hTP%d" % k], writes=["bank5"])
                    for k in range(8):
                        P.mm(lambda e, k=k, tt4=tt4: e.matmul(bank(6)[:, 0:128], lhsT=hTP[:, k, tt4 * 128:(tt4 + 1) * 128], rhs=Wv[:, k, 512:640], start=(k == 0), stop=(k == 7)), reads=["Wv%d" % k, "hTP%d" % k], writes=["bank6"])
                    for (d0, nh, src) in ((0, 4, bank(5)[:, 0:256]), (260, 2, bank(5)[:, 256:384]), (390, 2, bank(5)[:, 384:512]), (520, 2, bank(6)[:, 0:128])):
                        P.act(lambda e, vb=vb, d0=d0, nh=nh, src=src: e.activation(out=vb[:, d0:d0 + 65 * nh].rearrange("p (h c) -> p h c", c=65)[:, :, 0:64], in_=src.rearrange("p (h c) -> p h c", c=64), func=AF.Copy),
                              reads=["bank5", "bank6"], writes=[vn])
                    r0 = tg * GS + tt4 * 128
                    P.dma("sync", vd_rows(r0, 128), vb, reads=[vn], writes=["Vd%d" % (tg // 2)])
                if tg == 1:
                    exchange(0)
            P.barrier()
            exchange(1)

            load_kv(0, 2, 0, 260)
            for cp in range(2):
                for hh in range(2):
                    P.dma("sync", v2_sb[:, cp * 8 + 4 * hh:cp * 8 + 4 * hh + 4, :], Vdh[hh][cp:cp + 1023:2, 390:650].rearrange("(t p) w -> p t w", p=128), reads=["Vd%d" % hh], writes=["v2_sb"])
            for bt in range(2):
                P.dma("sync", v8_sb[:, bt:16:2, :], Vdh[bt][:, 390:650].rearrange("(p c) w -> p c w", c=8), reads=["Vd%d" % bt], writes=["v8_sb"])
            hcnt = [0]

            def prepA(h):
                si = hcnt[0] % 2
                hcnt[0] += 1
                sl = slotA[si]
                P.pool(lambda e: e.memset(sl, 0.0), writes=["qs%d" % si])
                for tau in range(2):
                    P.dma("sync", sl[32 * h:32 * h + 32, tau, :], QTd[32 * h:32 * h + 32, tau, :], reads=["QTd"], writes=["qs%d" % si])
                return sl, "qs%d" % si
            slotsA_ = {0: prepA(0)}
            scaleA = 32 ** -0.5
            for h in range(4):
                if h + 1 < 4:
                    slotsA_[h + 1] = prepA(h + 1)
                qsl, qsn = slotsA_[h]
                for g in range(4):
                    ubs = []
                    for tau in range(2):
                        ub = new_ubank()
                        ubs.append(ub)
                        for jp in range(4 * g + 4):
                            for a in range(2):
                                if jp < 4 * g:
                                    q0, n, m = g * GS, GS, None
                                else:
                                    q0 = jp * 128
                                    n = (g + 1) * GS - q0
                                    m = (msk[:, a, :], 128, a)
                                u = dict(kt=k_sb[:, a, tau, jp * 128:(jp + 1) * 128], q=qsl[:, tau, q0:q0 + n], n=n, mask=m,
                                         v=vwide(V_OFF, a * 16 + jp, 65 * h), u=bank(ub)[:, q0 - g * GS:q0 - g * GS + n], ures="U%d" % ub, ubank=ub,
                                         kres=["k_sb%d" % (jp // 8)], qres=[qsn], vres=["v_sb%d" % (jp // 8)])
                                run_batch([u], scaleA, 0)
                    def mkfin(ubs=ubs, h=h, g=g):
                        def stage1():
                            for tau in range(2):
                                rr_chain(ubs[tau], tau, None)

                        def stage2():
                            for tau in range(2):
                                ub = ubs[tau]
                                bcast(tau)
                                P.dve(lambda e, ub=ub, tau=tau: e.tensor_tensor(out=tmpA[tau][0:64, :], in0=bank(ub)[0:64, :], in1=bcs[0:64, :], op=ALU.mult), reads=["U%d" % ub, "bcs"], writes=["tmpA%d" % tau])
                            P.dve(lambda e: e.scalar_tensor_tensor(out=tmpA[0][0:64, :], in0=tmpA[1][0:64, :], scalar=lams[0:64, 4:5], in1=tmpA[0][0:64, :], op0=ALU.mult, op1=ALU.add), reads=["tmpA0", "tmpA1", "lams"], writes=["tmpA0"])
                            P.act(lambda e: e.activation(out=sqb[0:64, :], in_=tmpA[0][0:64, :], func=AF.Square), reads=["tmpA0"], writes=["sqb"])
                            P.mm(lambda e: e.matmul(bank(7)[0:64, :], lhsT=ones[0:64, 0:64], rhs=sqb[0:64, :], start=True, stop=True), reads=["sqb", "ones"], writes=["BC"])
                            P.act(lambda e: e.activation(out=bcs[0:64, :], in_=bank(7)[0:64, :], func=AF.Ln, scale=1.0 / 64, bias=epsb[0:64, :]), reads=["BC", "epsb"], writes=["bcs"])
                            P.act(lambda e: e.activation(out=bcs[0:64, :], in_=bcs[0:64, :], func=AF.Exp, scale=-0.5), reads=["bcs"], writes=["bcs"])
                            P.dve(lambda e: e.tensor_tensor(out=tmpA[0][0:64, :], in0=tmpA[0][0:64, :], in1=bcs[0:64, :], op=ALU.mult), reads=["tmpA0", "bcs"], writes=["tmpA0"])
                            ch, po = (64 * h) // 128, (64 * h) % 128
                            P.act(lambda e: e.activation(out=mix[po:po + 64, ch, g * GS:(g + 1) * GS], in_=tmpA[0][0:64, :], func=AF.Copy, scale=lams[0:64, 6:7]), reads=["tmpA0", "lams"], writes=["mix"])
                        return stage1, stage2
                    schedule_fin(*mkfin())
            drain_fins()

            for ci in range(2):
                for hh in range(2):
                    P.dma("sync", kown[:, ci, hh * TH:(hh + 1) * TH], ktd(3 + ci, hh), reads=["KTd%d" % hh], writes=["kown", "qs1"])
            load_kv(2, 1, 260, 130)

            def prepH(dst_row, src_chunk, src_row):
                si = hcnt[0] % 2
                hcnt[0] += 1
                sl = slotH[si]
                P.pool(lambda e: e.memset(sl, 0.0), writes=["qs%d" % si])
                P.dma("sync", sl[dst_row:dst_row + 64, :], QTd[src_row:src_row + 64, src_chunk, :], reads=["QTd"], writes=["qs%d" % si])
                return sl, "qs%d" % si
            slotsB_ = {0: prepH(0, 2, 0)}
            scaleB = 64 ** -0.5
            for h in range(8):
                kv = h // 4
                if h + 1 < 8:
                    slotsB_[h + 1] = prepH(64 * ((h + 1) // 4), 2 + (h + 1) // 2, 64 * ((h + 1) % 2))
                qsl, qsn = slotsB_[h]
                for g in range(4):
                    ub = new_ubank()
                    for j in range(4 * g, 4 * g + 4):
                        units = []
                        for (a, jj, mid) in ((0, j - 1, 2), (0, j, 3), (1, j - 1, 4), (1, j, 5)):
                            if jj < 0:
                                continue
                            units.append(dict(kt=k_sb[:, a, 0, jj * 128:(jj + 1) * 128], q=qsl[:, j * 128:(j + 1) * 128], n=128, mask=(msk[:, mid, :], 128, mid),
                                              v=vwide(V_OFF, a * 16 + jj, 65 * kv), u=bank(ub)[:, (j - 4 * g) * 128:(j - 4 * g + 1) * 128], ures="U%d" % ub, ubank=ub,
                                              kres=["k_sb%d" % (jj // 8)], qres=[qsn], vres=["v_sb%d" % (jj // 8)]))
                        run_batch(units, scaleB, 0)
                    finalize_simple(ub, 256 + 64 * h, g, h)
            drain_fins()

            load_kv(3, 2, 390, 260)
            slotsC_ = {0: prepH(0, 6, 0)}
            for h in range(4):
                rg = 64 * (h % 2)
                qc = h // 2
                if h + 1 < 4:
                    slotsC_[h + 1] = prepH(64 * ((h + 1) % 2), 6 + (h + 1) // 2, 64 * ((h + 1) % 2))
                qsl, qsn = slotsC_[h]
                for g in range(4):
                    ub = new_ubank()
                    for j in range(4 * g, 4 * g + 4):
                        units = []
                        for (a, jj, mid) in ((0, j - 1, 6), (0, j, 7), (1, j - 1, 8), (1, j, 9)):
                            if jj < 0:
                                continue
                            units.append(dict(kt=k_sb[:, a, qc, jj * 128:(jj + 1) * 128], q=qsl[:, j * 128:(j + 1) * 128], n=128, mask=(msk[:, mid, :], 128, mid),
                                              v=vwide(V_OFF, a * 16 + jj, 65 * h), u=bank(ub)[:, (j - 4 * g) * 128:(j - 4 * g + 1) * 128], ures="U%d" % ub, ubank=ub,
                                              kres=["k_sb%d" % (jj // 8)], qres=[qsn], vres=["v_sb%d" % (jj // 8)]))
                        run_batch(units, scaleB, 0)
                    units = []
                    for cp in range(2):
                        for bt in (2 * g, 2 * g + 1):
                            qn0 = cp + 256 * bt
                            for (bb_, mid) in ((bt - 1, 10), (bt, 11)):
                                if bb_ < 0:
                                    continue
                                kn0 = cp + 256 * bb_
                                units.append(dict(kt=kown[:, qc, kn0:kn0 + 255:2], q=qsl[:, qn0:qn0 + 255:2], n=128, mask=(msk[:, mid, :], 128, mid),
                                                  v=vwide(V2_OFF, cp * 8 + bb_, 65 * h), u=bank(ub)[:, qn0 - g * GS:qn0 - g * GS + 255:2], ures="U%d" % ub, ubank=ub,
                                                  kres=["kown"], qres=[qsn], vres=["v2_sb"], pair=(bt > 0)))
                    units.sort(key=lambda u: 0 if u["pair"] else 1)
                    for i in range(0, len(units), 4):
                        run_batch(units[i:i + 4], scaleB, 0)
                    units = []
                    bt = g // 2
                    half = g % 2
                    for cp in range(8):
                        qn0 = cp + 1024 * bt + 8 * 64 * half
                        for (bb_, mid) in ((bt - 1, 10), (bt, 11)):
                            if bb_ < 0:
                                continue
                            kn0 = cp + 1024 * bb_
                            units.append(dict(kt=kown[:, qc, kn0:kn0 + 1017:8], q=qsl[:, qn0:qn0 + 505:8], n=64, mask=(msk[:, mid, 64 * half:64 * half + 64], 64, mid),
                                              v=vwide(V8_OFF, cp * 2 + bb_, 65 * h), u=bank(ub)[:, qn0 - g * GS:qn0 - g * GS + 505:8], ures="U%d" % ub, ubank=ub,
                                              kres=["kown"], qres=[qsn], vres=["v8_sb"]))
                    for i in range(0, len(units), 8):
                        run_batch(units[i:i + 8], scaleB, 0, m8=(half if bt > 0 else 2 + half))
                    finalize_simple(ub, 768 + 64 * h, g, None)
            drain_fins()
            P.barrier()
            def load_expert(e_, l=l):
                wg_, wu_, wd_ = wbuf[e_ % 2]
                nm = "w%d" % (e_ % 2)
                extra = (["h32_%d" % k for k in range(8)] + ["sq"]) if e_ == 1 else []
                P.dma("gpsimd", wg_, wgate[l, e_].rearrange("(k p) f -> p k f", p=128), writes=[nm + "g"] + extra)
                P.dma("gpsimd", wu_, wup[l, e_].rearrange("(k p) f -> p k f", p=128), writes=[nm + "u"] + extra)
                P.dma("gpsimd", wd_, wdown[l, e_].rearrange("(c p) d -> p c d", p=128), writes=[nm + "d"] + extra)
            load_expert(0)
            wo_v = w_out[l].rearrange("(k p) n -> p k n", p=128)
            for k in range(8):
                P.dma("gpsimd", Wo[:, k, :], wo_v[:, k, :], writes=["Wo%d" % k])
            cnt = 0
            for g in range(4):
                for oc in range(8):
                    b = cnt % 2
                    cnt += 1
                    for k in range(8):
                        P.mm(lambda e, k=k, oc=oc, g=g, b=b: e.matmul(bank(b), lhsT=Wo[:, k, oc * 128:(oc + 1) * 128], rhs=mix[:, k, g * GS:(g + 1) * GS], start=(k == 0), stop=(k == 7)),
                             reads=["Wo%d" % k, "mix"], writes=["ob%d" % b])
                    P.dve(lambda e, oc=oc, g=g, b=b, l=l: e.scalar_tensor_tensor(out=x_sb[:, oc, g * GS:(g + 1) * GS], in0=bank(b), scalar=mod[:, l, 16 + oc:17 + oc], in1=x_sb[:, oc, g * GS:(g + 1) * GS], op0=ALU.mult, op1=ALU.add),
                          reads=["ob%d" % b, "mod", "x"], writes=["x"])
            P.barrier()
            def route_tile(ti, l=l):
                def R(i, n=1):
                    return rt[:, i:i + n]
                P.dve(lambda e, ti=ti, l=l: e.tensor_tensor(out=L[:, ti, :], in0=bank(6)[:, ti * 20:(ti + 1) * 20], in1=rb_sb[:, l, :], op=ALU.add), reads=["Rps", "rb_sb"], writes=["L"])
                gl = L[:, ti, 0:4]
                el = L[:, ti, 4:20]
                P.dve(lambda e, gl=gl: e.reduce_max(out=R(0), in_=gl, axis=X), reads=["L"], writes=["r0"])
                P.dve(lambda e: e.tensor_scalar(out=R(1), in0=R(0), scalar1=-1.0, scalar2=None, op0=ALU.mult), reads=["r0"], writes=["r1"])
                P.act(lambda e, gl=gl: e.activation(out=R(2, 4), in_=gl, func=AF.Exp, bias=R(1)), reads=["L", "r1"], writes=["r2"])
                P.dve(lambda e: e.reduce_sum(out=R(6), in_=R(2, 4), axis=X), reads=["r2"], writes=["r6"])
                P.dve(lambda e: e.reciprocal(R(6), R(6)), reads=["r6"], writes=["r6"])
                P.dve(lambda e, gl=gl: e.tensor_scalar(out=R(7, 4), in0=gl, scalar1=R(0), scalar2=None, op0=ALU.is_equal), reads=["L", "r0"], writes=["r7"])
                P.dve(lambda e: e.tensor_scalar(out=R(7, 4), in0=R(7, 4), scalar1=1.0, scalar2=BIG, op0=ALU.subtract, op1=ALU.mult), reads=["r7"], writes=["r7"])
                for gi in range(4):
                    P.dve(lambda e, gi=gi, el=el: e.tensor_scalar(out=R(12 + 4 * gi, 4), in0=el[:, 4 * gi:4 * gi + 4], scalar1=R(7 + gi), scalar2=None, op0=ALU.add), reads=["L", "r7"], writes=["elm"])
                elm = R(12, 16)
                P.dve(lambda e: e.reduce_max(out=R(28), in_=elm, axis=X), reads=["elm"], writes=["m1"])
                P.dve(lambda e: e.tensor_scalar(out=R(30, 16), in0=elm, scalar1=R(28), scalar2=None, op0=ALU.is_equal), reads=["elm", "m1"], writes=["oh1"])
                P.dve(lambda e: e.scalar_tensor_tensor(out=R(46, 16), in0=R(30, 16), scalar=-BIG, in1=elm, op0=ALU.mult, op1=ALU.add), reads=["oh1", "elm"], writes=["elm2"])
                P.dve(lambda e: e.reduce_max(out=R(29), in_=R(46, 16), axis=X), reads=["elm2"], writes=["m2"])
                P.dve(lambda e: e.tensor_scalar(out=R(46, 16), in0=R(46, 16), scalar1=R(29), scalar2=None, op0=ALU.is_equal), reads=["elm2", "m2"], writes=["elm2"])
                P.dve(lambda e: e.tensor_tensor(out=R(62), in0=R(29), in1=R(28), op=ALU.subtract), reads=["m1", "m2"], writes=["dl"])
                P.act(lambda e: e.activation(out=R(62), in_=R(62), func=AF.Exp), reads=["dl"], writes=["dl"])
                P.dve(lambda e: e.tensor_scalar(out=R(63), in0=R(62), scalar1=1.0, scalar2=None, op0=ALU.add), reads=["dl"], writes=["den"])
                P.dve(lambda e: e.reciprocal(R(63), R(63)), reads=["den"], writes=["den"])
                P.dve(lambda e: e.tensor_tensor(out=R(62), in0=R(62), in1=R(63), op=ALU.mult), reads=["dl", "den"], writes=["dl"])
                P.dve(lambda e: e.tensor_tensor(out=R(63), in0=R(63), in1=R(6), op=ALU.mult), reads=["den", "r6"], writes=["den"])
                P.dve(lambda e: e.tensor_tensor(out=R(62), in0=R(62), in1=R(6), op=ALU.mult), reads=["dl", "r6"], writes=["dl"])
                P.dve(lambda e, ti=ti: e.tensor_scalar(out=gates[:, ti, :], in0=R(30, 16), scalar1=R(63), scalar2=None, op0=ALU.mult), reads=["oh1", "den"], writes=["gates"])
                P.dve(lambda e, ti=ti: e.scalar_tensor_tensor(out=gates[:, ti, :], in0=R(46, 16), scalar=R(62), in1=gates[:, ti, :], op0=ALU.mult, op1=ALU.add), reads=["elm2", "dl", "gates"], writes=["gates"])
                P.mm(lambda e, ti=ti: e.transpose(out=bank(5)[0:16, (ti % 4) * 128:(ti % 4 + 1) * 128], in_=gates[:, ti, :], identity=id_sb[:]), reads=["gates", "id_sb"], writes=["gTps%d" % (ti % 4)])
                P.act(lambda e, ti=ti: e.activation(out=gT[0:16, ti * 128:(ti + 1) * 128], in_=bank(5)[0:16, (ti % 4) * 128:(ti % 4 + 1) * 128], func=AF.Copy), reads=["gTps%d" % (ti % 4)], writes=["gT"])
            for g in range(4):
                P.act(lambda e, g=g: e.activation(out=sq[:], in_=x_sb[:, :, g * GS:(g + 1) * GS], func=AF.Square), reads=["x"], writes=["sq"])
                for k in range(8):
                    P.mm(lambda e, k=k: e.matmul(bank(7), lhsT=ones[:], rhs=sq[:, k, :], start=(k == 0), stop=(k == 7)), reads=["sq", "ones"], writes=["ssps"])
                P.act(lambda e: e.activation(out=rstd, in_=bank(7), func=AF.Ln, scale=1.0 / 1024, bias=epsb[:]), reads=["ssps", "epsb"], writes=["rstd"])
                P.act(lambda e: e.activation(out=bank(7), in_=rstd, func=AF.Exp, scale=-0.5), reads=["rstd"], writes=["ssps"])
                for k in range(8):
                    hb = hn[k % 2]
                    hbn = "hn%d" % (k % 2)
                    P.dve(lambda e, k=k, hb=hb, g=g: e.tensor_tensor(out=hb, in0=x_sb[:, k, g * GS:(g + 1) * GS], in1=bank(7), op=ALU.mult), reads=["x", "ssps"], writes=[hbn])
                    P.act(lambda e, k=k, hb=hb, l=l: e.activation(out=h32[:, k, :], in_=hb, func=AF.Identity, scale=gs2[:, k:k + 1], bias=mod[:, l, 24 + k:25 + k]), reads=[hbn, "gs2", "mod"], writes=["h32_%d" % k])
                    P.dve(lambda e, k=k, g=g: e.tensor_copy(hT[:, k, g * GS:(g + 1) * GS], h32[:, k, :]), reads=["h32_%d" % k], writes=["hT"])
                for tt in range(4):
                    ti = g * 4 + tt
                    for k in range(8):
                        P.mm(lambda e, k=k, tt=tt, ti=ti, l=l: e.matmul(bank(6)[:, ti * 20:(ti + 1) * 20], lhsT=h32[:, k, tt * 128:(tt + 1) * 128], rhs=wr_sb[:, l, k, :], start=(k == 0), stop=(k == 7)),
                             reads=["h32_%d" % k, "wr_sb"], writes=["Rps"])
                for tt in range(4):
                    route_tile(g * 4 + tt)
            stage2 = [None]

            def emit_stage2(e_, g, ab, l=l):
                wg_, wu_, wd_ = wbuf[e_ % 2]
                nm = "w%d" % (e_ % 2)
                for oc in range(8):
                    yb = 4 + (oc % 2)
                    for fc in range(4):
                        P.mm(lambda e, fc=fc, oc=oc, yb=yb, wd_=wd_, ab=ab: e.matmul(bank(yb), lhsT=wd_[:, fc, oc * 128:(oc + 1) * 128], rhs=actb[ab][:, fc, :], start=(fc == 0), stop=(fc == 3)),
                             reads=[nm + "d", "act%d" % ab], writes=["y%d" % yb])
                    P.dve(lambda e, oc=oc, g=g, yb=yb: e.scalar_tensor_tensor(out=x_sb[:, oc, g * GS:(g + 1) * GS], in0=bank(yb), scalar=mod[:, l, 40 + oc:41 + oc], in1=x_sb[:, oc, g * GS:(g + 1) * GS], op0=ALU.mult, op1=ALU.add),
                          reads=["y%d" % yb, "mod", "x"], writes=["x"])
            it = 0
            for e_ in range(16):
                wg_, wu_, wd_ = wbuf[e_ % 2]
                nm = "w%d" % (e_ % 2)
                for g in range(4):
                    ab = it % 2
                    it += 1
                    P.mm(lambda e, e_=e_, g=g: e.matmul(bank(6), lhsT=sel_sb[0:16, e_, :], rhs=gT[0:16, g * GS:(g + 1) * GS], start=True, stop=True), reads=["gT", "sel_sb"], writes=["G"])
                    P.act(lambda e, ab=ab: e.activation(out=gbs[ab], in_=bank(6), func=AF.Copy), reads=["G"], writes=["gbs%d" % ab])
                    for fc in range(4):
                        hb_ = 2 * (fc % 2)
                        for k in range(8):
                            P.mm(lambda e, k=k, fc=fc, g=g, hb_=hb_, wg_=wg_: e.matmul(bank(hb_), lhsT=wg_[:, k, fc * 128:(fc + 1) * 128], rhs=hT[:, k, g * GS:(g + 1) * GS], start=(k == 0), stop=(k == 7)),
                                 reads=[nm + "g", "hT"], writes=["hg%d" % hb_])
                        for k in range(8):
                            P.mm(lambda e, k=k, fc=fc, g=g, hb_=hb_, wu_=wu_: e.matmul(bank(hb_ + 1), lhsT=wu_[:, k, fc * 128:(fc + 1) * 128], rhs=hT[:, k, g * GS:(g + 1) * GS], start=(k == 0), stop=(k == 7)),
                                 reads=[nm + "u", "hT"], writes=["hu%d" % hb_])
                        sbi = fc % 2
                        P.act(lambda e, hb_=hb_, sbi=sbi: e.activation(out=s_sb[sbi], in_=bank(hb_), func=AF.Silu), reads=["hg%d" % hb_], writes=["s%d" % sbi])
                        P.dve(lambda e, hb_=hb_, sbi=sbi: e.tensor_tensor(out=t_sb[sbi], in0=bank(hb_ + 1), in1=s_sb[sbi], op=ALU.mult), reads=["hu%d" % hb_, "s%d" % sbi], writes=["t%d" % sbi])
                        P.pool(lambda e, fc=fc, sbi=sbi, ab=ab: e.tensor_tensor(out=actb[ab][:, fc, :], in0=t_sb[sbi], in1=gbs[ab], op=ALU.mult), reads=["t%d" % sbi, "gbs%d" % ab], writes=["act%d" % ab])
                    if stage2[0] is not None:
                        emit_stage2(*stage2[0])
                    stage2[0] = (e_, g, ab)
                    if g == 0 and e_ + 1 < 16:
                        load_expert(e_ + 1)
            emit_stage2(*stage2[0])
            P.barrier()
        out_ops = []
        for g in range(4):
            P.act(lambda e, g=g: e.activation(out=sq[:], in_=x_sb[:, :, g * GS:(g + 1) * GS], func=AF.Square), reads=["x"], writes=["sq"])
            for k in range(8):
                P.mm(lambda e, k=k: e.matmul(bank(7), lhsT=ones[:], rhs=sq[:, k, :], start=(k == 0), stop=(k == 7)), reads=["sq", "ones"], writes=["ssps"])
            P.act(lambda e: e.activation(out=rstd, in_=bank(7), func=AF.Ln, scale=1.0 / 1024, bias=epsb[:]), reads=["ssps", "epsb"], writes=["rstd"])
            P.act(lambda e: e.activation(out=bank(7), in_=rstd, func=AF.Exp, scale=-0.5), reads=["rstd"], writes=["ssps"])
            for k in range(8):
                hb = hn[k % 2]
                hbn = "hn%d" % (k % 2)
                P.dve(lambda e, k=k, hb=hb, g=g: e.tensor_tensor(out=hb, in0=x_sb[:, k, g * GS:(g + 1) * GS], in1=bank(7), op=ALU.mult), reads=["x", "ssps"], writes=[hbn])
                P.act(lambda e, k=k, hb=hb, g=g: e.activation(out=x_sb[:, k, g * GS:(g + 1) * GS], in_=hb, func=AF.Copy, scale=fngs[:, k:k + 1]), reads=[hbn, "fngs"], writes=["xf"])
        out_ops.append(P.dma("sync", xo, x_sb[:], reads=["xf"]))
        P.emit(final_wait_ops=out_ops)
    return nc


from concourse.bass_utils import run_bass_kernel_spmd
import ml_dtypes

_BF = ml_dtypes.bfloat16
_CACHE = {}


def _fm(a):
    Tn = a.shape[0]
    return np.ascontiguousarray(a.T.reshape(8, 128, Tn).transpose(1, 0, 2))


def _pk(v):
    return np.ascontiguousarray(v.reshape(-1, 128).T)


def _pkl(v):
    return np.ascontiguousarray(np.stack([_pk(v[l]) for l in range(v.shape[0])], axis=1))


def _bc(v):
    return np.ascontiguousarray(np.broadcast_to(v, (128,) + v.shape)).astype(np.float32)


def _mkcst():
    p = np.arange(128)
    c = np.zeros((128, 8), np.float32)
    c[:, 0] = (10000.0 ** (-(2 * (p % 32)).astype(np.float32) / 64)).astype(np.float32)
    c[:, 1] = (10000.0 ** (-(2 * (p % 16)).astype(np.float32) / 32)).astype(np.float32)
    c[:, 2] = np.where((p % 64) < 32, -1.0, 1.0)
    c[:, 3] = np.where((p % 32) < 16, -1.0, 1.0)
    return c


def _mkmasks(r):
    k = np.arange(128)[:, None]
    q = np.arange(128)[None, :]
    own = {}
    oth = {}
    own['A'] = (k <= q)
    oth['A'] = (k < q) if r == 0 else (k <= q)
    own['Bp'] = (k >= q + 65)
    own['Bd'] = ((q - k) >= 0) & ((q - k) <= 63)
    oth['Bp'] = (k >= q + 64) if r == 0 else (k >= q + 65)
    oth['Bd'] = (((q - k) >= 1) & ((q - k) <= 64)) if r == 0 else (((q - k) >= 0) & ((q - k) <= 63))
    own['Cp'] = (k >= q + 64)
    own['Cd'] = ((q - k) >= 0) & ((q - k) <= 64)
    oth['Cp'] = (k >= q + 64) if r == 0 else (k >= q + 65)
    oth['Cd'] = (((q - k) >= 1) & ((q - k) <= 64)) if r == 0 else (((q - k) >= 0) & ((q - k) <= 63))

    def pm(a, key):
        return own[key] if a == r else oth[key]
    m = np.zeros((15, 128, 128), np.float32)
    m[0] = pm(0, 'A'); m[1] = pm(1, 'A')
    m[2] = pm(0, 'Bp'); m[3] = pm(0, 'Bd'); m[4] = pm(1, 'Bp'); m[5] = pm(1, 'Bd')
    m[6] = pm(0, 'Cp'); m[7] = pm(0, 'Cd'); m[8] = pm(1, 'Cp'); m[9] = pm(1, 'Cd')
    m[10] = (k >= q)
    m[11] = (k <= q)
    m[12] = m[10]
    m[13] = m[11]
    m[14] = m[11]
    return np.ascontiguousarray(m.transpose(1, 0, 2)).astype(_BF)


def _mkmasks8():
    k = np.arange(128)[:, None]
    q = np.arange(128)[None, :]
    dp = (k >= q).astype(np.float32)
    dd = (k <= q).astype(np.float32)
    m = np.zeros((4, 128, 512), np.float32)
    for half in range(2):
        sl = slice(64 * half, 64 * half + 64)
        m[half] = np.concatenate([dp[:, sl], dd[:, sl]] * 4, axis=1)
        m[2 + half] = np.concatenate([dd[:, sl]] * 8, axis=1)
    return np.ascontiguousarray(m.transpose(1, 0, 2)).astype(_BF)


def kernel(x, c, positions, ada_w, ada_b, norm_mix_g, norm_ffn_g, w_in, w_out,
           diff_lambda_q1, diff_lambda_k1, diff_lambda_q2, diff_lambda_k2, diff_subln_g,
           swa_sinks, router_group_w, router_group_b, router_expert_w, router_expert_b,
           expert_w_gate, expert_w_up, expert_w_down, final_norm_g):
    f32 = np.float32
    D = DEPTH
    A = lambda v: np.ascontiguousarray(np.asarray(v, f32))
    x = A(x)
    c = A(c)
    positions = np.asarray(positions)
    if "nc" not in _CACHE:
        _CACHE["nc"] = build_fused(D)
    sel = np.zeros((16, 16, 128), f32)
    for e in range(16):
        sel[e, e, :] = 1.0
    small = np.zeros((128, D, 16), f32)
    for l in range(D):
        small[:, l, 0] = 0.8 - 0.6 * math.exp(-0.3 * l)
        small[0:64, l, 1] = A(diff_subln_g)[l]
        small[:, l, 8:16] = A(swa_sinks)[l][None, :]
    lamv = _bc(np.stack([np.stack([A(diff_lambda_q1)[l], A(diff_lambda_k1)[l], A(diff_lambda_q2)[l], A(diff_lambda_k2)[l]]) for l in range(D)]))
    wrc = np.concatenate([A(router_group_w), A(router_expert_w)], axis=2)
    wr = np.ascontiguousarray(wrc.reshape(D, 8, 128, 20).transpose(2, 0, 1, 3))
    rb = _bc(np.concatenate([A(router_group_b), A(router_expert_b)], axis=1))
    common = dict(cst=_mkcst(), ada_w=A(ada_w), ada_b=_pkl(A(ada_b)), ngm=_pkl(A(norm_mix_g)), ngf=_pkl(A(norm_ffn_g)),
                  w_in=A(w_in), w_out=A(w_out), lamv=lamv, small=small, wr=wr, rbias=rb, ident=np.eye(128, dtype=f32), sel=sel,
                  wgate=A(expert_w_gate), wup=A(expert_w_up), wdown=A(expert_w_down), fng=_pk(A(final_norm_g)))
    in_maps = []
    for core in range(8):
        b, r = core // 2, core % 2
        m = dict(common)
        m.update(xT=_fm(x[b, r::2, :]), cT=_pk(c[b]),
                 posb=np.ascontiguousarray(np.broadcast_to(positions[b, r::2][None, :], (128, 2048))).astype(np.int32),
                 masks=_mkmasks(r), masks8=_mkmasks8())
        in_maps.append(m)
    res = run_bass_kernel_spmd(_CACHE["nc"], in_maps, core_ids=list(range(8))).results
    out = np.zeros((4, 4096, 1024), f32)
    for i in range(8):
        b, r = i // 2, i % 2
        out[b, r::2, :] = np.asarray(res[i]["xo"], f32).transpose(1, 0, 2).reshape(1024, 2048).T
    return out
```

```python
import math
import contextlib
import numpy as np
import concourse.bass as bass
import concourse.mybir as mybir

F32 = mybir.dt.float32
BF16 = mybir.dt.bfloat16
I32 = mybir.dt.int32
AF = mybir.ActivationFunctionType
ALU = mybir.AluOpType

ENGS = ("tensor", "vector", "scalar", "gpsimd", "sync")
NDMASEM = 12


class Prog:
    def __init__(self, nc):
        self.nc = nc
        self.ops = []
        self.last_writer = {}
        self.readers = {}

    def op(self, eng, fn, reads=(), writes=(), dma=False):
        idx = len(self.ops)
        deps = set()
        for r in reads:
            lw = self.last_writer.get(r)
            if lw is not None:
                deps.add(lw)
        for w in writes:
            lw = self.last_writer.get(w)
            if lw is not None:
                deps.add(lw)
            for rd in self.readers.get(w, ()):
                deps.add(rd)
        deps.discard(idx)
        for w in writes:
            self.last_writer[w] = idx
            self.readers[w] = []
        for r in reads:
            if r not in writes:
                self.readers.setdefault(r, []).append(idx)
        self.ops.append(dict(eng=eng, fn=fn, deps=deps, dma=dma, idx=idx, cc=False))
        return idx

    def cc(self, fn, reads=(), writes=()):
        idx = self.op("gpsimd", fn, reads, writes)
        self.ops[idx]["cc"] = True
        return idx

    def mm(self, fn, reads=(), writes=()):
        return self.op("tensor", fn, reads, writes)

    def dve(self, fn, reads=(), writes=()):
        return self.op("vector", fn, reads, writes)

    def act(self, fn, reads=(), writes=()):
        return self.op("scalar", fn, reads, writes)

    def pool(self, fn, reads=(), writes=()):
        return self.op("gpsimd", fn, reads, writes)

    def dma(self, eng, out, in_, reads=(), writes=()):
        return self.op(eng, lambda e: e.dma_start(out=out, in_=in_), reads, writes, dma=True)

    def emit(self, final_wait_ops=()):
        nc = self.nc
        ops = self.ops
        has_dep = [False] * len(ops)
        for o in ops:
            for d in o["deps"]:
                if ops[d]["eng"] == "tensor" and o["eng"] == "tensor" and not ops[d]["dma"]:
                    continue
                has_dep[d] = True
        for d in final_wait_ops:
            has_dep[d] = True
        eng_cnt = {e: 0 for e in ENGS}
        dma_cnt = {e: 0 for e in ENGS}
        dma_semval = {}
        for o in ops:
            e = o["eng"]
            if o["cc"]:
                o["sig"] = ("cc", o["idx"])
            elif o["dma"]:
                k = dma_cnt[e] % NDMASEM
                dma_cnt[e] += 1
                key = (e, k)
                dma_semval[key] = dma_semval.get(key, 0) + 16
                o["sig"] = ("dma", e, k, dma_semval[key])
            elif has_dep[o["idx"]]:
                eng_cnt[e] += 1
                o["sig"] = ("eng", e, eng_cnt[e])
            else:
                o["sig"] = None
        import contextlib
        with contextlib.ExitStack() as es:
            esem = {e: es.enter_context(nc.semaphore("s_" + e)) for e in ENGS}
            dsem = {}
            for e in ("sync", "gpsimd", "scalar"):
                for k in range(NDMASEM):
                    dsem[(e, k)] = es.enter_context(nc.semaphore("d_%s_%d" % (e, k)))
            ccsem = {o["idx"]: es.enter_context(nc.semaphore("cc_%d" % o["idx"])) for o in ops if o["cc"]}
            block = es.enter_context(nc.Block())

            def make(engname):
                def body(eng):
                    waited = {}
                    for o in ops:
                        if o["eng"] != engname:
                            continue
                        need = {}
                        for d in o["deps"]:
                            od = ops[d]
                            if od["eng"] == "tensor" and engname == "tensor" and not od["dma"]:
                                continue
                            s = od["sig"]
                            if s is None:
                                continue
                            if s[0] == "dma":
                                key = ("dma", s[1], s[2])
                                val = s[3]
                            elif s[0] == "cc":
                                key = ("cc", s[1])
                                val = 1
                            else:
                                key = ("eng", s[1])
                                val = s[2]
                            if need.get(key, 0) < val:
                                need[key] = val
                        if o["dma"]:
                            s = o["sig"]
                            if s[3] > 16:
                                key = ("dma", s[1], s[2])
                                if need.get(key, 0) < s[3] - 16:
                                    need[key] = s[3] - 16
                        for key, val in need.items():
                            if waited.get(key, 0) >= val:
                                continue
                            waited[key] = val
                            if key[0] == "dma":
                                sem = dsem[(key[1], key[2])]
                            elif key[0] == "cc":
                                sem = ccsem[key[1]]
                            else:
                                sem = esem[key[1]]
                            eng.wait_ge(sem, val)
                        ins = o["fn"](eng)
                        s = o["sig"]
                        if s is not None:
                            if s[0] == "dma":
                                ins.then_inc(dsem[(s[1], s[2])], 16)
                            elif s[0] == "cc":
                                ins.then_inc(ccsem[s[1]])
                            else:
                                ins.then_inc(esem[s[1]], 1)
                    if engname == "sync":
                        for d in final_wait_ops:
                            s = ops[d]["sig"]
                            if s[0] == "dma":
                                eng.wait_ge(dsem[(s[1], s[2])], s[3])
                            else:
                                eng.wait_ge(esem[s[1]], s[2])
                return body

            block.tensor(make("tensor"))
            block.vector(make("vector"))
            block.scalar(make("scalar"))
            block.gpsimd(make("gpsimd"))
            block.sync(make("sync"))


def _barrier(self):
    last = {}
    dmas = {}
    for o in self.ops:
        if o["dma"]:
            dmas.setdefault(o["eng"], []).append(o["idx"])
        elif o["cc"]:
            pass
        else:
            last[o["eng"]] = o["idx"]
    s = set(last.values())
    for e, lst in dmas.items():
        s.update(lst[-NDMASEM:])
    for o in self.ops:
        if o["cc"]:
            s.add(o["idx"])
    self.pending_barrier = s
    self.barrier_done = set()


_orig_op = Prog.op


def _op(self, eng, fn, reads=(), writes=(), dma=False):
    idx = _orig_op(self, eng, fn, reads, writes, dma)
    pb = getattr(self, "pending_barrier", None)
    if pb and eng not in self.barrier_done:
        self.ops[idx]["deps"] |= set(pb)
        self.barrier_done.add(eng)
    return idx


Prog.op = _op
Prog.barrier = _barrier


T = 2048
GS = 512
NM = 15
LOOK = 2
NST = 3
NPT = 4
NUB = 4
MKINDS = ['A0', 'A1', 'B0p', 'B0d', 'B1p', 'B1d', 'C0p', 'C0d', 'C1p', 'C1d', 'Dp', 'Dd', 'Dp', 'Dd', 'Dd']
BIG = 1.0e9
PI = math.pi
DEPTH = 4
X = mybir.AxisListType.X


def build_fused(depth=DEPTH):
    nc = bass.Bass("TRN2", target_bir_lowering=False)
    dt = nc.dram_tensor

    def inp(name, shape, dtype=F32):
        return dt(name, shape, dtype, kind="ExternalInput").ap()
    xT = inp("xT", [128, 8, T])
    cT = inp("cT", [128, 8])
    posb = inp("posb", [128, T], I32)
    cst = inp("cst", [128, 8])
    masks = inp("masks", [128, NM, 128], BF16)
    masks8 = inp("masks8", [128, 4, 512], BF16)
    ada_w = inp("ada_w", [depth, 1024, 3072])
    ada_b = inp("ada_b", [128, depth, 24])
    ngm = inp("ngm", [128, depth, 8])
    ngf = inp("ngf", [128, depth, 8])
    w_in = inp("w_in", [depth, 1024, 2304])
    w_out = inp("w_out", [depth, 1024, 1024])
    lamv = inp("lamv", [128, depth, 4, 32])
    small = inp("small", [128, depth, 16])
    wr = inp("wr", [128, depth, 8, 20])
    rbias = inp("rbias", [128, depth, 20])
    ident = inp("ident", [128, 128])
    sel = inp("sel", [16, 16, 128])
    wgate = inp("wgate", [depth, 16, 1024, 512])
    wup = inp("wup", [depth, 16, 1024, 512])
    wdown = inp("wdown", [depth, 16, 512, 1024])
    fng = inp("fng", [128, 8])
    xo = dt("xo", [128, 8, T], F32, kind="ExternalOutput").ap()
    tabD = dt("tabD", [128, 4, T], F32).ap()
    modD_h = dt("modD", [128, depth * 24], F32)
    modG_h = dt("modG", [256, depth * 24], F32)
    QTd = dt("QTd", [128, 8, T], BF16).ap()
    TH = T // 2
    KTd_h = [dt("KTd%d" % i, [128, 5 * TH], BF16) for i in range(2)]
    KTg_h = [dt("KTg%d" % i, [256, 5 * TH], BF16) for i in range(2)]
    Vd_h = [dt("Vd%d" % i, [T // 2, 650], BF16) for i in range(2)]
    Vg_h = [dt("Vg%d" % i, [T, 650], BF16) for i in range(2)]
    KTd3 = [h.ap().rearrange("p (c n) -> p c n", c=5) for i, h in enumerate(KTd_h)]
    KTg4 = [h.ap().rearrange("(a p) (c n) -> a p c n", a=2, c=5) for i, h in enumerate(KTg_h)]
    Vdh = [h.ap() for h in Vd_h]
    Vgh = [h.ap() for h in Vg_h]

    def ktd(c, hh):
        return KTd3[hh][:, c, :]

    def ktg(a, c, hh):
        return KTg4[hh][a][:, c, :]

    def vd_rows(n0, cnt, step=1):
        hh = n0 // 1024
        r = n0 % 1024
        return Vdh[hh][r:r + step * (cnt - 1) + 1:step, :]

    def vg_tile(a, t):
        hh = t // 8
        r = a * 1024 + (t % 8) * 128
        return Vgh[hh][r:r + 128, :]
    groups = [[0, 1], [2, 3], [4, 5], [6, 7]]

    P = Prog(nc)
    with contextlib.ExitStack() as es:
        def sb(name, shape, dtype):
            return es.enter_context(nc.sbuf_tensor(name, shape, dtype))
        x_sb = sb("x_sb", [128, 8, T], F32)
        ARN = 50496
        arena = sb("arena", [128, ARN], BF16)
        tar = sb("tar", [128, 9, 512], F32)
        off = [0]

        def carve(n):
            a = arena[:, off[0]:off[0] + n]
            off[0] += n
            assert off[0] <= ARN, off[0]
            return a
        off[0] = 0
        Wqk = carve(8 * 1664).rearrange("p (k n) -> p k n", k=8)
        Wsqk = carve(8 * 1664).rearrange("p (k n) -> p k n", k=8)
        Wkb = carve(2048).rearrange("p (k g n) -> p k g n", k=8, g=2)
        Wskb = carve(2048).rearrange("p (k g n) -> p k g n", k=8, g=2)
        Wv = carve(8 * 640).rearrange("p (k n) -> p k n", k=8)
        sqP = carve(4096).rearrange("p (k n) -> p k n", k=8)
        hTP = carve(4096).rearrange("p (k n) -> p k n", k=8)
        tab = carve(4096).bitcast(F32).rearrange("p (k n) -> p k n", k=4)
        qkst = [carve(512) for i in range(2)]
        vst = [carve(650) for i in range(2)]
        WadaP = [arena[:, i * 8192:(i + 1) * 8192].rearrange("p (k n) -> p k n", k=8) for i in range(2)]
        posi = arena[:, 16384:16384 + 1024].bitcast(I32)
        posf = arena[:, 17408:17408 + 1024].bitcast(F32)
        ang = arena[:, 18432:18432 + 1024].bitcast(F32)
        mi = arena[:, 19456:19456 + 1024].bitcast(I32)
        off[0] = 0
        Qreg = carve(4 * T)
        slotA = [Qreg[:, i * 4096:(i + 1) * 4096].rearrange("p (t n) -> p t n", t=2) for i in range(2)]
        slotH = [Qreg[:, i * 2048:(i + 1) * 2048] for i in range(2)]
        kown = Qreg[:, 4096:8192].rearrange("p (c n) -> p c n", c=2)
        k_sb = carve(4 * T).rearrange("p (a c n) -> p a c n", a=2, c=2)
        v_sb = carve(2 * 16 * 260).rearrange("p (a t c) -> p a t c", a=2, t=16)
        v2_sb = carve(16 * 260).rearrange("p (t c) -> p t c", t=16)
        v8_sb = carve(16 * 260).rearrange("p (t c) -> p t c", t=16)
        mix = carve(8 * T).rearrange("p (c n) -> p c n", c=8)
        V_OFF = 8 * T
        V2_OFF = V_OFF + 2 * 16 * 260
        V8_OFF = V2_OFF + 16 * 260

        def vwide(base, tile_idx, col):
            o = base + tile_idx * 260 + col
            return arena[:, o:o + 128]
        off[0] = 0
        hT = carve(8 * T).rearrange("p (c n) -> p c n", c=8)
        wbuf = []
        for i in range(2):
            wg_ = carve(8 * 512).rearrange("p (k f) -> p k f", k=8)
            wu_ = carve(8 * 512).rearrange("p (k f) -> p k f", k=8)
            wd_ = carve(4 * 1024).rearrange("p (c d) -> p c d", c=4)
            wbuf.append((wg_, wu_, wd_))
        actb = [carve(4 * 512).rearrange("p (c n) -> p c n", c=4) for i in range(2)]
        gT = carve(2 * T).bitcast(F32)
        w1base = 8 * T + 3 * 4096
        h32 = arena[:, w1base:w1base + 8192].bitcast(F32).rearrange("p (k n) -> p k n", k=8)
        sq = arena[:, w1base + 8192:w1base + 8192 + 4096].rearrange("p (k n) -> p k n", k=8)
        Wo = arena[:, 0:8192].rearrange("p (k n) -> p k n", k=8)
        rr = tar[:, 0, :]; bcs = tar[:, 1, :]; tmpA = [tar[:, 2 + i, :] for i in range(3)]
        rstd = tar[:, 0, :]; hn = [tar[:, 1 + i, :] for i in range(2)]
        gbs = [tar[:, 3 + i, :] for i in range(2)]; s_sb = [tar[:, 5 + i, :] for i in range(2)]; t_sb = [tar[:, 7 + i, :] for i in range(2)]
        t1 = [tar[:, 3 + i, :] for i in range(2)]; t2 = [tar[:, 5 + i, :] for i in range(2)]
        rrs = [tar[:, 0, :], tar[:, 7, :]]
        rrh = [tar[:, 5 + i, :].bitcast(BF16)[:, 0:512] for i in range(2)]
        rrl = [tar[:, 5 + i, :].bitcast(BF16)[:, 512:1024] for i in range(2)]
        sqb = tar[:, 8, :].bitcast(BF16)[:, 0:512]

        PT = [sb("PT%d" % i, [128, 512], BF16) for i in range(NPT)]
        msk = sb("msk", [128, NM, 128], BF16)
        msk8 = sb("msk8", [128, 4, 512], BF16)
        c_sb = sb("c_sb", [128, 8], F32)
        cact = sb("cact", [128, 8], BF16)
        adab = sb("adab", [128, depth, 24], F32)
        modh = sb("modh", [128, depth, 24], F32)
        ngm_sb = sb("ngm_sb", [128, depth, 8], F32)
        ngf_sb = sb("ngf_sb", [128, depth, 8], F32)
        fngs = sb("fngs", [128, 8], F32)
        cs = sb("cs", [128, 8], F32)
        mod = sb("mod", [128, depth, 48], F32)
        gs1 = sb("gs1", [128, 8], F32)
        gs2 = sb("gs2", [128, 8], F32)
        ones = sb("ones", [128, 128], BF16)
        onesf = sb("onesf", [128, 128], F32)
        epsb = sb("epsb", [128, 1], F32)
        lam_sb = sb("lam_sb", [128, 4, 32], F32)
        lamt = sb("lamt", [128, 2, 32], F32)
        lams = sb("lams", [128, 8], F32)
        sm = sb("sm", [128, depth, 16], F32)
        sinke = sb("sinke", [128, 8], F32)
        wr_sb = sb("wr_sb", [128, depth, 8, 20], F32)
        rb_sb = sb("rb_sb", [128, depth, 20], F32)
        id_sb = sb("id_sb", [128, 128], F32)
        sel_sb = sb("sel_sb", [16, 16, 128], F32)
        L = tar[:, 4, 0:320].rearrange("p (a b) -> p a b", a=16)
        gates = tar[:, 5, 0:256].rearrange("p (a b) -> p a b", a=16)
        rt = tar[:, 3, 0:64]
        ps = es.enter_context(nc.psum_tensor("ps", [128, 8 * 512], F32))

        def bank(i):
            return ps[:, i * 512:(i + 1) * 512]

        P.dma("sync", x_sb[:], xT, writes=["x"])
        for (d_, s_, n_) in ((c_sb, cT, "c_sb"), (adab, ada_b, "adab"), (ngm_sb, ngm, "ngm"), (ngf_sb, ngf, "ngf"), (fngs, fng, "fngs"), (sm, small, "sm"),
                             (wr_sb, wr, "wr_sb"), (rb_sb, rbias, "rb_sb"), (id_sb, ident, "id_sb"), (sel_sb, sel, "sel_sb"), (msk, masks, "msk"), (msk8, masks8, "msk"), (cs, cst, "cs")):
            P.dma("sync", d_[:], s_, writes=[n_])
        P.pool(lambda e: e.memset(ones[:], 1.0), writes=["ones"])
        P.pool(lambda e: e.memset(onesf[:], 1.0), writes=["onesf"])
        P.pool(lambda e: e.memset(epsb[:], 1e-6), writes=["epsb"])
        P.act(lambda e: e.activation(out=cact[:], in_=c_sb[:], func=AF.Silu), reads=["c_sb"], writes=["cact"])
        for tg in range(4):
            tsl = slice(tg * GS, (tg + 1) * GS)
            P.dma("sync", posi, posb[:, tsl], writes=["posi"])
            P.dve(lambda e: e.tensor_copy(posf, posi), reads=["posi"], writes=["posf"])
            for ti, (fcol, scol) in enumerate(((0, 2), (1, 3))):
                P.dve(lambda e, fcol=fcol: e.tensor_scalar(out=ang, in0=posf, scalar1=cs[:, fcol:fcol + 1], scalar2=None, op0=ALU.mult), reads=["posf", "cs"], writes=["ang"])
                for which in range(2):
                    if which == 0:
                        P.dve(lambda e: e.tensor_scalar(out=t1[0], in0=ang, scalar1=0.5 * PI, scalar2=None, op0=ALU.add), reads=["ang"], writes=["t1_0"])
                    else:
                        P.dve(lambda e: e.tensor_copy(t1[0], ang), reads=["ang"], writes=["t1_0"])
                    P.dve(lambda e: e.tensor_scalar(out=t2[0], in0=t1[0], scalar1=1.0 / (2 * PI), scalar2=None, op0=ALU.mult), reads=["t1_0"], writes=["t2_0"])
                    P.dve(lambda e: e.tensor_copy(mi, t2[0]), reads=["t2_0"], writes=["mi"])
                    P.dve(lambda e: e.tensor_copy(t2[0], mi), reads=["mi"], writes=["t2_0"])
                    P.dve(lambda e: e.scalar_tensor_tensor(out=t1[0], in0=t2[0], scalar=-2 * PI, in1=t1[0], op0=ALU.mult, op1=ALU.add), reads=["t2_0", "t1_0"], writes=["t1_0"])
                    P.dve(lambda e: e.tensor_scalar(out=t2[0], in0=t1[0], scalar1=PI, scalar2=2 * PI, op0=ALU.is_gt, op1=ALU.mult), reads=["t1_0"], writes=["t2_0"])
                    P.dve(lambda e: e.tensor_tensor(out=t1[0], in0=t1[0], in1=t2[0], op=ALU.subtract), reads=["t1_0", "t2_0"], writes=["t1_0"])
                    P.dve(lambda e: e.tensor_scalar(out=t1[0], in0=t1[0], scalar1=PI, scalar2=-PI, op0=ALU.min, op1=ALU.max), reads=["t1_0"], writes=["t1_0"])
                    if which == 0:
                        P.act(lambda e, ti=ti: e.activation(out=tab[:, 2 * ti, :], in_=t1[0], func=AF.Sin), reads=["t1_0"], writes=["tab"])
                    else:
                        P.act(lambda e, ti=ti, scol=scol: e.activation(out=tab[:, 2 * ti + 1, :], in_=t1[0], func=AF.Sin, scale=cs[:, scol:scol + 1]), reads=["t1_0", "cs"], writes=["tab"])
            P.dma("sync", tabD[:, :, tsl], tab, reads=["tab"], writes=["tabD"])
        cnt = 0
        for l in range(depth):
            ada_v = ada_w[l].rearrange("(k p) n -> p k n", p=128)
            for which in range(3):
                Wa = WadaP[cnt % 2]
                wn = "Wada%d" % (cnt % 2)
                cnt += 1
                P.dma("gpsimd", Wa, ada_v[:, :, which * 1024:(which + 1) * 1024], writes=[wn])
                for j in range(8):
                    for k in range(8):
                        P.mm(lambda e, j=j, k=k, which=which, Wa=Wa: e.matmul(bank(0)[:, which * 8 + j: which * 8 + j + 1], lhsT=Wa[:, k, j * 128:(j + 1) * 128], rhs=cact[:, k:k + 1], start=(k == 0), stop=(k == 7)),
                             reads=[wn, "cact"], writes=["modps"])
            P.dve(lambda e, l=l: e.tensor_tensor(out=modh[:, l, :], in0=bank(0)[:, 0:24], in1=adab[:, l, :], op=ALU.add), reads=["modps", "adab"], writes=["modh"])
        P.dma("sync", modD_h.ap(), modh[:].rearrange("p a b -> p (a b)"), reads=["modh"], writes=["modD"])
        groups = [[0, 1], [2, 3], [4, 5], [6, 7]]
        P.cc(lambda e: e.collective_compute("AllGather", ALU.bypass, replica_groups=groups, ins=[modD_h.ap().opt()], outs=[modG_h.ap().opt()]), reads=["modD"], writes=["modG"])
        for hf in range(2):
            P.dma("sync", mod[:, :, 24 * hf:24 * hf + 24], modG_h.ap()[128 * hf:128 * hf + 128, :].rearrange("p (a b) -> p a b", a=depth), reads=["modG"], writes=["mod"])
        P.barrier()

        st = dict(i=0, pend=[], mi=0, ubank_started=set())

        def run_batch(units, scale, rowgrp, m8=None):
            b = st["i"] % NST
            pi = st["i"] % NPT
            st["i"] += 1
            ptn = "PT%d" % pi
            stn = "ST%d" % b
            o = 0
            for u in units:
                u["off"] = o
                P.mm(lambda e, u=u, o=o, b=b: e.matmul(bank(b)[:, o:o + u["n"]], lhsT=u["kt"], rhs=u["q"], start=True, stop=True),
                     reads=u["kres"] + u["qres"], writes=[stn])
                o += u["n"]
            tot = o
            ptu = ["%s_%d" % (ptn, i) for i in range(8)]
            P.act(lambda e, b=b, pi=pi, tot=tot: e.activation(out=PT[pi][:, 0:tot], in_=bank(b)[:, 0:tot], func=AF.Exp, scale=scale), reads=[stn], writes=ptu)
            merged = False
            if m8 is not None:
                P.dve(lambda e, pi=pi, tot=tot: e.tensor_tensor(out=PT[pi][:, 0:tot], in0=PT[pi][:, 0:tot], in1=msk8[:, m8, 0:tot], op=ALU.mult), reads=ptu + ["msk"], writes=ptu)
                merged = True
            elif len(units) > 1 and all(u["mask"] is not None and u["mask"][1] == 128 and u["n"] == 128 for u in units):
                kinds = [MKINDS[u["mask"][2]] for u in units]
                for s0 in range(NM - len(units) + 1):
                    if MKINDS[s0:s0 + len(units)] == kinds:
                        nn = len(units)
                        P.dve(lambda e, pi=pi, s0=s0, nn=nn: e.tensor_tensor(out=PT[pi][:, 0:nn * 128], in0=PT[pi][:, 0:nn * 128], in1=msk[:, s0:s0 + nn, :].rearrange("p a b -> p (a b)"), op=ALU.mult),
                              reads=ptu + ["msk"], writes=ptu)
                        merged = True
                        break
            if not merged:
                for ui, u in enumerate(units):
                    if u["mask"] is not None:
                        if u["mask"][1] >= 128:
                            eng = "vector"
                        else:
                            eng = "gpsimd" if st["mi"] % 2 == 0 else "vector"
                            st["mi"] += 1
                        o = u["off"]
                        m = u["mask"]
                        P.op(eng, lambda e, pi=pi, o=o, m=m: e.tensor_tensor(out=PT[pi][:, o:o + m[1]], in0=PT[pi][:, o:o + m[1]], in1=m[0], op=ALU.mult), reads=[ptu[ui], "msk"], writes=[ptu[ui]])
            st["pend"].append((units, pi))
            while len(st["pend"]) > LOOK:
                emit_pv(*st["pend"].pop(0))
            for it_ in fin_q:
                it_[1] -= 1
            while fin_q and fin_q[0][1] <= 0:
                fin_q.pop(0)[0]()

        def emit_pv(units, b):
            ptn = "PT%d" % b
            for ui, u in enumerate(units):
                first = u["ubank"] not in st["ubank_started"]
                st["ubank_started"].add(u["ubank"])
                o = u["off"]
                P.mm(lambda e, u=u, o=o, b=b, first=first: e.matmul(u["u"], lhsT=u["v"], rhs=PT[b][:, o:o + u["n"]], start=first, stop=False, skip_group_check=True),
                     reads=["%s_%d" % (ptn, ui)] + u["vres"], writes=[u["ures"]])

        def flush():
            while st["pend"]:
                emit_pv(*st["pend"].pop(0))
        fin_q = []

        def schedule_fin(stage1, stage2):
            fin_q.append([stage1, LOOK + 1])
            fin_q.append([stage2, LOOK + 4])
            fin_q.sort(key=lambda t: t[1])

        def drain_fins():
            flush()
            while fin_q:
                fin_q.pop(0)[0]()
        ucount = [0]

        def new_ubank():
            ub = NST + (ucount[0] % NUB)
            ucount[0] += 1
            st["ubank_started"].discard(ub)
            return ub

        def rr_chain(ub, ri, sink_col):
            ures = "U%d" % ub
            rr_ = rrs[ri]
            rn = "rr%d" % ri
            if sink_col is not None:
                P.act(lambda e: e.activation(out=rr_[64:65, :], in_=bank(ub)[64:65, :], func=AF.Ln, bias=sinke[64:65, sink_col:sink_col + 1]), reads=[ures, "sinke"], writes=[rn])
            else:
                P.act(lambda e: e.activation(out=rr_[64:65, :], in_=bank(ub)[64:65, :], func=AF.Ln), reads=[ures], writes=[rn])
            P.act(lambda e: e.activation(out=rr_[64:65, :], in_=rr_[64:65, :], func=AF.Exp, scale=-1.0), reads=[rn], writes=[rn])

        def bcast(ri):
            rn = "rr%d" % ri
            rr_ = rrs[ri]
            P.mm(lambda e: e.matmul(bank(7)[0:64, :], lhsT=onesf[64:65, 0:64], rhs=rr_[64:65, :], start=True, stop=True), reads=[rn, "onesf"], writes=["BC"])
            P.dve(lambda e: e.tensor_copy(bcs[0:64, :], bank(7)[0:64, :]), reads=["BC"], writes=["bcs"])

        def finalize_simple(ub, head_feat, g, sink_col):
            ures = "U%d" % ub
            ri = ucount[0] % 2

            def stage1():
                rr_chain(ub, ri, sink_col)

            def stage2():
                bcast(ri)
                ch, po = head_feat // 128, head_feat % 128
                if po == 0:
                    P.dve(lambda e: e.tensor_tensor(out=mix[0:64, ch, g * GS:(g + 1) * GS], in0=bank(ub)[0:64, :], in1=bcs[0:64, :], op=ALU.mult), reads=[ures, "bcs"], writes=["mix"])
                else:
                    P.dve(lambda e: e.tensor_tensor(out=tmpA[0][0:64, :], in0=bank(ub)[0:64, :], in1=bcs[0:64, :], op=ALU.mult), reads=[ures, "bcs"], writes=["tmpA0"])
                    P.act(lambda e: e.activation(out=mix[po:po + 64, ch, g * GS:(g + 1) * GS], in_=tmpA[0][0:64, :], func=AF.Copy), reads=["tmpA0"], writes=["mix"])
            schedule_fin(stage1, stage2)

        def load_kv(c0, nc_, vc0, vw):
            for hh in range(2):
                for a in range(2):
                    P.dma("sync", k_sb[:, a, 0:nc_, hh * TH:(hh + 1) * TH], KTg4[hh][a][:, c0:c0 + nc_, :], reads=["KTg%d" % hh], writes=["k_sb%d" % hh])
                    P.dma("sync", v_sb[:, a, 8 * hh:8 * hh + 8, 0:vw], Vgh[hh][a * TH:(a + 1) * TH, vc0:vc0 + vw].rearrange("(t p) w -> p t w", p=128),
                          reads=["Vg%d" % hh], writes=["v_sb%d" % hh])
        vq = [0]

        def load_v1(dst, src_rows_ap, nheads, resname, extra_reads):
            vq[0] += 1
            P.dma("sync", dst.rearrange("p (h c) -> p h c", c=65)[:, :, 0:64], src_rows_ap.rearrange("p (h c) -> p h c", c=64), reads=extra_reads, writes=[resname])

        chunks = []
        chunks += [("w", 0, 32, ("q", 0)), ("w", 128, 32, ("q", 1)), ("w", 256, 32, ("k", 0)), ("w", 384, 32, ("k", 1))]
        chunks += [("w", 512 + 128 * i, 64, ("q", 2 + i)) for i in range(4)]
        chunks += [("w", 1024, 64, ("k", 2))]
        chunks += [("w", 1152 + 128 * i, 64, ("q", 6 + i)) for i in range(2)]
        chunks += [("w", 1408 + 128 * i, 64, ("k", 3 + i)) for i in range(2)]

        for l in range(depth):
            last = (l == depth - 1)
            lam_init_col = sm[:, l, 0:1]
            P.dve(lambda e, l=l: e.scalar_tensor_tensor(out=gs1[:], in0=mod[:, l, 8:16], scalar=1.0, in1=ngm_sb[:, l, :], op0=ALU.add, op1=ALU.mult), reads=["mod", "ngm"], writes=["gs1"])
            P.dve(lambda e, l=l: e.scalar_tensor_tensor(out=gs2[:], in0=mod[:, l, 32:40], scalar=1.0, in1=ngf_sb[:, l, :], op0=ALU.add, op1=ALU.mult), reads=["mod", "ngf"], writes=["gs2"])
            P.dma("sync", lam_sb[:], lamv[:, l, :, :], writes=["lam_sb"])
            for i in range(2):
                P.dve(lambda e, i=i, l=l: e.tensor_tensor(out=lamt[:, i, :], in0=lam_sb[:, 2 * i, :], in1=lam_sb[:, 2 * i + 1, :], op=ALU.mult), reads=["lam_sb"], writes=["lamt"])
                P.dve(lambda e, i=i: e.reduce_sum(out=lams[:, i:i + 1], in_=lamt[:, i, :], axis=X), reads=["lamt"], writes=["lams"])
            P.act(lambda e: e.activation(out=lams[:, 2:4], in_=lams[:, 0:2], func=AF.Exp), reads=["lams"], writes=["lams"])
            P.dve(lambda e: e.tensor_tensor(out=lams[:, 4:5], in0=lams[:, 3:4], in1=lams[:, 2:3], op=ALU.subtract), reads=["lams"], writes=["lams"])
            P.dve(lambda e, l=l: e.tensor_tensor(out=lams[:, 4:5], in0=lams[:, 4:5], in1=sm[:, l, 0:1], op=ALU.subtract), reads=["lams", "sm"], writes=["lams"])
            P.dve(lambda e, l=l: e.tensor_scalar(out=lams[:, 5:6], in0=sm[:, l, 0:1], scalar1=-1.0, scalar2=1.0, op0=ALU.mult, op1=ALU.add), reads=["sm"], writes=["lams"])
            P.dve(lambda e, l=l: e.tensor_tensor(out=lams[:, 6:7], in0=lams[:, 5:6], in1=sm[:, l, 1:2], op=ALU.mult), reads=["lams", "sm"], writes=["lams"])
            P.act(lambda e, l=l: e.activation(out=sinke[:], in_=sm[:, l, 8:16], func=AF.Exp), reads=["sm"], writes=["sinke"])

            w_in_v = w_in[l].rearrange("(k p) n -> p k n", p=128)
            for k in range(8):
                for (d0, s0, n) in ((0, 0, 512), (512, 768, 640), (1152, 1536, 512)):
                    P.dma("gpsimd", Wqk[:, k, d0:d0 + n], w_in_v[:, k, s0:s0 + n], writes=["W%d" % k])
                for (d0, s0, n) in ((0, 512, 256), (256, 1408, 128), (384, 2048, 256)):
                    P.dma("gpsimd", Wv[:, k, d0:d0 + n], w_in_v[:, k, s0:s0 + n], writes=["Wv%d" % k])

            def swapcopy(k, c0, nheads, dh):
                h = dh // 2
                d = Wsqk[:, k, c0:c0 + nheads * dh].rearrange("p (a t c) -> p a t c", t=2, c=h)
                s = Wqk[:, k, c0:c0 + nheads * dh].rearrange("p (a t c) -> p a t c", t=2, c=h)
                P.act(lambda e: e.activation(out=d[:, :, 0, :], in_=s[:, :, 1, :], func=AF.Copy), reads=["W%d" % k], writes=["Ws%d" % k])
                P.act(lambda e: e.activation(out=d[:, :, 1, :], in_=s[:, :, 0, :], func=AF.Copy), reads=["W%d" % k], writes=["Ws%d" % k])
            for k in range(8):
                swapcopy(k, 0, 16, 32)
                swapcopy(k, 512, 10, 64)
                swapcopy(k, 1152, 8, 64)
            def exchange(hh):
                P.cc(lambda e: e.collective_compute("AllGather", ALU.bypass, replica_groups=groups, ins=[KTd_h[hh].ap().opt()], outs=[KTg_h[hh].ap().opt()]), reads=["KTd%d" % hh], writes=["KTg%d" % hh])
                P.cc(lambda e: e.collective_compute("AllGather", ALU.bypass, replica_groups=groups, ins=[Vd_h[hh].ap().opt()], outs=[Vg_h[hh].ap().opt()]), reads=["Vd%d" % hh], writes=["Vg%d" % hh])
            for i in range(2):
                P.pool(lambda e, i=i: e.memset(vst[i], 1.0), writes=["vst%d" % i])
            qi = 0
            for tg in range(4):
                tsl = slice(tg * GS, (tg + 1) * GS)
                P.dma("sync", tab, tabD[:, :, tsl], reads=["tabD"], writes=["tab"])
                P.act(lambda e, tg=tg: e.activation(out=sqP[:], in_=x_sb[:, :, tg * GS:(tg + 1) * GS], func=AF.Square), reads=["x"], writes=["sqP"])
                for k in range(8):
                    P.mm(lambda e, k=k: e.matmul(bank(7), lhsT=ones[:], rhs=sqP[:, k, :], start=(k == 0), stop=(k == 7)), reads=["sqP", "ones"], writes=["ssps"])
                P.act(lambda e: e.activation(out=rstd, in_=bank(7), func=AF.Ln, scale=1.0 / 1024, bias=epsb[:]), reads=["ssps", "epsb"], writes=["rstd"])
                P.act(lambda e: e.activation(out=bank(7), in_=rstd, func=AF.Exp, scale=-0.5), reads=["rstd"], writes=["ssps"])
                for k in range(8):
                    hb = hn[k % 2]
                    hbn = "hn%d" % (k % 2)
                    P.dve(lambda e, k=k, hb=hb, tg=tg: e.tensor_tensor(out=hb, in0=x_sb[:, k, tg * GS:(tg + 1) * GS], in1=bank(7), op=ALU.mult), reads=["x", "ssps"], writes=[hbn])
                    P.act(lambda e, k=k, hb=hb, l=l: e.activation(out=hTP[:, k, :], in_=hb, func=AF.Identity, scale=gs1[:, k:k + 1], bias=mod[:, l, k:k + 1]), reads=[hbn, "gs1", "mod"], writes=["hTP%d" % k])
                for ci, (kind, src, tt, dest) in enumerate(chunks):
                    b1 = 1 + 2 * (ci % 2)
                    b2 = b1 + 1
                    ti = 0 if tt == 64 else 1
                    for k in range(8):
                        if kind == "w":
                            l1 = Wqk[:, k, src:src + 128]; r1 = "W%d" % k
                        else:
                            l1 = Wkb[:, k, src, :]; r1 = "Wkb%d" % k
                        P.mm(lambda e, k=k, l1=l1, b1=b1: e.matmul(bank(b1), lhsT=l1, rhs=hTP[:, k, :], start=(k == 0), stop=(k == 7)), reads=[r1, "hTP%d" % k], writes=["bank%d" % b1])
                    for k in range(8):
                        if kind == "w":
                            l2 = Wsqk[:, k, src:src + 128]; r2 = "Ws%d" % k
                        else:
                            l2 = Wskb[:, k, src, :]; r2 = "Wskb%d" % k
                        P.mm(lambda e, k=k, l2=l2, b2=b2: e.matmul(bank(b2), lhsT=l2, rhs=hTP[:, k, :], start=(k == 0), stop=(k == 7)), reads=[r2, "hTP%d" % k], writes=["bank%d" % b2])
                    a = t1[ci % 2]; an = "t1_%d" % (ci % 2)
                    bb = t2[ci % 2]; bn = "t2_%d" % (ci % 2)
                    P.dve(lambda e, a=a, b1=b1, ti=ti: e.tensor_tensor(out=a, in0=bank(b1), in1=tab[:, 2 * ti, :], op=ALU.mult), reads=["bank%d" % b1, "tab"], writes=[an])
                    P.dve(lambda e, bb=bb, b2=b2, ti=ti: e.tensor_tensor(out=bb, in0=bank(b2), in1=tab[:, 2 * ti + 1, :], op=ALU.mult), reads=["bank%d" % b2, "tab"], writes=[bn])
                    so = qkst[qi % 2]; son = "qkst%d" % (qi % 2)
                    qi += 1
                    P.pool(lambda e, a=a, bb=bb, so=so: e.tensor_tensor(out=so, in0=a, in1=bb, op=ALU.add), reads=[an, bn], writes=[son])
                    if dest[0] == "q":
                        P.dma("sync", QTd[:, dest[1], tsl], so, reads=[son], writes=["QTd"])
                    else:
                        P.dma("sync", ktd(dest[1], tg // 2)[:, (tg % 2) * GS:(tg % 2 + 1) * GS], so, reads=[son], writes=["KTd%d" % (tg // 2)])
                for tt4 in range(4):
                    vb = vst[tt4 % 2]; vn = "vst%d" % (tt4 % 2)
                    for k in range(8):
                        P.mm(lambda e, k=k, tt4=tt4: e.matmul(bank(5), lhsT=hTP[:, k, tt4 * 128:(tt4 + 1) * 128], rhs=Wv[:, k, 0:512], start=(k == 0), stop=(k == 7)), reads=["Wv%d" % k, "hTP%d" % k], writes=["bank5"])
                    for k in range(8):
                        P.mm(lambda e, k=k, tt4=tt4: e.matmul(bank(6)[:, 0:128], lhsT=hTP[:, k, tt4 * 128:(tt4 + 1) * 128], rhs=Wv[:, k, 512:640], start=(k == 0), stop=(k == 7)), reads=["Wv%d" % k, "hTP%d" % k], writes=["bank6"])
                    for (d0, nh, src) in ((0, 4, bank(5)[:, 0:256]), (260, 2, bank(5)[:, 256:384]), (390, 2, bank(5)[:, 384:512]), (520, 2, bank(6)[:, 0:128])):
                        P.act(lambda e, vb=vb, d0=d0, nh=nh, src=src: e.activation(out=vb[:, d0:d0 + 65 * nh].rearrange("p (h c) -> p h c", c=65)[:, :, 0:64], in_=src.rearrange("p (h c) -> p h c", c=64), func=AF.Copy),
                              reads=["bank5", "bank6"], writes=[vn])
                    r0 = tg * GS + tt4 * 128
                    P.dma("sync", vd_rows(r0, 128), vb, reads=[vn], writes=["Vd%d" % (tg // 2)])
                if tg == 1:
                    exchange(0)
            P.barrier()
            exchange(1)

            load_kv(0, 2, 0, 260)
            for cp in range(2):
                for hh in range(2):
                    P.dma("sync", v2_sb[:, cp * 8 + 4 * hh:cp * 8 + 4 * hh + 4, :], Vdh[hh][cp:cp + 1023:2, 390:650].rearrange("(t p) w -> p t w", p=128), reads=["Vd%d" % hh], writes=["v2_sb"])
            for bt in range(2):
                P.dma("sync", v8_sb[:, bt:16:2, :], Vdh[bt][:, 390:650].rearrange("(p c) w -> p c w", c=8), reads=["Vd%d" % bt], writes=["v8_sb"])
            hcnt = [0]

            def prepA(h):
                si = hcnt[0] % 2
                hcnt[0] += 1
                sl = slotA[si]
                P.pool(lambda e: e.memset(sl, 0.0), writes=["qs%d" % si])
                for tau in range(2):
                    P.dma("sync", sl[32 * h:32 * h + 32, tau, :], QTd[32 * h:32 * h + 32, tau, :], reads=["QTd"], writes=["qs%d" % si])
                return sl, "qs%d" % si
            slotsA_ = {0: prepA(0)}
            scaleA = 32 ** -0.5
            for h in range(4):
                if h + 1 < 4:
                    slotsA_[h + 1] = prepA(h + 1)
                qsl, qsn = slotsA_[h]
                for g in range(4):
                    ubs = []
                    for tau in range(2):
                        ub = new_ubank()
                        ubs.append(ub)
                        for jp in range(4 * g + 4):
                            for a in range(2):
                                if jp < 4 * g:
                                    q0, n, m = g * GS, GS, None
                                else:
                                    q0 = jp * 128
                                    n = (g + 1) * GS - q0
                                    m = (msk[:, a, :], 128, a)
                                u = dict(kt=k_sb[:, a, tau, jp * 128:(jp + 1) * 128], q=qsl[:, tau, q0:q0 + n], n=n, mask=m,
                                         v=vwide(V_OFF, a * 16 + jp, 65 * h), u=bank(ub)[:, q0 - g * GS:q0 - g * GS + n], ures="U%d" % ub, ubank=ub,
                                         kres=["k_sb%d" % (jp // 8)], qres=[qsn], vres=["v_sb%d" % (jp // 8)])
                                run_batch([u], scaleA, 0)
                    def mkfin(ubs=ubs, h=h, g=g):
                        def stage1():
                            for tau in range(2):
                                rr_chain(ubs[tau], tau, None)

                        def stage2():
                            for tau in range(2):
                                ub = ubs[tau]
                                bcast(tau)
                                P.dve(lambda e, ub=ub, tau=tau: e.tensor_tensor(out=tmpA[tau][0:64, :], in0=bank(ub)[0:64, :], in1=bcs[0:64, :], op=ALU.mult), reads=["U%d" % ub, "bcs"], writes=["tmpA%d" % tau])
                            P.dve(lambda e: e.scalar_tensor_tensor(out=tmpA[0][0:64, :], in0=tmpA[1][0:64, :], scalar=lams[0:64, 4:5], in1=tmpA[0][0:64, :], op0=ALU.mult, op1=ALU.add), reads=["tmpA0", "tmpA1", "lams"], writes=["tmpA0"])
                            P.dve(lambda e: e.tensor_tensor(out=sqb[0:64, :], in0=tmpA[0][0:64, :], in1=tmpA[0][0:64, :], op=ALU.mult), reads=["tmpA0"], writes=["sqb"])
                            P.mm(lambda e: e.matmul(bank(7)[0:64, :], lhsT=ones[0:64, 0:64], rhs=sqb[0:64, :], start=True, stop=True), reads=["sqb", "ones"], writes=["BC"])
                            P.act(lambda e: e.activation(out=bcs[0:64, :], in_=bank(7)[0:64, :], func=AF.Ln, scale=1.0 / 64, bias=epsb[0:64, :]), reads=["BC", "epsb"], writes=["bcs"])
                            P.act(lambda e: e.activation(out=bcs[0:64, :], in_=bcs[0:64, :], func=AF.Exp, scale=-0.5), reads=["bcs"], writes=["bcs"])
                            P.dve(lambda e: e.tensor_tensor(out=tmpA[0][0:64, :], in0=tmpA[0][0:64, :], in1=bcs[0:64, :], op=ALU.mult), reads=["tmpA0", "bcs"], writes=["tmpA0"])
                            ch, po = (64 * h) // 128, (64 * h) % 128
                            if po == 0:
                                P.dve(lambda e: e.tensor_scalar(out=mix[0:64, ch, g * GS:(g + 1) * GS], in0=tmpA[0][0:64, :], scalar1=lams[0:64, 6:7], scalar2=None, op0=ALU.mult), reads=["tmpA0", "lams"], writes=["mix"])
                            else:
                                P.act(lambda e: e.activation(out=mix[po:po + 64, ch, g * GS:(g + 1) * GS], in_=tmpA[0][0:64, :], func=AF.Copy, scale=lams[0:64, 6:7]), reads=["tmpA0", "lams"], writes=["mix"])
                        return stage1, stage2
                    schedule_fin(*mkfin())
            drain_fins()

            for ci in range(2):
                for hh in range(2):
                    P.dma("sync", kown[:, ci, hh * TH:(hh + 1) * TH], ktd(3 + ci, hh), reads=["KTd%d" % hh], writes=["kown", "qs1"])
            load_kv(2, 1, 260, 130)

            def prepH(dst_row, src_chunk, src_row):
                si = hcnt[0] % 2
                hcnt[0] += 1
                sl = slotH[si]
                P.pool(lambda e: e.memset(sl, 0.0), writes=["qs%d" % si])
                P.dma("sync", sl[dst_row:dst_row + 64, :], QTd[src_row:src_row + 64, src_chunk, :], reads=["QTd"], writes=["qs%d" % si])
                return sl, "qs%d" % si
            slotsB_ = {0: prepH(0, 2, 0)}
            scaleB = 64 ** -0.5
            for h in range(8):
                kv = h // 4
                if h + 1 < 8:
                    slotsB_[h + 1] = prepH(64 * ((h + 1) // 4), 2 + (h + 1) // 2, 64 * ((h + 1) % 2))
                qsl, qsn = slotsB_[h]
                for g in range(4):
                    ub = new_ubank()
                    for j in range(4 * g, 4 * g + 4):
                        units = []
                        for (a, jj, mid) in ((0, j - 1, 2), (0, j, 3), (1, j - 1, 4), (1, j, 5)):
                            if jj < 0:
                                continue
                            units.append(dict(kt=k_sb[:, a, 0, jj * 128:(jj + 1) * 128], q=qsl[:, j * 128:(j + 1) * 128], n=128, mask=(msk[:, mid, :], 128, mid),
                                              v=vwide(V_OFF, a * 16 + jj, 65 * kv), u=bank(ub)[:, (j - 4 * g) * 128:(j - 4 * g + 1) * 128], ures="U%d" % ub, ubank=ub,
                                              kres=["k_sb%d" % (jj // 8)], qres=[qsn], vres=["v_sb%d" % (jj // 8)]))
                        run_batch(units, scaleB, 0)
                    finalize_simple(ub, 256 + 64 * h, g, h)
            drain_fins()

            load_kv(3, 2, 390, 260)
            slotsC_ = {0: prepH(0, 6, 0)}
            for h in range(4):
                rg = 64 * (h % 2)
                qc = h // 2
                if h + 1 < 4:
                    slotsC_[h + 1] = prepH(64 * ((h + 1) % 2), 6 + (h + 1) // 2, 64 * ((h + 1) % 2))
                qsl, qsn = slotsC_[h]
                for g in range(4):
                    ub = new_ubank()
                    for j in range(4 * g, 4 * g + 4):
                        units = []
                        for (a, jj, mid) in ((0, j - 1, 6), (0, j, 7), (1, j - 1, 8), (1, j, 9)):
                            if jj < 0:
                                continue
                            units.append(dict(kt=k_sb[:, a, qc, jj * 128:(jj + 1) * 128], q=qsl[:, j * 128:(j + 1) * 128], n=128, mask=(msk[:, mid, :], 128, mid),
                                              v=vwide(V_OFF, a * 16 + jj, 65 * h), u=bank(ub)[:, (j - 4 * g) * 128:(j - 4 * g + 1) * 128], ures="U%d" % ub, ubank=ub,
                                              kres=["k_sb%d" % (jj // 8)], qres=[qsn], vres=["v_sb%d" % (jj // 8)]))
                        run_batch(units, scaleB, 0)
                    units = []
                    for cp in range(2):
                        for bt in (2 * g, 2 * g + 1):
                            qn0 = cp + 256 * bt
                            for (bb_, mid) in ((bt - 1, 10), (bt, 11)):
                                if bb_ < 0:
                                    continue
                                kn0 = cp + 256 * bb_
                                units.append(dict(kt=kown[:, qc, kn0:kn0 + 255:2], q=qsl[:, qn0:qn0 + 255:2], n=128, mask=(msk[:, mid, :], 128, mid),
                                                  v=vwide(V2_OFF, cp * 8 + bb_, 65 * h), u=bank(ub)[:, qn0 - g * GS:qn0 - g * GS + 255:2], ures="U%d" % ub, ubank=ub,
                                                  kres=["kown"], qres=[qsn], vres=["v2_sb"], pair=(bt > 0)))
                    units.sort(key=lambda u: 0 if u["pair"] else 1)
                    for i in range(0, len(units), 4):
                        run_batch(units[i:i + 4], scaleB, 0)
                    units = []
                    bt = g // 2
                    half = g % 2
                    for cp in range(8):
                        qn0 = cp + 1024 * bt + 8 * 64 * half
                        for (bb_, mid) in ((bt - 1, 10), (bt, 11)):
                            if bb_ < 0:
                                continue
                            kn0 = cp + 1024 * bb_
                            units.append(dict(kt=kown[:, qc, kn0:kn0 + 1017:8], q=qsl[:, qn0:qn0 + 505:8], n=64, mask=(msk[:, mid, 64 * half:64 * half + 64], 64, mid),
                                              v=vwide(V8_OFF, cp * 2 + bb_, 65 * h), u=bank(ub)[:, qn0 - g * GS:qn0 - g * GS + 505:8], ures="U%d" % ub, ubank=ub,
                                              kres=["kown"], qres=[qsn], vres=["v8_sb"]))
                    for i in range(0, len(units), 8):
                        run_batch(units[i:i + 8], scaleB, 0, m8=(half if bt > 0 else 2 + half))
                    finalize_simple(ub, 768 + 64 * h, g, None)
            drain_fins()
            P.barrier()
            def load_expert(e_, l=l):
                wg_, wu_, wd_ = wbuf[e_ % 2]
                nm = "w%d" % (e_ % 2)
                extra = (["h32_%d" % k for k in range(8)] + ["sq"]) if e_ == 1 else []
                P.dma("gpsimd", wg_, wgate[l, e_].rearrange("(k p) f -> p k f", p=128), writes=[nm + "g"] + extra)
                P.dma("gpsimd", wu_, wup[l, e_].rearrange("(k p) f -> p k f", p=128), writes=[nm + "u"] + extra)
                P.dma("gpsimd", wd_, wdown[l, e_].rearrange("(c p) d -> p c d", p=128), writes=[nm + "d"] + extra)
            load_expert(0)
            wo_v = w_out[l].rearrange("(k p) n -> p k n", p=128)
            for k in range(8):
                P.dma("gpsimd", Wo[:, k, :], wo_v[:, k, :], writes=["Wo%d" % k])
            cnt = 0
            for g in range(4):
                for oc in range(8):
                    b = cnt % 2
                    cnt += 1
                    for k in range(8):
                        P.mm(lambda e, k=k, oc=oc, g=g, b=b: e.matmul(bank(b), lhsT=Wo[:, k, oc * 128:(oc + 1) * 128], rhs=mix[:, k, g * GS:(g + 1) * GS], start=(k == 0), stop=(k == 7)),
                             reads=["Wo%d" % k, "mix"], writes=["ob%d" % b])
                    P.dve(lambda e, oc=oc, g=g, b=b, l=l: e.scalar_tensor_tensor(out=x_sb[:, oc, g * GS:(g + 1) * GS], in0=bank(b), scalar=mod[:, l, 16 + oc:17 + oc], in1=x_sb[:, oc, g * GS:(g + 1) * GS], op0=ALU.mult, op1=ALU.add),
                          reads=["ob%d" % b, "mod", "x"], writes=["x"])
            P.barrier()
            def route_tile(ti, l=l):
                def R(i, n=1):
                    return rt[:, i:i + n]
                P.dve(lambda e, ti=ti, l=l: e.tensor_tensor(out=L[:, ti, :], in0=bank(6)[:, ti * 20:(ti + 1) * 20], in1=rb_sb[:, l, :], op=ALU.add), reads=["Rps", "rb_sb"], writes=["L"])
                gl = L[:, ti, 0:4]
                el = L[:, ti, 4:20]
                P.dve(lambda e, gl=gl: e.reduce_max(out=R(0), in_=gl, axis=X), reads=["L"], writes=["r0"])
                P.dve(lambda e: e.tensor_scalar(out=R(1), in0=R(0), scalar1=-1.0, scalar2=None, op0=ALU.mult), reads=["r0"], writes=["r1"])
                P.act(lambda e, gl=gl: e.activation(out=R(2, 4), in_=gl, func=AF.Exp, bias=R(1)), reads=["L", "r1"], writes=["r2"])
                P.dve(lambda e: e.reduce_sum(out=R(6), in_=R(2, 4), axis=X), reads=["r2"], writes=["r6"])
                P.dve(lambda e: e.reciprocal(R(6), R(6)), reads=["r6"], writes=["r6"])
                P.dve(lambda e, gl=gl: e.tensor_scalar(out=R(7, 4), in0=gl, scalar1=R(0), scalar2=None, op0=ALU.is_equal), reads=["L", "r0"], writes=["r7"])
                P.dve(lambda e: e.tensor_scalar(out=R(7, 4), in0=R(7, 4), scalar1=1.0, scalar2=BIG, op0=ALU.subtract, op1=ALU.mult), reads=["r7"], writes=["r7"])
                for gi in range(4):
                    P.dve(lambda e, gi=gi, el=el: e.tensor_scalar(out=R(12 + 4 * gi, 4), in0=el[:, 4 * gi:4 * gi + 4], scalar1=R(7 + gi), scalar2=None, op0=ALU.add), reads=["L", "r7"], writes=["elm"])
                elm = R(12, 16)
                P.dve(lambda e: e.reduce_max(out=R(28), in_=elm, axis=X), reads=["elm"], writes=["m1"])
                P.dve(lambda e: e.tensor_scalar(out=R(30, 16), in0=elm, scalar1=R(28), scalar2=None, op0=ALU.is_equal), reads=["elm", "m1"], writes=["oh1"])
                P.dve(lambda e: e.scalar_tensor_tensor(out=R(46, 16), in0=R(30, 16), scalar=-BIG, in1=elm, op0=ALU.mult, op1=ALU.add), reads=["oh1", "elm"], writes=["elm2"])
                P.dve(lambda e: e.reduce_max(out=R(29), in_=R(46, 16), axis=X), reads=["elm2"], writes=["m2"])
                P.dve(lambda e: e.tensor_scalar(out=R(46, 16), in0=R(46, 16), scalar1=R(29), scalar2=None, op0=ALU.is_equal), reads=["elm2", "m2"], writes=["elm2"])
                P.dve(lambda e: e.tensor_tensor(out=R(62), in0=R(29), in1=R(28), op=ALU.subtract), reads=["m1", "m2"], writes=["dl"])
                P.act(lambda e: e.activation(out=R(62), in_=R(62), func=AF.Exp), reads=["dl"], writes=["dl"])
                P.dve(lambda e: e.tensor_scalar(out=R(63), in0=R(62), scalar1=1.0, scalar2=None, op0=ALU.add), reads=["dl"], writes=["den"])
                P.dve(lambda e: e.reciprocal(R(63), R(63)), reads=["den"], writes=["den"])
                P.dve(lambda e: e.tensor_tensor(out=R(62), in0=R(62), in1=R(63), op=ALU.mult), reads=["dl", "den"], writes=["dl"])
                P.dve(lambda e: e.tensor_tensor(out=R(63), in0=R(63), in1=R(6), op=ALU.mult), reads=["den", "r6"], writes=["den"])
                P.dve(lambda e: e.tensor_tensor(out=R(62), in0=R(62), in1=R(6), op=ALU.mult), reads=["dl", "r6"], writes=["dl"])
                P.dve(lambda e, ti=ti: e.tensor_scalar(out=gates[:, ti, :], in0=R(30, 16), scalar1=R(63), scalar2=None, op0=ALU.mult), reads=["oh1", "den"], writes=["gates"])
                P.dve(lambda e, ti=ti: e.scalar_tensor_tensor(out=gates[:, ti, :], in0=R(46, 16), scalar=R(62), in1=gates[:, ti, :], op0=ALU.mult, op1=ALU.add), reads=["elm2", "dl", "gates"], writes=["gates"])
                P.mm(lambda e, ti=ti: e.transpose(out=bank(5)[0:16, (ti % 4) * 128:(ti % 4 + 1) * 128], in_=gates[:, ti, :], identity=id_sb[:]), reads=["gates", "id_sb"], writes=["gTps%d" % (ti % 4)])
                P.act(lambda e, ti=ti: e.activation(out=gT[0:16, ti * 128:(ti + 1) * 128], in_=bank(5)[0:16, (ti % 4) * 128:(ti % 4 + 1) * 128], func=AF.Copy), reads=["gTps%d" % (ti % 4)], writes=["gT"])
            for g in range(4):
                P.act(lambda e, g=g: e.activation(out=sq[:], in_=x_sb[:, :, g * GS:(g + 1) * GS], func=AF.Square), reads=["x"], writes=["sq"])
                for k in range(8):
                    P.mm(lambda e, k=k: e.matmul(bank(7), lhsT=ones[:], rhs=sq[:, k, :], start=(k == 0), stop=(k == 7)), reads=["sq", "ones"], writes=["ssps"])
                P.act(lambda e: e.activation(out=rstd, in_=bank(7), func=AF.Ln, scale=1.0 / 1024, bias=epsb[:]), reads=["ssps", "epsb"], writes=["rstd"])
                P.act(lambda e: e.activation(out=bank(7), in_=rstd, func=AF.Exp, scale=-0.5), reads=["rstd"], writes=["ssps"])
                for k in range(8):
                    hb = hn[k % 2]
                    hbn = "hn%d" % (k % 2)
                    P.dve(lambda e, k=k, hb=hb, g=g: e.tensor_tensor(out=hb, in0=x_sb[:, k, g * GS:(g + 1) * GS], in1=bank(7), op=ALU.mult), reads=["x", "ssps"], writes=[hbn])
                    P.act(lambda e, k=k, hb=hb, l=l: e.activation(out=h32[:, k, :], in_=hb, func=AF.Identity, scale=gs2[:, k:k + 1], bias=mod[:, l, 24 + k:25 + k]), reads=[hbn, "gs2", "mod"], writes=["h32_%d" % k])
                    P.dve(lambda e, k=k, g=g: e.tensor_copy(hT[:, k, g * GS:(g + 1) * GS], h32[:, k, :]), reads=["h32_%d" % k], writes=["hT"])
                for tt in range(4):
                    ti = g * 4 + tt
                    for k in range(8):
                        P.mm(lambda e, k=k, tt=tt, ti=ti, l=l: e.matmul(bank(6)[:, ti * 20:(ti + 1) * 20], lhsT=h32[:, k, tt * 128:(tt + 1) * 128], rhs=wr_sb[:, l, k, :], start=(k == 0), stop=(k == 7)),
                             reads=["h32_%d" % k, "wr_sb"], writes=["Rps"])
                for tt in range(4):
                    route_tile(g * 4 + tt)
            stage2 = [None]

            def emit_stage2(e_, g, ab, l=l):
                wg_, wu_, wd_ = wbuf[e_ % 2]
                nm = "w%d" % (e_ % 2)
                for oc in range(8):
                    yb = 4 + (oc % 2)
                    for fc in range(4):
                        P.mm(lambda e, fc=fc, oc=oc, yb=yb, wd_=wd_, ab=ab: e.matmul(bank(yb), lhsT=wd_[:, fc, oc * 128:(oc + 1) * 128], rhs=actb[ab][:, fc, :], start=(fc == 0), stop=(fc == 3)),
                             reads=[nm + "d", "act%d" % ab], writes=["y%d" % yb])
                    P.dve(lambda e, oc=oc, g=g, yb=yb: e.scalar_tensor_tensor(out=x_sb[:, oc, g * GS:(g + 1) * GS], in0=bank(yb), scalar=mod[:, l, 40 + oc:41 + oc], in1=x_sb[:, oc, g * GS:(g + 1) * GS], op0=ALU.mult, op1=ALU.add),
                          reads=["y%d" % yb, "mod", "x"], writes=["x"])
            it = 0
            for e_ in range(16):
                wg_, wu_, wd_ = wbuf[e_ % 2]
                nm = "w%d" % (e_ % 2)
                for g in range(4):
                    ab = it % 2
                    it += 1
                    P.mm(lambda e, e_=e_, g=g: e.matmul(bank(6), lhsT=sel_sb[0:16, e_, :], rhs=gT[0:16, g * GS:(g + 1) * GS], start=True, stop=True), reads=["gT", "sel_sb"], writes=["G"])
                    P.act(lambda e, ab=ab: e.activation(out=gbs[ab], in_=bank(6), func=AF.Copy), reads=["G"], writes=["gbs%d" % ab])
                    for fc in range(4):
                        hb_ = 2 * (fc % 2)
                        for k in range(8):
                            P.mm(lambda e, k=k, fc=fc, g=g, hb_=hb_, wg_=wg_: e.matmul(bank(hb_), lhsT=wg_[:, k, fc * 128:(fc + 1) * 128], rhs=hT[:, k, g * GS:(g + 1) * GS], start=(k == 0), stop=(k == 7)),
                                 reads=[nm + "g", "hT"], writes=["hg%d" % hb_])
                        for k in range(8):
                            P.mm(lambda e, k=k, fc=fc, g=g, hb_=hb_, wu_=wu_: e.matmul(bank(hb_ + 1), lhsT=wu_[:, k, fc * 128:(fc + 1) * 128], rhs=hT[:, k, g * GS:(g + 1) * GS], start=(k == 0), stop=(k == 7)),
                                 reads=[nm + "u", "hT"], writes=["hu%d" % hb_])
                        sbi = fc % 2
                        P.act(lambda e, hb_=hb_, sbi=sbi: e.activation(out=s_sb[sbi], in_=bank(hb_), func=AF.Silu), reads=["hg%d" % hb_], writes=["s%d" % sbi])
                        P.dve(lambda e, hb_=hb_, sbi=sbi: e.tensor_tensor(out=t_sb[sbi], in0=bank(hb_ + 1), in1=s_sb[sbi], op=ALU.mult), reads=["hu%d" % hb_, "s%d" % sbi], writes=["t%d" % sbi])
                        P.pool(lambda e, fc=fc, sbi=sbi, ab=ab: e.tensor_tensor(out=actb[ab][:, fc, :], in0=t_sb[sbi], in1=gbs[ab], op=ALU.mult), reads=["t%d" % sbi, "gbs%d" % ab], writes=["act%d" % ab])
                    if stage2[0] is not None:
                        emit_stage2(*stage2[0])
                    stage2[0] = (e_, g, ab)
                    if g == 0 and e_ + 1 < 16:
                        load_expert(e_ + 1)
            emit_stage2(*stage2[0])
            P.barrier()
        out_ops = []
        for g in range(4):
            P.act(lambda e, g=g: e.activation(out=sq[:], in_=x_sb[:, :, g * GS:(g + 1) * GS], func=AF.Square), reads=["x"], writes=["sq"])
            for k in range(8):
                P.mm(lambda e, k=k: e.matmul(bank(7), lhsT=ones[:], rhs=sq[:, k, :], start=(k == 0), stop=(k == 7)), reads=["sq", "ones"], writes=["ssps"])
            P.act(lambda e: e.activation(out=rstd, in_=bank(7), func=AF.Ln, scale=1.0 / 1024, bias=epsb[:]), reads=["ssps", "epsb"], writes=["rstd"])
            P.act(lambda e: e.activation(out=bank(7), in_=rstd, func=AF.Exp, scale=-0.5), reads=["rstd"], writes=["ssps"])
            for k in range(8):
                hb = hn[k % 2]
                hbn = "hn%d" % (k % 2)
                P.dve(lambda e, k=k, hb=hb, g=g: e.tensor_tensor(out=hb, in0=x_sb[:, k, g * GS:(g + 1) * GS], in1=bank(7), op=ALU.mult), reads=["x", "ssps"], writes=[hbn])
                P.act(lambda e, k=k, hb=hb, g=g: e.activation(out=x_sb[:, k, g * GS:(g + 1) * GS], in_=hb, func=AF.Copy, scale=fngs[:, k:k + 1]), reads=[hbn, "fngs"], writes=["xf"])
        out_ops.append(P.dma("sync", xo, x_sb[:], reads=["xf"]))
        P.emit(final_wait_ops=out_ops)
    return nc


from concourse.bass_utils import run_bass_kernel_spmd
import ml_dtypes

_BF = ml_dtypes.bfloat16
_CACHE = {}


def _fm(a):
    Tn = a.shape[0]
    return np.ascontiguousarray(a.T.reshape(8, 128, Tn).transpose(1, 0, 2))


def _pk(v):
    return np.ascontiguousarray(v.reshape(-1, 128).T)


def _pkl(v):
    return np.ascontiguousarray(np.stack([_pk(v[l]) for l in range(v.shape[0])], axis=1))


def _bc(v):
    return np.ascontiguousarray(np.broadcast_to(v, (128,) + v.shape)).astype(np.float32)


def _mkcst():
    p = np.arange(128)
    c = np.zeros((128, 8), np.float32)
    c[:, 0] = (10000.0 ** (-(2 * (p % 32)).astype(np.float32) / 64)).astype(np.float32)
    c[:, 1] = (10000.0 ** (-(2 * (p % 16)).astype(np.float32) / 32)).astype(np.float32)
    c[:, 2] = np.where((p % 64) < 32, -1.0, 1.0)
    c[:, 3] = np.where((p % 32) < 16, -1.0, 1.0)
    return c


def _mkmasks(r):
    k = np.arange(128)[:, None]
    q = np.arange(128)[None, :]
    own = {}
    oth = {}
    own['A'] = (k <= q)
    oth['A'] = (k < q) if r == 0 else (k <= q)
    own['Bp'] = (k >= q + 65)
    own['Bd'] = ((q - k) >= 0) & ((q - k) <= 63)
    oth['Bp'] = (k >= q + 64) if r == 0 else (k >= q + 65)
    oth['Bd'] = (((q - k) >= 1) & ((q - k) <= 64)) if r == 0 else (((q - k) >= 0) & ((q - k) <= 63))
    own['Cp'] = (k >= q + 64)
    own['Cd'] = ((q - k) >= 0) & ((q - k) <= 64)
    oth['Cp'] = (k >= q + 64) if r == 0 else (k >= q + 65)
    oth['Cd'] = (((q - k) >= 1) & ((q - k) <= 64)) if r == 0 else (((q - k) >= 0) & ((q - k) <= 63))

    def pm(a, key):
        return own[key] if a == r else oth[key]
    m = np.zeros((15, 128, 128), np.float32)
    m[0] = pm(0, 'A'); m[1] = pm(1, 'A')
    m[2] = pm(0, 'Bp'); m[3] = pm(0, 'Bd'); m[4] = pm(1, 'Bp'); m[5] = pm(1, 'Bd')
    m[6] = pm(0, 'Cp'); m[7] = pm(0, 'Cd'); m[8] = pm(1, 'Cp'); m[9] = pm(1, 'Cd')
    m[10] = (k >= q)
    m[11] = (k <= q)
    m[12] = m[10]
    m[13] = m[11]
    m[14] = m[11]
    return np.ascontiguousarray(m.transpose(1, 0, 2)).astype(_BF)


def _mkmasks8():
    k = np.arange(128)[:, None]
    q = np.arange(128)[None, :]
    dp = (k >= q).astype(np.float32)
    dd = (k <= q).astype(np.float32)
    m = np.zeros((4, 128, 512), np.float32)
    for half in range(2):
        sl = slice(64 * half, 64 * half + 64)
        m[half] = np.concatenate([dp[:, sl], dd[:, sl]] * 4, axis=1)
        m[2 + half] = np.concatenate([dd[:, sl]] * 8, axis=1)
    return np.ascontiguousarray(m.transpose(1, 0, 2)).astype(_BF)


def kernel(x, c, positions, ada_w, ada_b, norm_mix_g, norm_ffn_g, w_in, w_out,
           diff_lambda_q1, diff_lambda_k1, diff_lambda_q2, diff_lambda_k2, diff_subln_g,
           swa_sinks, router_group_w, router_group_b, router_expert_w, router_expert_b,
           expert_w_gate, expert_w_up, expert_w_down, final_norm_g):
    f32 = np.float32
    D = DEPTH
    A = lambda v: np.ascontiguousarray(np.asarray(v, f32))
    x = A(x)
    c = A(c)
    positions = np.asarray(positions)
    if "nc" not in _CACHE:
        _CACHE["nc"] = build_fused(D)
    sel = np.zeros((16, 16, 128), f32)
    for e in range(16):
        sel[e, e, :] = 1.0
    small = np.zeros((128, D, 16), f32)
    for l in range(D):
        small[:, l, 0] = 0.8 - 0.6 * math.exp(-0.3 * l)
        small[0:64, l, 1] = A(diff_subln_g)[l]
        small[:, l, 8:16] = A(swa_sinks)[l][None, :]
    lamv = _bc(np.stack([np.stack([A(diff_lambda_q1)[l], A(diff_lambda_k1)[l], A(diff_lambda_q2)[l], A(diff_lambda_k2)[l]]) for l in range(D)]))
    wrc = np.concatenate([A(router_group_w), A(router_expert_w)], axis=2)
    wr = np.ascontiguousarray(wrc.reshape(D, 8, 128, 20).transpose(2, 0, 1, 3))
    rb = _bc(np.concatenate([A(router_group_b), A(router_expert_b)], axis=1))
    common = dict(cst=_mkcst(), ngm=_pkl(A(norm_mix_g)), ngf=_pkl(A(norm_ffn_g)),
                  w_in=A(w_in), w_out=A(w_out), lamv=lamv, small=small, wr=wr, rbias=rb, ident=np.eye(128, dtype=f32), sel=sel,
                  wgate=A(expert_w_gate), wup=A(expert_w_up), wdown=A(expert_w_down), fng=_pk(A(final_norm_g)))
    aw = A(ada_w)
    ab = A(ada_b)
    adaw_h = [np.ascontiguousarray(aw[:, :, 3072 * r:3072 * (r + 1)]) for r in range(2)]
    adab_h = [_pkl(np.ascontiguousarray(ab[:, 3072 * r:3072 * (r + 1)])) for r in range(2)]
    in_maps = []
    for core in range(8):
        b, r = core // 2, core % 2
        m = dict(common)
        m.update(xT=_fm(x[b, r::2, :]), cT=_pk(c[b]), ada_w=adaw_h[r], ada_b=adab_h[r],
                 posb=np.ascontiguousarray(np.broadcast_to(positions[b, r::2][None, :], (128, 2048))).astype(np.int32),
                 masks=_mkmasks(r), masks8=_mkmasks8())
        in_maps.append(m)
    res = run_bass_kernel_spmd(_CACHE["nc"], in_maps, core_ids=list(range(8))).results
    out = np.zeros((4, 4096, 1024), f32)
    for i in range(8):
        b, r = i // 2, i % 2
        out[b, r::2, :] = np.asarray(res[i]["xo"], f32).transpose(1, 0, 2).reshape(1024, 2048).T
    return out
```
